# Optimizing a Trainium2 kernel written in Bass

```python
import math
import jax, jax.numpy as jnp
from jax import lax
import numpy as np

D_MODEL = 1024
BATCH = 8
SEQ = 4096
DEPTH = 1

NSA_HEADS = 8
NSA_KV_HEADS = 2
NSA_GROUP = NSA_HEADS // NSA_KV_HEADS
HEAD_DIM = 64
CMP_LEN = 32
CMP_STRIDE = 16
CMP_HIDDEN = 64
SLC_BLOCK = 64
SLC_TOPN = 16
WINDOW = 512
Q_CHUNK = 64
FORCED_BONUS = 1.0e4
DN_HEADS = 8
DN_KDIM = 64
DN_VDIM = 64
DN_CONV = 4
DN_CHUNK = 64
DT_MIN = 0.001
DT_MAX = 0.1
PEER_HEADS = 8
PEER_NKEYS = 128
PEER_EXPERTS = PEER_NKEYS * PEER_NKEYS
PEER_DKEY = 128
PEER_HALF = PEER_DKEY // 2
PEER_TOPK = 16
PEER_TOK_CHUNK = 128
REL_BUCKETS = 32
REL_MAX_DIST = 128
EPS = 1e-6
NEG_INF = -1e30

NSA_Q_W = NSA_HEADS * HEAD_DIM
NSA_KV_W = NSA_KV_HEADS * HEAD_DIM
DN_QK_W = DN_HEADS * DN_KDIM
DN_V_W = DN_HEADS * DN_VDIM
DN_CONV_W = 2 * DN_QK_W + DN_V_W
IN_SPLIT_SIZES = (NSA_Q_W, NSA_KV_W, NSA_KV_W, NSA_KV_W, NSA_KV_W, NSA_KV_W, NSA_KV_W, NSA_HEADS * 3, DN_QK_W, DN_QK_W, DN_V_W, DN_V_W, DN_HEADS, DN_HEADS, D_MODEL, D_MODEL)
IN_COLS = sum(IN_SPLIT_SIZES)
IN_SPLIT_POINTS = tuple(int(v) for v in np.cumsum(IN_SPLIT_SIZES)[:-1])

kernel_name = 'hybrid_nsa_gdn_peer_block'


def rms_norm(x, g):
    xf = x.astype(jnp.float32)
    y = xf * lax.rsqrt(jnp.mean(xf * xf, axis=-1, keepdims=True) + EPS)
    return (y * g.astype(jnp.float32)).astype(x.dtype)


def l2_normalize(x):
    xf = x.astype(jnp.float32)
    return (xf * lax.rsqrt(jnp.sum(xf * xf, axis=-1, keepdims=True) + EPS)).astype(x.dtype)


def masked_softmax(logits, mask):
    l = jnp.where(mask, logits.astype(jnp.float32), NEG_INF)
    m = jnp.max(l, axis=-1, keepdims=True)
    e = jnp.where(mask, jnp.exp(l - m), 0.0)
    return e / jnp.maximum(jnp.sum(e, axis=-1, keepdims=True), 1e-30)


def rel_bucket(dist):
    dist = jnp.maximum(dist, 0)
    max_exact = REL_BUCKETS // 2
    scaled = jnp.log(jnp.maximum(dist, 1).astype(jnp.float32) / max_exact) / math.log(REL_MAX_DIST / max_exact)
    large = jnp.minimum(max_exact + (scaled * (REL_BUCKETS - max_exact)).astype(jnp.int32), REL_BUCKETS - 1)
    return jnp.where(dist < max_exact, dist, large)


def causal_depthwise_conv(x, w):
    return lax.conv_general_dilated(x, w[:, None, :].astype(x.dtype), (1,), [(DN_CONV - 1, 0)], dimension_numbers=('NWC', 'WIO', 'NWC'), feature_group_count=x.shape[-1])


def compress_blocks(kv, blk_idx, pos_emb, w1, w2):
    B = kv.shape[0]
    n_cmp = blk_idx.shape[0]
    blk = kv[:, blk_idx] + pos_emb[None, None, :, None, :]
    blk = blk.transpose(0, 1, 3, 2, 4).reshape(B, n_cmp, NSA_KV_HEADS, CMP_LEN * HEAD_DIM)
    return jnp.einsum('bnhe,ed->bhnd', jax.nn.gelu(blk @ w1), w2)


def nsa_attention(q, k_cmp, v_cmp, k_slc, v_slc, k_win, v_win, gate_logits, rel_bias, cmp_pos_emb, w_ck1, w_ck2, w_cv1, w_cv2):
    B, S, _ = q.shape
    KV, G, DH = NSA_KV_HEADS, NSA_GROUP, HEAD_DIM
    q = q.reshape(B, S, KV, G, DH) * (DH ** -0.5)
    k_cmp, v_cmp, k_slc, v_slc, k_win, v_win = (t.reshape(B, S, KV, DH) for t in (k_cmp, v_cmp, k_slc, v_slc, k_win, v_win))
    dist_bias = rel_bias[rel_bucket(jnp.arange(S))].T.reshape(KV, G, S)
    n_cmp = (S - CMP_LEN) // CMP_STRIDE + 1
    cmp_start = jnp.arange(n_cmp) * CMP_STRIDE
    cmp_end = cmp_start + CMP_LEN - 1
    blk_idx = cmp_start[:, None] + jnp.arange(CMP_LEN)[None, :]
    kc = compress_blocks(k_cmp, blk_idx, cmp_pos_emb, w_ck1, w_ck2)
    vc = compress_blocks(v_cmp, blk_idx, cmp_pos_emb, w_cv1, w_cv2)
    n_slc = S // SLC_BLOCK
    top_n = min(SLC_TOPN, n_slc)
    slc_ids = jnp.arange(n_slc)
    slc_start = slc_ids * SLC_BLOCK
    overlap = ((cmp_start[:, None] <= slc_start[None, :] + SLC_BLOCK - 1) & (cmp_end[:, None] >= slc_start[None, :])).astype(q.dtype)
    ks = k_slc.reshape(B, n_slc, SLC_BLOCK, KV, DH).transpose(0, 3, 1, 2, 4)
    vs = v_slc.reshape(B, n_slc, SLC_BLOCK, KV, DH).transpose(0, 3, 1, 2, 4)
    gather_blocks = jax.vmap(jax.vmap(lambda tab, ix: tab[ix]))
    bias_lookup = jax.vmap(lambda tb, d: tb[:, d], in_axes=(0, 1))
    kw = jnp.pad(k_win, ((0, 0), (WINDOW, 0), (0, 0), (0, 0)))
    vw = jnp.pad(v_win, ((0, 0), (WINDOW, 0), (0, 0), (0, 0)))

    def chunk(i):
        s0 = i * Q_CHUNK
        t = s0 + jnp.arange(Q_CHUNK)
        qc = lax.dynamic_slice_in_dim(q, s0, Q_CHUNK, axis=1)
        valid_c = cmp_end[None, :] <= t[:, None]
        bias_c = dist_bias[:, :, jnp.clip(t[:, None] - cmp_end[None, :], 0, S - 1)]
        p_c = masked_softmax(jnp.einsum('bqhgd,bhnd->bhgqn', qc, kc) + bias_c, valid_c).astype(q.dtype)
        o_c = jnp.einsum('bhgqn,bhnd->bqhgd', p_c, vc)
        imp = jnp.einsum('bhgqn,nj->bhqj', p_c, overlap).astype(jnp.float32)
        cur = t[:, None] // SLC_BLOCK
        forced = (slc_ids[None, :] == 0) | (slc_ids[None, :] == cur) | (slc_ids[None, :] == cur - 1)
        future = slc_start[None, :] > t[:, None]
        score = jnp.where(future, NEG_INF, imp + jnp.where(forced, FORCED_BONUS, 0.0))
        _, sel = lax.top_k(score, top_n)
        k_sel = gather_blocks(ks, sel)
        v_sel = gather_blocks(vs, sel)
        pos = sel[..., None] * SLC_BLOCK + jnp.arange(SLC_BLOCK)
        valid_s = (pos <= t[:, None, None]).reshape(B, KV, 1, Q_CHUNK, top_n * SLC_BLOCK)
        bias_s = bias_lookup(dist_bias, jnp.clip(t[:, None, None] - pos, 0, S - 1)).transpose(2, 0, 1, 3, 4, 5)
        logit_s = (jnp.einsum('bqhgd,bhqnkd->bhgqnk', qc, k_sel) + bias_s).reshape(B, KV, G, Q_CHUNK, top_n * SLC_BLOCK)
        p_s = masked_softmax(logit_s, valid_s).astype(q.dtype).reshape(B, KV, G, Q_CHUNK, top_n, SLC_BLOCK)
        o_s = jnp.einsum('bhgqnk,bhqnkd->bqhgd', p_s, v_sel)
        kpos = s0 - WINDOW + jnp.arange(Q_CHUNK + WINDOW)
        k_w = lax.dynamic_slice_in_dim(kw, s0, Q_CHUNK + WINDOW, axis=1)
        v_w = lax.dynamic_slice_in_dim(vw, s0, Q_CHUNK + WINDOW, axis=1)
        rel = t[:, None] - kpos[None, :]
        valid_w = (kpos[None, :] >= 0) & (rel >= 0) & (rel < WINDOW)
        bias_w = dist_bias[:, :, jnp.clip(rel, 0, S - 1)]
        p_w = masked_softmax(jnp.einsum('bqhgd,bkhd->bhgqk', qc, k_w) + bias_w, valid_w).astype(q.dtype)
        o_w = jnp.einsum('bhgqk,bkhd->bqhgd', p_w, v_w)
        return o_c, o_s, o_w

    o_c, o_s, o_w = lax.map(chunk, jnp.arange(S // Q_CHUNK))

    def unchunk(o):
        return jnp.moveaxis(o, 0, 1).reshape(B, S, KV, G, DH)

    gates = jax.nn.sigmoid(gate_logits.reshape(B, S, KV, G, 3))
    o = gates[..., 0:1] * unchunk(o_c) + gates[..., 1:2] * unchunk(o_s) + gates[..., 2:3] * unchunk(o_w)
    return o.reshape(B, S, NSA_Q_W)


def chunk_gated_delta_rule(q, k, v, g, beta):
    B, S, H, DK = q.shape
    DV = v.shape[-1]
    C = DN_CHUNK
    n = S // C
    f32 = jnp.float32

    def chunks(t):
        t = t.astype(f32).reshape((B, n, C, H) + t.shape[3:])
        return jnp.moveaxis(t, 3, 1)

    q, k, v, g, beta = (chunks(t) for t in (q, k, v, g, beta))
    gc = jnp.cumsum(g, axis=-1)
    idx = jnp.arange(C)
    tril = idx[:, None] >= idx[None, :]
    strict = idx[:, None] > idx[None, :]
    diff = gc[..., :, None] - gc[..., None, :]
    decay = jnp.where(tril, jnp.exp(jnp.where(tril, diff, 0.0)), 0.0)
    kb = k * beta[..., None]
    vb = v * beta[..., None]
    a_mat = jnp.where(strict, jnp.einsum('bhnid,bhnjd->bhnij', kb, k) * decay, 0.0) + jnp.eye(C, dtype=f32)
    rhs = jnp.concatenate([vb, kb * jnp.exp(gc)[..., None]], axis=-1)
    sol = lax.linalg.triangular_solve(a_mat, rhs, left_side=True, lower=True, unit_diagonal=True)
    u, w = sol[..., :DV], sol[..., DV:]
    attn = jnp.einsum('bhnid,bhnjd->bhnij', q, k) * decay
    q_dec = q * jnp.exp(gc)[..., None]
    k_dec = k * jnp.exp(gc[..., -1:] - gc)[..., None]
    g_last = jnp.exp(gc[..., -1])

    def step(state, inp):
        q_i, k_i, u_i, w_i, a_i, gl_i = inp
        v_new = u_i - jnp.einsum('bhck,bhkv->bhcv', w_i, state)
        o_i = jnp.einsum('bhck,bhkv->bhcv', q_i, state) + jnp.einsum('bhij,bhjv->bhiv', a_i, v_new)
        state = state * gl_i[..., None, None] + jnp.einsum('bhck,bhcv->bhkv', k_i, v_new)
        return state, o_i

    xs = tuple(jnp.moveaxis(t, 2, 0) for t in (q_dec, k_dec, u, w, attn, g_last))
    _, o = lax.scan(step, jnp.zeros((B, H, DK, DV), f32), xs)
    return jnp.transpose(o, (1, 0, 3, 2, 4)).reshape(B, S, H, DV)


def gated_deltanet(q, k, v, z, b_logits, a_logits, conv_w, A_log, dt_bias, norm_g):
    B, S, _ = q.shape
    f32 = jnp.float32
    qkv = jax.nn.silu(causal_depthwise_conv(jnp.concatenate([q, k, v], axis=-1), conv_w))
    q, k, v = jnp.split(qkv, [DN_QK_W, 2 * DN_QK_W], axis=-1)
    q = l2_normalize(q.reshape(B, S, DN_HEADS, DN_KDIM)) * (DN_KDIM ** -0.5)
    k = l2_normalize(k.reshape(B, S, DN_HEADS, DN_KDIM))
    v = v.reshape(B, S, DN_HEADS, DN_VDIM)
    beta = jax.nn.sigmoid(b_logits.astype(f32))
    g = -jnp.exp(A_log.astype(f32)) * jax.nn.softplus(a_logits.astype(f32) + dt_bias.astype(f32))
    o = chunk_gated_delta_rule(q, k, v, g, beta).astype(z.dtype)
    o = rms_norm(o, norm_g) * jax.nn.silu(z.reshape(B, S, DN_HEADS, DN_VDIM))
    return o.reshape(B, S, DN_V_W)


def token_mixer(h, rel_bias, w_in, cmp_pos_emb, w_ck1, w_ck2, w_cv1, w_cv2, conv_w, A_log, dt_bias, dn_norm_g, w_branch_nsa, w_branch_dn, w_out):
    (nsa_q, k_c, v_c, k_s, v_s, k_w, v_w, nsa_gates, dn_q, dn_k, dn_v, dn_z, dn_b, dn_a, gate_nsa, gate_dn) = jnp.split(h @ w_in, list(IN_SPLIT_POINTS), axis=-1)
    o_nsa = nsa_attention(nsa_q, k_c, v_c, k_s, v_s, k_w, v_w, nsa_gates, rel_bias, cmp_pos_emb, w_ck1, w_ck2, w_cv1, w_cv2)
    o_dn = gated_deltanet(dn_q, dn_k, dn_v, dn_z, dn_b, dn_a, conv_w, A_log, dt_bias, dn_norm_g)
    merged = jax.nn.sigmoid(gate_nsa) * (o_nsa @ w_branch_nsa) + jax.nn.sigmoid(gate_dn) * (o_dn @ w_branch_dn)
    return merged @ w_out


def peer_ffn(h, w_query, keys1, keys2, expert_u, expert_v):
    B, S, D = h.shape
    qry = (h @ w_query).reshape(B, S, PEER_HEADS, PEER_DKEY)
    s1 = jnp.einsum('bshd,hkd->bshk', qry[..., :PEER_HALF], keys1).astype(jnp.float32)
    s2 = jnp.einsum('bshd,hkd->bshk', qry[..., PEER_HALF:], keys2).astype(jnp.float32)
    v1, i1 = lax.top_k(s1, PEER_TOPK)
    v2, i2 = lax.top_k(s2, PEER_TOPK)
    n_cand = PEER_TOPK * PEER_TOPK
    cand_s = (v1[..., :, None] + v2[..., None, :]).reshape(B, S, PEER_HEADS, n_cand)
    cand_i = (i1[..., :, None] * PEER_NKEYS + i2[..., None, :]).reshape(B, S, PEER_HEADS, n_cand)
    top_s, top_pos = lax.top_k(cand_s, PEER_TOPK)
    expert_idx = jnp.take_along_axis(cand_i, top_pos, axis=-1)
    gate = jax.nn.softmax(top_s, axis=-1).astype(h.dtype)
    n_chunks = (B * S) // PEER_TOK_CHUNK
    E = PEER_HEADS * PEER_TOPK
    xs = (h.reshape(n_chunks, PEER_TOK_CHUNK, D), expert_idx.reshape(n_chunks, PEER_TOK_CHUNK, E), gate.reshape(n_chunks, PEER_TOK_CHUNK, E))

    def chunk(args):
        xc, ic, gc = args
        act = jax.nn.gelu(jnp.einsum('td,ted->te', xc, expert_u[ic]))
        return jnp.einsum('te,ted->td', gc * act, expert_v[ic])

    return lax.map(chunk, xs).reshape(B, S, D)


def setup_inputs(seed: int = 0) -> dict:
    key = jax.random.key(seed)
    ks = jax.random.split(key, 32)
    L = DEPTH
    f32 = jnp.float32

    def nrm(k, shape, scale):
        return jax.random.normal(k, shape, f32) * scale

    dt = jnp.exp(jax.random.uniform(ks[16], (L, DN_HEADS), f32, math.log(DT_MIN), math.log(DT_MAX)))
    return {
        'x': nrm(ks[0], (BATCH, SEQ, D_MODEL), 1.0),
        'c': nrm(ks[1], (BATCH, D_MODEL), 1.0),
        'rel_bias': nrm(ks[2], (REL_BUCKETS, NSA_HEADS), 0.2),
        'final_g': 1.0 + nrm(ks[3], (D_MODEL,), 0.02),
        'w_ada': nrm(ks[4], (L, D_MODEL, 6 * D_MODEL), 0.5 * D_MODEL ** -0.5),
        'b_ada': nrm(ks[5], (L, 6 * D_MODEL), 0.02),
        'norm1_g': 1.0 + nrm(ks[6], (L, D_MODEL), 0.02),
        'w_in': nrm(ks[7], (L, D_MODEL, IN_COLS), D_MODEL ** -0.5),
        'cmp_pos_emb': nrm(ks[8], (L, CMP_LEN, HEAD_DIM), 0.1),
        'w_cmp_k1': nrm(ks[9], (L, CMP_LEN * HEAD_DIM, CMP_HIDDEN), (CMP_LEN * HEAD_DIM) ** -0.5),
        'w_cmp_k2': nrm(ks[10], (L, CMP_HIDDEN, HEAD_DIM), CMP_HIDDEN ** -0.5),
        'w_cmp_v1': nrm(ks[11], (L, CMP_LEN * HEAD_DIM, CMP_HIDDEN), (CMP_LEN * HEAD_DIM) ** -0.5),
        'w_cmp_v2': nrm(ks[12], (L, CMP_HIDDEN, HEAD_DIM), CMP_HIDDEN ** -0.5),
        'dn_conv_w': nrm(ks[13], (L, DN_CONV, DN_CONV_W), DN_CONV ** -0.5),
        'dn_A_log': jnp.log(jax.random.uniform(ks[15], (L, DN_HEADS), f32, 1.0, 16.0)),
        'dn_dt_bias': dt + jnp.log(-jnp.expm1(-dt)),
        'dn_norm_g': 1.0 + nrm(ks[17], (L, DN_VDIM), 0.02),
        'w_branch_nsa': nrm(ks[18], (L, NSA_Q_W, D_MODEL), NSA_Q_W ** -0.5),
        'w_branch_dn': nrm(ks[19], (L, DN_V_W, D_MODEL), DN_V_W ** -0.5),
        'w_out': nrm(ks[20], (L, D_MODEL, D_MODEL), D_MODEL ** -0.5),
        'norm2_g': 1.0 + nrm(ks[21], (L, D_MODEL), 0.02),
        'peer_w_query': nrm(ks[22], (L, D_MODEL, PEER_HEADS * PEER_DKEY), D_MODEL ** -0.5),
        'peer_keys1': nrm(ks[23], (L, PEER_HEADS, PEER_NKEYS, PEER_HALF), PEER_HALF ** -0.5),
        'peer_keys2': nrm(ks[24], (L, PEER_HEADS, PEER_NKEYS, PEER_HALF), PEER_HALF ** -0.5),
        'peer_u': nrm(ks[25], (L, PEER_EXPERTS, D_MODEL), D_MODEL ** -0.5),
        'peer_v': nrm(ks[26], (L, PEER_EXPERTS, D_MODEL), 1.0),
    }


def reference(x, c, rel_bias, final_g, w_ada, b_ada, norm1_g, w_in, cmp_pos_emb, w_cmp_k1, w_cmp_k2, w_cmp_v1, w_cmp_v2, dn_conv_w, dn_A_log, dn_dt_bias, dn_norm_g, w_branch_nsa, w_branch_dn, w_out, norm2_g, peer_w_query, peer_keys1, peer_keys2, peer_u, peer_v):
    cond = jax.nn.silu(c)
    for l in range(DEPTH):
        mod = cond @ w_ada[l] + b_ada[l]
        sh1, sc1, g1, sh2, sc2, g2 = (m[:, None, :] for m in jnp.split(mod, 6, axis=-1))
        h = rms_norm(x, norm1_g[l]) * (1.0 + sc1) + sh1
        x = x + g1 * token_mixer(h, rel_bias, w_in[l], cmp_pos_emb[l], w_cmp_k1[l], w_cmp_k2[l], w_cmp_v1[l], w_cmp_v2[l], dn_conv_w[l], dn_A_log[l], dn_dt_bias[l], dn_norm_g[l], w_branch_nsa[l], w_branch_dn[l], w_out[l])
        h = rms_norm(x, norm2_g[l]) * (1.0 + sc2) + sh2
        x = x + g2 * peer_ffn(h, peer_w_query[l], peer_keys1[l], peer_keys2[l], peer_u[l], peer_v[l])
    return rms_norm(x, final_g)
```

```python
import numpy as np
import concourse.bass as bass
import concourse.mybir as mybir
from contextlib import ExitStack

F32 = mybir.dt.float32
BF16 = mybir.dt.bfloat16
U32 = mybir.dt.uint32
I32 = mybir.dt.int32
ALU = mybir.AluOpType
AF = mybir.ActivationFunctionType
AX = mybir.AxisListType

EPOCH = 1 << 30
ENGS = ("pe", "act", "dve", "pool", "sp")
WRITE_KW = ("out", "accum_out", "out_max", "out_indices")


class Res:
    __slots__ = ("name", "last_w", "readers", "children", "parent")

    def __init__(self, name, parent=None):
        self.name = name
        self.last_w = None
        self.readers = []
        self.children = {}
        self.parent = parent


class Op:
    __slots__ = ("eng", "fn", "deps", "needed", "is_dma", "sig", "idx", "slotwait", "name")

    def __init__(self, eng, fn, is_dma=False, name=""):
        self.eng = eng
        self.fn = fn
        self.deps = []
        self.needed = False
        self.is_dma = is_dma
        self.sig = None
        self.slotwait = None
        self.name = name


class Sched:
    def __init__(self, nc, dma_slots=None):
        self.nc = nc
        self.ops = {e: [] for e in ENGS}
        self.res = {}
        self.tags = {}
        self._keep = []
        self.dma_count = {e: 0 for e in ENGS}
        self.dma_ops = {e: [] for e in ENGS}
        self.dma_slots = dma_slots or {"sp": 8, "pool": 12, "act": 4}
        self.stack = ExitStack()
        self.scopes = []
        self.nbar = {}
        self.all_ops = []

    def push(self):
        self.scopes.append(ExitStack())

    def pop(self):
        self.barrier()
        self.scopes.pop().close()

    def sbuf(self, name, shape, dtype):
        t = (self.scopes[-1] if self.scopes else self.stack).enter_context(self.nc.sbuf_tensor(name, list(shape), dtype))
        self.res[t.name] = Res(t.name)
        return t

    def psum(self, name, shape, dtype):
        t = self.stack.enter_context(self.nc.psum_tensor(name, list(shape), dtype))
        self.res[t.name] = Res(t.name)
        return t

    def dram(self, name, shape, dtype, kind="Internal"):
        t = self.nc.dram_tensor(name, list(shape), dtype, kind=kind)
        self.res[t.name] = Res(t.name)
        return t

    def tag(self, ap, key):
        base = self.res[ap.name]
        if key not in base.children:
            base.children[key] = Res(base.name + ":" + str(key), parent=base)
        self.tags[id(ap)] = base.children[key]
        self._keep.append(ap)
        return ap

    def _res_of(self, ap):
        r = self.tags.get(id(ap))
        if r is not None:
            return r
        nm = ap.name
        if nm not in self.res:
            self.res[nm] = Res(nm)
        return self.res[nm]

    def _related(self, r):
        out = [r]
        if r.parent is not None:
            out.append(r.parent)
        out.extend(r.children.values())
        return out

    def _track(self, op, reads, writes):
        deps = set()
        for r in reads:
            for rr in self._related(r):
                if rr.last_w is not None:
                    deps.add(rr.last_w)
        for w in writes:
            for rr in self._related(w):
                if rr.last_w is not None:
                    deps.add(rr.last_w)
                for q in rr.readers:
                    deps.add(q)
        deps.discard(op)
        for r in reads:
            r.readers.append(op)
        for w in writes:
            w.last_w = op
            w.readers = []
        best = {}
        final = []
        for d in deps:
            if d.is_dma:
                final.append(d)
            else:
                if d.eng == "pe" and op.eng == "pe":
                    continue
                b = best.get(d.eng)
                if b is None or d.idx > b.idx:
                    best[d.eng] = d
        final.extend(best.values())
        for d in final:
            d.needed = True
        op.deps = final

    def _add(self, op, reads, writes):
        op.idx = len(self.ops[op.eng])
        self.ops[op.eng].append(op)
        self.all_ops.append(op)
        self._track(op, reads, writes)
        return op

    def I(self, eng, meth, *args, extra_reads=(), extra_writes=(), **kw):
        reads, writes = [], []
        for k, v in kw.items():
            if hasattr(v, "tensor") and hasattr(v, "ap"):
                (writes if k in WRITE_KW else reads).append(self._res_of(v))
        for v in args:
            if hasattr(v, "tensor") and hasattr(v, "ap"):
                reads.append(self._res_of(v))
        for v in extra_reads:
            reads.append(self._res_of(v))
        for v in extra_writes:
            writes.append(self._res_of(v))

        def fn(e, meth=meth, args=args, kw=kw):
            return getattr(e, meth)(*args, **kw)

        return self._add(Op(eng, fn, name=meth), reads, writes)

    def dma(self, out, in_, q="sp", **kw):
        def fn(e, out=out, in_=in_, kw=kw):
            return e.dma_start(out=out, in_=in_, **kw)

        op = Op(q, fn, is_dma=True, name="dma")
        op.needed = True
        d = self.dma_count[q]
        self.dma_count[q] += 1
        op.sig = ("dma", q, d)
        self.dma_ops[q].append(op)
        return self._add(op, [self._res_of(in_)], [self._res_of(out)])

    def idma(self, out, in_, idx_ap, axis=0, **kw):
        def fn(e, out=out, in_=in_, idx_ap=idx_ap, kw=kw):
            return e.indirect_dma_start(out=out, out_offset=None, in_=in_,
                                        in_offset=bass.IndirectOffsetOnAxis(ap=idx_ap, axis=axis), **kw)

        q = "pool"
        op = Op(q, fn, is_dma=True, name="idma")
        op.needed = True
        d = self.dma_count[q]
        self.dma_count[q] += 1
        op.sig = ("dma", q, d)
        self.dma_ops[q].append(op)
        return self._add(op, [self._res_of(in_), self._res_of(idx_ap)], [self._res_of(out)])

    def barrier(self):
        marks = []
        for e in ENGS:
            last = None
            for op in reversed(self.ops[e]):
                if (not op.is_dma) and op.fn is not None and op.fn != "SEMINC":
                    last = op
                    break
            if last is not None:
                last.needed = True
                marks.append(last)
            P = self.dma_slots.get(e, 0)
            if P and self.dma_ops[e]:
                sg = Op(e, "SEMINC", name="barsig")
                sg.idx = len(self.ops[e])
                sg.deps = list(self.dma_ops[e][-P:])
                sg.needed = True
                self.nbar[e] = self.nbar.get(e, 0) + 1
                sg.sig = ("b", e, self.nbar[e])
                self.ops[e].append(sg)
                self.all_ops.append(sg)
                marks.append(sg)
        for e in ENGS:
            op = Op(e, None, name="barwait")
            op.idx = len(self.ops[e])
            op.deps = [m for m in marks if m.eng != e]
            self.ops[e].append(op)
            self.all_ops.append(op)
        for r in self.res.values():
            r.last_w = None
            r.readers = []
            for c in r.children.values():
                c.last_w = None
                c.readers = []

    def final_wait(self, dma_ops):
        op = Op("sp", None, name="finalwait")
        op.idx = len(self.ops["sp"])
        op.deps = list(dma_ops)
        self.ops["sp"].append(op)

    def emit(self):
        nc = self.nc
        sems = {}
        nsig = {}
        for e in ENGS:
            k = 0
            for op in self.ops[e]:
                if op.is_dma:
                    continue
                if op.needed and op.fn is not None and op.fn != "SEMINC":
                    op.sig = ("c", e, k)
                    k += 1
            nsig[e] = k
            n_ep = (k + EPOCH - 1) // EPOCH
            sems[e] = [self.stack.enter_context(nc.semaphore(f"s_{e}_{i}")) for i in range(n_ep)]
        dsems = {}
        for e in ENGS:
            if self.dma_count[e] > 0:
                P = self.dma_slots[e]
                dsems[e] = [self.stack.enter_context(nc.semaphore(f"d_{e}_{i}")) for i in range(P)]
                pass

        bsems = {e: self.stack.enter_context(nc.semaphore(f"b_{e}")) for e in self.nbar}

        def sigval(op):
            kind, e, k = op.sig
            if kind == "b":
                return bsems[e], k
            if kind == "c":
                return sems[e][k // EPOCH], k % EPOCH + 1
            P = self.dma_slots[e]
            return dsems[e][k % P], 16 * (k // P + 1)

        self.stats = {e: len(self.ops[e]) for e in ENGS}

        def run(e, eng):
            waited = {}

            def wait(sem, val):
                key = sem.num if hasattr(sem, "num") else id(sem)
                if waited.get(key, 0) >= val:
                    return
                eng.wait_ge(sem, val)
                waited[key] = val

            for op in self.ops[e]:
                for d in op.deps:
                    s, v = sigval(d)
                    wait(s, v)
                if op.is_dma:
                    kind, q, k = op.sig
                    P = self.dma_slots[q]
                    if k >= P:
                        wait(dsems[q][k % P], 16 * (k // P))
                if op.fn is None:
                    continue
                if op.fn == "SEMINC":
                    s, v = sigval(op)
                    eng.sem_inc(s, 1)
                    continue
                ins = op.fn(eng)
                if op.needed:
                    s, v = sigval(op)
                    ins.then_inc(s, 16 if op.is_dma else 1)

        with nc.Block() as block:
            @block.tensor
            def _(eng):
                run("pe", eng)

            @block.scalar
            def _(eng):
                run("act", eng)

            @block.vector
            def _(eng):
                run("dve", eng)

            @block.gpsimd
            def _(eng):
                run("pool", eng)

            @block.sync
            def _(eng):
                run("sp", eng)

    def close(self):
        self.stack.close()
import math
import ml_dtypes
from concourse.bass_utils import run_bass_kernel_spmd

SEQ = 4096
DM = 1024
NT = SEQ // 128
IN_COLS = 5416
C_Q, C_KC, C_VC, C_KS, C_VS, C_KW, C_VW, C_GT = 0, 512, 640, 768, 896, 1024, 1152, 1280
C_DQ, C_DK, C_DV, C_DZ, C_DB, C_DA, C_GN, C_GD = 1304, 1816, 2328, 2840, 3352, 3360, 3368, 4392
NEG = -30000.0
EPS = 1e-6
GELU_C = 1.5957691216057308


def _rel_bucket(dist):
    dist = np.maximum(dist, 0)
    scaled = np.log(np.maximum(dist, 1).astype(np.float32) / 16) / np.float32(math.log(128 / 16))
    large = np.minimum(16 + (scaled.astype(np.float32) * 16).astype(np.int32), 31)
    return np.where(dist < 16, dist, large)


def host_consts():
    c = {}
    c["ident_f"] = np.eye(128, dtype=np.float32)
    c["ident_b"] = np.eye(128, dtype=np.float32).astype(ml_dtypes.bfloat16)
    c["anti_f"] = np.eye(128, dtype=np.float32)[::-1].copy()
    m = np.arange(16384) - 8192
    bucket = _rel_bucket(m)
    oh = np.zeros((33, 16384), np.float32)
    pos = m >= 0
    oh[bucket[pos], np.nonzero(pos)[0]] = 1.0
    oh[31, pos] -= 1.0
    oh[32, ~pos] = 1.0
    c["ohp"] = oh
    t = np.arange(SEQ)
    cur = t // 64
    j = np.arange(64)[None, :]
    forced = (j == 0) | (j == cur[:, None]) | (j == cur[:, None] - 1)
    future = j * 64 > t[:, None]
    c["slc_add"] = np.where(future, np.float32(-1e30), np.where(forced, np.float32(1e4), np.float32(0))).astype(np.float32)
    c["e_blk"] = (np.arange(SEQ)[None, :] // 64 == np.arange(64)[:, None]).astype(np.float32).astype(ml_dtypes.bfloat16)
    p = np.arange(128)[:, None]
    f = np.arange(128)[None, :]
    c["w4t"] = np.where(f >= p, np.float32(NEG), np.float32(0)).astype(ml_dtypes.bfloat16)
    n = np.arange(255)
    cs, ce = n * 16, n * 16 + 31
    ss = np.arange(64) * 64
    c["overlap"] = ((cs[:, None] <= ss[None, :] + 63) & (ce[:, None] >= ss[None, :])).astype(np.float32)
    c["m_cum"] = (p[:64] <= f[:, :64]).astype(np.float32)
    c["m_incl_u"] = np.where(p[:64] <= f[:, :64], 0.0, NEG).astype(np.float32)
    c["m_incl_l"] = np.where(f[:, :64] <= p[:64], 0.0, NEG).astype(np.float32)
    c["m_nstr_u"] = np.where(p[:64] < f[:, :64], -1.0, 0.0).astype(np.float32)
    c["m_nstr_l"] = np.where(f[:, :64] < p[:64], -1.0, 0.0).astype(np.float32)
    c["iota16"] = np.tile(np.arange(16, dtype=np.float32)[None, :], (128, 1))
    return c


CONST_SPECS = {
    "ident_f": ([128, 128], F32), "ident_b": ([128, 128], BF16), "anti_f": ([128, 128], F32),
    "ohp": ([33, 16384], F32), "slc_add": ([SEQ, 64], F32), "e_blk": ([64, SEQ], BF16),
    "w4t": ([128, 128], BF16), "overlap": ([255, 64], F32),
    "m_cum": ([64, 64], F32), "m_incl_u": ([64, 64], F32), "m_incl_l": ([64, 64], F32),
    "m_nstr_u": ([64, 64], F32), "m_nstr_l": ([64, 64], F32), "iota16": ([128, 16], F32),
}

INPUT_SPECS = {
    "x": [SEQ, DM], "c": [DM], "rel_bias": [32, 8], "final_g": [1, DM], "w_ada": [DM, 6 * DM], "b_ada": [1, 6 * DM],
    "norm1_g": [1, DM], "w_in": [DM, IN_COLS], "cmp_pos_emb": [32, 64], "w_cmp_k1": [2048, 64], "w_cmp_k2": [64, 64],
    "w_cmp_v1": [2048, 64], "w_cmp_v2": [64, 64], "dn_conv_w": [4, 1536], "dn_A_log": [1, 8], "dn_dt_bias": [1, 8],
    "dn_norm_g": [1, 64], "w_branch_nsa": [512, DM], "w_branch_dn": [512, DM], "w_out": [DM, DM], "norm2_g": [1, DM],
    "peer_w_query": [DM, DM], "peer_keys1": [8, 128, 64], "peer_keys2": [8, 128, 64], "peer_u": [16384, DM], "peer_v": [16384, DM],
}


GDN_CFG = {'solve_bf16': False, 'prod_bf16': True, 'scan_bf16': True}


class K:
    pass


def build_program(stop_after=None, debug=(), sub=None, skip=()):
    nc = bass.Bass("TRN2", target_bir_lowering=False)
    S = Sched(nc)
    k = K()
    k.nc, k.S = nc, S
    k.sub = sub
    k.gdn_cfg = dict(GDN_CFG)
    class _Lazy(dict):
        def __init__(self, specs, isconst):
            self.specs, self.isconst = specs, isconst
        def __missing__(self, n):
            if self.isconst:
                shp, dt = self.specs[n]
            else:
                shp, dt = self.specs[n], F32
            t = nc.dram_tensor(n, shp, dt, kind="ExternalInput")
            S.res[t.name] = Res(t.name)
            self[n] = t
            return t
    k.inp = _Lazy(INPUT_SPECS, False)
    k.cst = _Lazy(CONST_SPECS, True)
    k.out = nc.dram_tensor("out", [SEQ, DM], F32, kind="ExternalOutput")
    S.res[k.out.name] = Res(k.out.name)
    dbg = set(debug)

    def scratch(name, shape, dt):
        return S.dram(name, shape, dt, kind=("ExternalOutput" if name in dbg else "Internal"))

    k.d = {}
    for name, shape, dt in [
        ("qT_d", [8, 64, SEQ], BF16), ("kcr_d", [2, 64, SEQ], F32), ("vcr_d", [2, 64, SEQ], F32),
        ("ksT_d", [2, 64, SEQ], BF16), ("kwT_d", [2, 64, SEQ], BF16), ("vsw_d", [SEQ, 256], BF16),
        ("gts_d", [SEQ, 24], F32), ("dnqkv_d", [SEQ, 1536], F32), ("z_d", [SEQ, 512], F32), ("ba_d", [SEQ, 16], F32),
        ("gnT_d", [DM, SEQ], BF16), ("gdT_d", [DM, SEQ], BF16), ("onsaT_d", [512, SEQ], BF16), ("odnT_d", [512, SEQ], BF16),
        ("x1_d", [SEQ, DM], F32), ("h2_d", [SEQ, DM], F32), ("tab_d", [8, 16384], F32),
        ("onsa_d", [SEQ, 512], F32), ("odn_d", [SEQ, 512], F32), ("uv_d", [16384, 2 * DM], BF16),
    ]:
        k.d[name] = scratch(name, shape, dt)

    k.ps = [S.psum(f"ps{i}", [128, 512], F32) for i in range(6)]
    k.pb = [S.psum(f"pb{i}", [128, 1024], BF16) for i in range(2)]
    k.ident_f = S.sbuf("ident_f_s", [128, 128], F32)
    k.ident_b = S.sbuf("ident_b_s", [128, 128], BF16)
    k.modb = S.sbuf("modb", [128, 6 * DM], F32)
    S.dma(out=k.ident_f[:], in_=k.cst["ident_f"].ap())
    S.dma(out=k.ident_b[:], in_=k.cst["ident_b"].ap())
    k.cnt = 0

    phases = [phase_mod, phase_norm_proj, phase_nsa, phase_gdn, phase_merge, phase_peer]
    last = None
    for ph in phases:
        if ph.__name__ in skip:
            continue
        last = ph(k)
        if stop_after == ph.__name__:
            break
    outs = list(last) if last else []
    S.barrier()
    S.final_wait(outs)
    S.emit()
    S.close()
    nc.used_inputs = list(k.inp.keys()) + list(k.cst.keys())
    return nc


def alt(k):
    k.cnt += 1
    return k.cnt


def evac(k, out, in_, scale=None, eng=None):
    S = k.S
    e = eng or ("act" if alt(k) % 2 else "dve")
    if e == "act":
        if scale is None:
            return S.I("act", "activation", out=out, in_=in_, func=AF.Copy)
        return S.I("act", "activation", out=out, in_=in_, func=AF.Copy, scale=float(scale))
    if scale is None:
        return S.I("dve", "tensor_copy", out=out, in_=in_)
    return S.I("dve", "tensor_scalar", out=out, in0=in_, scalar1=float(scale), scalar2=None, op0=ALU.mult)


def phase_mod(k):
    S, nc = k.S, k.nc
    S.push()
    ct = S.sbuf("ct", [128, 8], F32)
    cs = S.sbuf("cs", [128, 8], F32)
    cb = S.sbuf("cb", [128, 8, 128], F32)
    ones = S.sbuf("ones1", [1, 128], F32)
    brow = S.sbuf("brow", [1, 6 * DM], F32)
    wt = [S.sbuf(f"wt{i}", [128, 8, 512], F32) for i in range(2)]
    gb = S.sbuf("gb", [128, DM], F32)
    S.dma(out=ct[:], in_=k.inp["c"].ap().rearrange("(kc k) -> k kc", k=128), allow_slow_non_contiguous=True)
    S.dma(out=brow[:], in_=k.inp["b_ada"].ap())
    S.I("pool", "memset", ones[:], 1.0, extra_writes=[ones[:]])
    S.I("act", "activation", out=cs[:], in_=ct[:], func=AF.Silu)
    S.I("dve", "tensor_copy", out=cb[:], in_=cs[:].unsqueeze(2).to_broadcast([128, 8, 128]))
    wv = k.inp["w_ada"].ap().rearrange("(kc k) n -> k kc n", k=128)
    for n in range(12):
        w = wt[n % 2]
        S.dma(out=w[:], in_=wv[:, :, n * 512:(n + 1) * 512])
        p = k.ps[n % 2]
        for kc in range(8):
            S.I("pe", "matmul", out=p[:], lhsT=cb[:, kc, :], rhs=w[:, kc, :], start=(kc == 0), stop=False)
        S.I("pe", "matmul", out=p[:], lhsT=ones[:], rhs=brow[:, n * 512:(n + 1) * 512], start=False, stop=True)
        evac(k, k.modb[:, n * 512:(n + 1) * 512], p[:])
    for (gname, off) in (("norm1_g", 1 * DM), ("norm2_g", 4 * DM)):
        S.dma(out=gb[:], in_=k.inp[gname].ap().partition_broadcast(128))
        S.I("dve", "scalar_tensor_tensor", out=k.modb[:, off:off + DM], in0=k.modb[:, off:off + DM], scalar=1.0, in1=gb[:],
            op0=ALU.add, op1=ALU.mult)
    S.pop()
    return []


def rms_mod_tile(k, xt, A, sh, out_ap, tmp, ss, junk):
    S = k.S
    S.I("act", "activation", out=junk, in_=xt, func=AF.Square, accum_out=ss[:, 0:1])
    S.I("dve", "tensor_scalar", out=ss[:, 1:2], in0=ss[:, 0:1], scalar1=1.0 / DM, scalar2=EPS, op0=ALU.mult, op1=ALU.add)
    S.I("act", "activation", out=ss[:, 2:3], in_=ss[:, 1:2], func=AF.Sqrt)
    S.I("dve", "reciprocal", out=ss[:, 3:4], in_=ss[:, 2:3])
    S.I("dve", "scalar_tensor_tensor", out=tmp, in0=xt, scalar=ss[:, 3:4], in1=A, op0=ALU.mult, op1=ALU.mult)
    S.I("pool", "tensor_tensor", out=out_ap, in0=tmp, in1=sh, op=ALU.add)


def phase_norm_proj(k):
    S, nc = k.S, k.nc
    S.push()
    hT = S.sbuf("hT", [128, 8, SEQ], BF16)
    S.push()
    xt = [S.sbuf(f"xt{i}", [128, DM], F32) for i in range(2)]
    tmp = [S.sbuf(f"tmpn{i}", [128, DM], F32) for i in range(2)]
    junk = S.sbuf("junkn", [128, DM], F32)
    hb = [S.sbuf(f"hb{i}", [128, DM], BF16) for i in range(2)]
    ss = [S.sbuf(f"ss{i}", [128, 4], F32) for i in range(2)]
    for i in range(NT):
        x = xt[i % 2]
        S.dma(out=x[:], in_=k.inp["x"].ap()[i * 128:(i + 1) * 128, :])
        rms_mod_tile(k, x[:], k.modb[:, DM:2 * DM], k.modb[:, 0:DM], hb[i % 2][:], tmp[i % 2][:], ss[i % 2], junk[:])
        pt = k.pb[i % 2]
        ptv = pt[:].rearrange("p (a b) -> p a b", a=8)
        for kc in range(8):
            S.I("pe", "transpose", out=ptv[:, kc, :], in_=hb[i % 2][:, kc * 128:(kc + 1) * 128], identity=k.ident_b[:])
        evac(k, hT[:, :, i * 128:(i + 1) * 128], ptv)
    S.pop()
    if k.sub == 'p1':
        S.pop()
        return []
    w_in = k.inp["w_in"].ap().rearrange("(kc k) n -> k kc n", k=128)
    S.push()
    wst = [S.sbuf(f"wst{i}", [128, 8, 128], F32) for i in range(2)]
    wtokA = S.sbuf("wtokA", [128, 8, 296], BF16)
    wtokZ = S.sbuf("wtokZ", [128, 8, 512], BF16)
    vstg = S.sbuf("vstg", [128, NT, 256], BF16)
    gstg = S.sbuf("gstg", [128, NT, 24], F32)
    bastg = S.sbuf("bastg", [128, NT, 16], F32)
    gbstg = S.sbuf("gbstg", [128, NT, 40], F32)
    zstg = [S.sbuf(f"zstg{i}", [128, 512], F32) for i in range(2)]
    pieces = [(C_VS, 128, wtokA, 0), (C_VW, 128, wtokA, 128), (C_GT, 24, wtokA, 256), (C_DB, 16, wtokA, 280)] + \
             [(C_DZ + 128 * q, 128, wtokZ, 128 * q) for q in range(4)]
    for n, (c0, w, dst, off) in enumerate(pieces):
        st = wst[n % 2]
        S.dma(out=st[:, :, 0:w], in_=w_in[:, :, c0:c0 + w])
        S.I("pool", "tensor_copy", out=dst[:, :, off:off + w], in_=st[:, :, 0:w])
    for i in range(NT):
        pa, pz = k.ps[(2 * i) % 4], k.ps[(2 * i + 1) % 4]
        for kc in range(8):
            S.I("pe", "matmul", out=pa[:, 0:296], lhsT=hT[:, kc, i * 128:(i + 1) * 128], rhs=wtokA[:, kc, :], start=(kc == 0), stop=(kc == 7))
        for kc in range(8):
            S.I("pe", "matmul", out=pz[:], lhsT=hT[:, kc, i * 128:(i + 1) * 128], rhs=wtokZ[:, kc, :], start=(kc == 0), stop=(kc == 7))
        S.I("dve", "tensor_copy", out=vstg[:, i, :], in_=pa[:, 0:256])
        S.I("dve", "tensor_copy", out=gbstg[:, i, :], in_=pa[:, 256:296])
        z = zstg[i % 2]
        S.I("act", "activation", out=z[:], in_=pz[:], func=AF.Copy)
        S.dma(out=k.d["z_d"].ap()[i * 128:(i + 1) * 128, :], in_=z[:])
    S.I("act", "activation", out=gstg[:], in_=gbstg[:, :, 0:24], func=AF.Sigmoid)
    S.I("dve", "tensor_copy", out=bastg[:], in_=gbstg[:, :, 24:40])
    S.dma(out=k.d["vsw_d"].ap().rearrange("(i p) c -> p i c", p=128), in_=vstg[:])
    S.dma(out=k.d["gts_d"].ap().rearrange("(i p) c -> p i c", p=128), in_=gstg[:])
    S.dma(out=k.d["ba_d"].ap().rearrange("(i p) c -> p i c", p=128), in_=bastg[:])
    S.pop()
    if k.sub and k.sub.startswith('p2a'):
        S.pop()
        return []
    S.push()
    wst = [S.sbuf(f"wstb{i}", [128, 8, 128], F32) for i in range(2)]
    wbf = [S.sbuf(f"wbf{i}", [128, 8, 128], BF16) for i in range(2)]
    stg_f = S.sbuf("stg_f", [128, SEQ + 3], F32)
    stg_b = [S.sbuf(f"stg_b{i}", [128, SEQ], BF16) for i in range(2)]
    acc = S.sbuf("acc_cv", [128, SEQ], F32)
    tokstg = S.sbuf("tokstg", [128, NT, 128], F32)
    cw = S.sbuf("cw", [128, 4, 12], F32)
    for j in range(4):
        S.dma(out=cw[:, j, :], in_=k.inp["dn_conv_w"].ap()[j, :].rearrange("(g c) -> c g", c=128), allow_slow_non_contiguous=True)
    S.I("pool", "memset", stg_f[:, 0:3], 0.0, extra_writes=[stg_f[:]])
    groups = []
    for q in range(4):
        groups.append((C_Q + 128 * q, "q", k.d["qT_d"].ap().rearrange("h d t -> (h d) t")[128 * q:128 * (q + 1), :]))
    groups.append((C_KC, "f32", k.d["kcr_d"].ap().rearrange("h d t -> (h d) t")))
    groups.append((C_VC, "f32", k.d["vcr_d"].ap().rearrange("h d t -> (h d) t")))
    groups.append((C_KS, "bf", k.d["ksT_d"].ap().rearrange("h d t -> (h d) t")))
    groups.append((C_KW, "bf", k.d["kwT_d"].ap().rearrange("h d t -> (h d) t")))
    for q in range(8):
        groups.append((C_GN + 128 * q, "sig", k.d["gnT_d"].ap()[128 * q:128 * (q + 1), :]))
    for q in range(8):
        groups.append((C_GD + 128 * q, "sig", k.d["gdT_d"].ap()[128 * q:128 * (q + 1), :]))
    for q in range(12):
        groups.append((C_DQ + 128 * q, "dn", q))
    nb = 0
    for g, (c0, kind, dest) in enumerate(groups):
        st, wb = wst[g % 2], wbf[g % 2]
        S.dma(out=st[:], in_=w_in[:, :, c0:c0 + 128])
        S.I("pool", "tensor_copy", out=wb[:], in_=st[:])
        if kind in ("q", "bf", "sig"):
            sb = stg_b[nb % 2]
            nb += 1
        for G in range(8):
            p = k.ps[(g * 8 + G) % 4]
            for kc in range(8):
                S.I("pe", "matmul", out=p[:], lhsT=wb[:, kc, :], rhs=hT[:, kc, G * 512:(G + 1) * 512], start=(kc == 0), stop=(kc == 7))
            if kind == "q":
                evac(k, sb[:, G * 512:(G + 1) * 512], p[:], scale=0.125)
            elif kind == "bf":
                evac(k, sb[:, G * 512:(G + 1) * 512], p[:])
            elif kind == "sig":
                S.I("act", "activation", out=sb[:, G * 512:(G + 1) * 512], in_=p[:], func=AF.Sigmoid)
            else:
                evac(k, stg_f[:, 3 + G * 512:3 + (G + 1) * 512], p[:])
        if kind in ("q", "bf", "sig"):
            S.dma(out=dest, in_=sb[:])
        elif kind == "f32":
            S.dma(out=dest, in_=stg_f[:, 3:3 + SEQ])
        else:
            q = dest
            S.I("dve", "tensor_scalar", out=acc[:], in0=stg_f[:, 0:SEQ], scalar1=cw[:, 0, q:q + 1], scalar2=None, op0=ALU.mult)
            for j in range(1, 4):
                S.I("dve", "scalar_tensor_tensor", out=acc[:], in0=stg_f[:, j:j + SEQ], scalar=cw[:, j, q:q + 1], in1=acc[:],
                    op0=ALU.mult, op1=ALU.add)
            S.I("act", "activation", out=acc[:], in_=acc[:], func=AF.Silu)
            for i4 in range(8):
                p = k.ps[i4 % 4]
                pv = p[:].rearrange("p (a b) -> p a b", a=4)
                for qq in range(4):
                    i = 4 * i4 + qq
                    S.I("pe", "transpose", out=pv[:, qq, :], in_=acc[:, i * 128:(i + 1) * 128], identity=k.ident_f[:])
                evac(k, tokstg[:, 4 * i4:4 * i4 + 4, :], pv)
            S.dma(out=k.d["dnqkv_d"].ap().rearrange("(i p) c -> p i c", p=128)[:, :, 128 * q:128 * (q + 1)], in_=tokstg[:])
    S.pop()
    S.pop()
    return []


def phase_nsa(k):
    S, nc = k.S, k.nc
    from concourse.ap import AP as RawAP
    S.push()
    tab_d = k.d["tab_d"]
    S.push()
    rb33 = S.sbuf("rb33", [33, 8], F32)
    S.I("pool", "memset", rb33[:], NEG, extra_writes=[rb33[:]])
    S.dma(out=rb33[0:32, :], in_=k.inp["rel_bias"].ap())
    ohs = [S.sbuf(f"ohs{i}", [33, 2048], F32) for i in range(2)]
    tbs = [S.sbuf(f"tbs{i}", [8, 2048], F32) for i in range(2)]
    for c8 in range(8):
        oh, tb = ohs[c8 % 2], tbs[c8 % 2]
        S.dma(out=oh[:], in_=k.cst["ohp"].ap()[:, c8 * 2048:(c8 + 1) * 2048])
        for q in range(4):
            p = k.ps[q]
            S.I("pe", "matmul", out=p[0:8, :], lhsT=rb33[:], rhs=oh[:, q * 512:(q + 1) * 512], start=True, stop=True)
            evac(k, tb[:, q * 512:(q + 1) * 512], p[0:8, :])
        S.dma(out=tab_d.ap()[:, c8 * 2048:(c8 + 1) * 2048], in_=tb[:])
    S.pop()
    anti = S.sbuf("anti", [128, 128], F32)
    S.dma(out=anti[:], in_=k.cst["anti_f"].ap())
    strips = S.sbuf("strips", [128, 8, 640], BF16)
    S.I("pool", "memset", strips[:], 0.0, extra_writes=[strips[:]])
    hank = [S.sbuf(f"hank{i}", [128, 512], F32) for i in range(2)]
    for h in range(8):
        S.dma(out=strips[:, h, 512:640], in_=k.cst["w4t"].ap())
        hk = hank[h % 2]
        S.dma(out=hk[:, 0:256], in_=RawAP(tab_d, h * 16384 + 8192 - 127, [[1, 128], [1, 256]]))
        p = k.ps[h % 4]
        S.I("pe", "matmul", out=p[:, 0:256], lhsT=anti[:], rhs=hk[:, 0:256], start=True, stop=True)
        evac(k, strips[:, h, 0:256], p[:, 0:256])
    kcT = S.sbuf("kcT", [64, 2, 256], BF16)
    vcaug = S.sbuf("vcaug", [128, 2, 2, 129], F32)
    S.I("pool", "memset", vcaug[:], 0.0, extra_writes=[vcaug[:]])
    S.I("pool", "memset", vcaug[:, :, :, 64:65], 1.0, extra_writes=[vcaug[:]])
    for kv in range(2):
        for a in range(2):
            rows = 128 if a == 0 else 127
            S.dma(out=vcaug[0:rows, kv, a, 65:129], in_=k.cst["overlap"].ap()[a * 128:a * 128 + rows, :])
    S.push()
    w1 = S.sbuf("w1c", [64, 32, 64], F32)
    w2 = S.sbuf("w2c", [64, 64], F32)
    posT = S.sbuf("posT", [64, 32], F32)
    cbias = S.sbuf("cbias", [64, 1], F32)
    kcr = [S.sbuf(f"kcr{i}", [64, SEQ], F32) for i in range(2)]
    xh = S.sbuf("xh", [64, 256], F32)
    t1 = S.sbuf("t1c", [64, 256], F32)
    t2 = S.sbuf("t2c", [64, 256], F32)
    gh = S.sbuf("ghc", [64, 256], F32)
    S.dma(out=posT[:], in_=k.inp["cmp_pos_emb"].ap().rearrange("l d -> d l"), allow_slow_non_contiguous=True)
    n = 0
    for which in ("k", "v"):
        S.dma(out=w1[:], in_=k.inp["w_cmp_%s1" % which].ap().rearrange("(l d) h -> d l h", d=64))
        S.dma(out=w2[:], in_=k.inp["w_cmp_%s2" % which].ap())
        pc = k.ps[0]
        for l in range(32):
            S.I("pe", "matmul", out=pc[0:64, 0:1], lhsT=w1[:, l, :], rhs=posT[:, l:l + 1], start=(l == 0), stop=(l == 31))
        S.I("dve", "tensor_copy", out=cbias[:], in_=pc[0:64, 0:1])
        for kv in range(2):
            kc = kcr[n % 2]
            n += 1
            S.dma(out=kc[:], in_=k.d["kcr_d" if which == "k" else "vcr_d"].ap()[kv])
            kcv = kc[:].rearrange("p (n s) -> p n s", s=16)
            ph = k.ps[1 + (n % 2)]
            for l in range(32):
                rhs = kcv[:, 0:255, l] if l < 16 else kcv[:, 1:256, l - 16]
                S.I("pe", "matmul", out=ph[0:64, 0:255], lhsT=w1[:, l, :], rhs=rhs, start=(l == 0), stop=(l == 31))
            S.I("act", "activation", out=xh[:, 0:255], in_=ph[0:64, 0:255], func=AF.Identity, bias=cbias[:, 0:1])
            S.I("dve", "tensor_tensor", out=t1[:, 0:255], in0=xh[:, 0:255], in1=xh[:, 0:255], op=ALU.mult)
            S.I("dve", "tensor_scalar", out=t1[:, 0:255], in0=t1[:, 0:255], scalar1=0.044715, scalar2=1.0, op0=ALU.mult, op1=ALU.add)
            S.I("dve", "tensor_tensor", out=t2[:, 0:255], in0=t1[:, 0:255], in1=xh[:, 0:255], op=ALU.mult)
            S.I("act", "activation", out=t1[:, 0:255], in_=t2[:, 0:255], func=AF.Sigmoid, scale=GELU_C)
            S.I("dve", "tensor_tensor", out=gh[:, 0:255], in0=t1[:, 0:255], in1=xh[:, 0:255], op=ALU.mult)
            if which == "k":
                pk = k.ps[3]
                S.I("pe", "matmul", out=pk[0:64, 0:255], lhsT=w2[:], rhs=gh[:, 0:255], start=True, stop=True)
                S.I("dve", "tensor_copy", out=kcT[:, kv, 0:255], in_=pk[0:64, 0:255])
            else:
                for a in range(2):
                    rows = 128 if a == 0 else 127
                    pv = k.ps[4 + a]
                    S.I("pe", "matmul", out=pv[0:rows, 0:64], lhsT=gh[:, a * 128:a * 128 + rows], rhs=w2[:], start=True, stop=True)
                    S.I("dve", "tensor_copy", out=vcaug[0:rows, kv, a, 0:64], in_=pv[0:rows, 0:64])
    S.pop()
    if k.sub == "n1":
        S.pop()
        return []
    gates = S.sbuf("gatesb", [128, NT, 24], F32)
    S.dma(out=gates[:], in_=k.d["gts_d"].ap().rearrange("(i p) c -> p i c", p=128))
    slc = S.sbuf("slcadd", [128, NT, 64], F32)
    S.dma(out=slc[:], in_=k.cst["slc_add"].ap().rearrange("(i p) c -> p i c", p=128))
    o_acc = S.sbuf("o_acc", [128, NT, 256], F32)
    imp = S.sbuf("imp", [128, NT, 64], F32)
    qaug = [S.sbuf(f"qaug{g}", [128, SEQ], BF16) for g in range(4)]
    ksaug = S.sbuf("ksaug", [128, SEQ], BF16)
    kwT = S.sbuf("kwT", [64, SEQ], BF16)
    vsa = S.sbuf("vsa", [128, NT, 65], BF16)
    vwa = S.sbuf("vwa", [128, NT, 65], BF16)
    pTb = [S.sbuf(f"pTb{i}", [128, 512], BF16) for i in range(3)]
    pTc4 = [S.sbuf(f"pTc{i}", [128, 512], F32) for i in range(4)]
    hank4 = hank + [S.sbuf(f"hankx{i}", [128, 512], F32) for i in range(2)]
    ftb4 = [S.sbuf(f"ftb{i}", [128, 512], BF16) for i in range(4)]
    selb = [S.sbuf(f"selb{i}", [128, 128], BF16) for i in range(2)]
    sc = [S.sbuf(f"scr{i}", [128, 64], F32) for i in range(2)]
    sc2 = [S.sbuf(f"scr2{i}", [128, 64], F32) for i in range(2)]
    m8 = [S.sbuf(f"m8{i}", [128, 16], F32) for i in range(2)]
    rz = [S.sbuf(f"rz{i}", [128, 8], F32) for i in range(2)]
    tmpo = [S.sbuf(f"tmpo{i}", [128, 4, 64], F32) for i in range(2)]
    ob = [S.sbuf(f"ob{i}", [128, 256], BF16) for i in range(2)]
    oT = S.sbuf("oTn", [128, 2, SEQ], BF16)
    for i in range(2):
        S.I("pool", "memset", selb[i][:], 0.0, extra_writes=[selb[i][:]])
    S.dma(out=ksaug[64:128, :], in_=k.cst["e_blk"].ap())
    S.I("pool", "memset", vsa[:, :, 64:65], 1.0, extra_writes=[vsa[:]])
    S.I("pool", "memset", vwa[:, :, 64:65], 1.0, extra_writes=[vwa[:]])
    vsw_v = k.d["vsw_d"].ap().rearrange("(i p) c -> p i c", p=128)
    cnt = [0]

    def finish_branch(h, g, G, po_views, zcol, br, first):
        r = rz[cnt[0] % 2]
        tm = tmpo[cnt[0] % 2]
        cnt[0] += 1
        for c in range(4):
            S.I("dve", "tensor_scalar", out=r[:, c:c + 1], in0=po_views[c][:, zcol:zcol + 1], scalar1=1e-30, scalar2=None, op0=ALU.max)
        S.I("dve", "reciprocal", out=r[:, 0:4], in_=r[:, 0:4])
        S.I("dve", "tensor_tensor", out=r[:, 4:8], in0=r[:, 0:4], in1=gates[:, 4 * G:4 * G + 4, h * 3 + br], op=ALU.mult)
        for c in range(4):
            dst = o_acc[:, 4 * G + c, g * 64:(g + 1) * 64]
            if first:
                S.I("dve", "tensor_scalar", out=dst, in0=po_views[c][:, 0:64], scalar1=r[:, 4 + c:5 + c], scalar2=None, op0=ALU.mult)
            else:
                S.I("dve", "scalar_tensor_tensor", out=dst, in0=po_views[c][:, 0:64], scalar=r[:, 4 + c:5 + c], in1=dst,
                    op0=ALU.mult, op1=ALU.add)
        return r

    nps = [0]

    def next_ps():
        nps[0] += 1
        return k.ps[nps[0] % 4]

    for kv in range(2):
        for g in range(4):
            S.dma(out=qaug[g][0:64, :], in_=k.d["qT_d"].ap()[kv * 4 + g])
        S.dma(out=ksaug[0:64, :], in_=k.d["ksT_d"].ap()[kv])
        S.dma(out=kwT[:], in_=k.d["kwT_d"].ap()[kv])
        S.dma(out=vsa[:, :, 0:64], in_=vsw_v[:, :, kv * 64:kv * 64 + 64])
        S.dma(out=vwa[:, :, 0:64], in_=vsw_v[:, :, 128 + kv * 64:128 + kv * 64 + 64])
        def cmp_front(g, G, rnd):
            h = kv * 4 + g
            a_list = [0] if G < 4 else [0, 1]
            tiles = []
            for a in a_list:
                rows = 128 if a == 0 else 127
                need_corr = (a == 0 and G <= 4) or (a == 1)
                pss = next_ps()
                S.I("pe", "matmul", out=pss[0:rows, :], lhsT=kcT[:, kv, a * 128:a * 128 + rows], rhs=qaug[g][0:64, G * 512:(G + 1) * 512],
                    start=True, stop=not need_corr)
                if need_corr:
                    hk = hank4[cnt[0] % 4]
                    ft = ftb4[cnt[0] % 4]
                    cnt[0] += 1
                    S.dma(out=hk[:], in_=RawAP(tab_d, h * 16384 + 8192 + 512 * G - 2048 * a - 2063, [[16, 128], [1, 512]]))
                    pj = next_ps()
                    S.I("pe", "matmul", out=pj[:], lhsT=anti[:], rhs=hk[:], start=True, stop=True)
                    evac(k, ft[:], pj[:], eng="dve")
                    S.I("pe", "matmul", out=pss[0:rows, :], lhsT=k.ident_b[0:rows, 0:rows], rhs=ft[0:rows, :], start=False, stop=True)
                pt_ = pTc4[(rnd % 2) * 2 + a]
                S.I("act", "activation", out=pt_[0:rows, :], in_=pss[0:rows, :], func=AF.Exp)
                tiles.append((a, rows, pt_))
            return (g, G, h, tiles)

        def cmp_back(ctx):
            g, G, h, tiles = ctx
            pA, pB = k.ps[4], k.ps[5]
            views = []
            for c in range(4):
                pv = (pA if c < 2 else pB)[:, (c % 2) * 256:(c % 2) * 256 + 129]
                views.append(pv)
                for ti, (a, rows, pt_) in enumerate(tiles):
                    S.I("pe", "matmul", out=pv, lhsT=pt_[0:rows, c * 128:(c + 1) * 128], rhs=vcaug[0:rows, kv, a, :],
                        start=(ti == 0), stop=(ti == len(tiles) - 1))
            r = finish_branch(h, g, G, views, 64, 0, True)
            for c in range(4):
                dst = imp[:, 4 * G + c, :]
                if g == 0:
                    S.I("dve", "tensor_scalar", out=dst, in0=views[c][:, 65:129], scalar1=r[:, c:c + 1], scalar2=None, op0=ALU.mult)
                else:
                    S.I("dve", "scalar_tensor_tensor", out=dst, in0=views[c][:, 65:129], scalar=r[:, c:c + 1], in1=dst,
                        op0=ALU.mult, op1=ALU.add)

        rounds = [(g, G) for g in range(4) for G in range(8)]
        prev = cmp_front(rounds[0][0], rounds[0][1], 0)
        for ri in range(1, len(rounds)):
            cur = cmp_front(rounds[ri][0], rounds[ri][1], ri)
            cmp_back(prev)
            prev = cur
        cmp_back(prev)
        for i in range(NT):
            s1, s2, mm, sb = sc[i % 2], sc2[i % 2], m8[i % 2], selb[i % 2]
            S.I("dve", "tensor_tensor", out=s1[:], in0=imp[:, i, :], in1=slc[:, i, :], op=ALU.add)
            S.I("dve", "max", out=mm[:, 0:8], in_=s1[:])
            S.I("dve", "match_replace", out=s2[:], in_to_replace=mm[:, 0:8], in_values=s1[:], imm_value=-3.0e38)
            S.I("dve", "max", out=mm[:, 8:16], in_=s2[:])
            S.I("dve", "tensor_scalar", out=sb[:, 64:128], in0=s1[:], scalar1=mm[:, 15:16], scalar2=NEG, op0=ALU.is_lt, op1=ALU.mult)
            pt = k.pb[i % 2]
            S.I("pe", "transpose", out=pt[:, 0:128], in_=sb[:], identity=k.ident_b[:])
            for g in range(4):
                evac(k, qaug[g][64:128, i * 128:(i + 1) * 128], pt[64:128, 0:128], eng="dve" if i % 2 else "act")
        if k.sub == "n2":
            continue
        for g in range(4):
            h = kv * 4 + g
            for G in range(8):
                po = k.ps[4 + (G % 2)]
                views = [po[:, c * 128:c * 128 + 65] for c in range(4)]

                def qk_sel(P, g=g, h=h, G=G):
                    c_lo = max(0, P - 4 * G)
                    ncol = 4 - c_lo
                    t0 = G * 512 + c_lo * 128
                    pss = next_ps()
                    if P >= 4 * G:
                        S.I("pe", "matmul", out=pss[:, 0:ncol * 128], lhsT=ksaug[:, P * 128:(P + 1) * 128], rhs=qaug[g][:, t0:(G + 1) * 512],
                            start=True, stop=False)
                        S.I("pe", "matmul", out=pss[:, 0:ncol * 128], lhsT=k.ident_b[:], rhs=strips[:, h, 0:ncol * 128], start=False, stop=True)
                    elif P == 4 * G - 1:
                        S.I("pe", "matmul", out=pss[:, 0:128], lhsT=ksaug[:, P * 128:(P + 1) * 128], rhs=qaug[g][:, t0:t0 + 128],
                            start=True, stop=False)
                        S.I("pe", "matmul", out=pss[:, 0:128], lhsT=k.ident_b[:], rhs=strips[:, h, 128:256], start=False, stop=True)
                        S.I("pe", "matmul", out=pss[:, 128:512], lhsT=ksaug[:, P * 128:(P + 1) * 128], rhs=qaug[g][:, t0 + 128:(G + 1) * 512],
                            start=True, stop=True)
                    else:
                        S.I("pe", "matmul", out=pss[:, 0:ncol * 128], lhsT=ksaug[:, P * 128:(P + 1) * 128], rhs=qaug[g][:, t0:(G + 1) * 512],
                            start=True, stop=True)
                    pT = pTb[nps[0] % 3]
                    S.I("act", "activation", out=pT[:, 0:ncol * 128], in_=pss[:, 0:ncol * 128], func=AF.Exp)
                    return (P, c_lo, pT)

                def pv_sel(ctx, G=G, views=views):
                    P, c_lo, pT = ctx
                    for c in range(c_lo, 4):
                        S.I("pe", "matmul", out=views[c], lhsT=pT[:, (c - c_lo) * 128:(c - c_lo + 1) * 128], rhs=vsa[:, P, :],
                            start=(P == 0 and c == 0), stop=(P == 4 * G + c), skip_group_check=True)

                ncv = (kv * 4 + g) * 8 + G
                src_t = k.inp["peer_u" if ncv < 32 else "peer_v"].ap()
                r0 = (ncv % 32) * 512
                S.dma(out=k.d["uv_d"].ap()[r0:r0 + 512, (ncv // 32) * DM:(ncv // 32 + 1) * DM], in_=src_t[r0:r0 + 512, :], q="pool")
                Ps = list(range(0, 4 * G + 4))
                prev = qk_sel(Ps[0])
                for P in Ps[1:]:
                    cur = qk_sel(P)
                    pv_sel(prev)
                    prev = cur
                pv_sel(prev)
                finish_branch(h, g, G, views, 64, 1, False)
        for g in range(4):
            h = kv * 4 + g
            for G in range(8):
                po = k.ps[4 + (G % 2)]
                views = [po[:, c * 128:c * 128 + 65] for c in range(4)]

                def qk_win(P, g=g, h=h, G=G):
                    c_lo = max(0, P - 4 * G)
                    c_hi = min(3, P - 4 * G + 4)
                    ncol = c_hi - c_lo + 1
                    r_lo = 4 * G + c_lo - P
                    t0 = G * 512 + c_lo * 128
                    pss = next_ps()
                    S.I("pe", "matmul", out=pss[:, 0:ncol * 128], lhsT=kwT[:, P * 128:(P + 1) * 128], rhs=qaug[g][0:64, t0:t0 + ncol * 128],
                        start=True, stop=False)
                    S.I("pe", "matmul", out=pss[:, 0:ncol * 128], lhsT=k.ident_b[:], rhs=strips[:, h, r_lo * 128:(r_lo + ncol) * 128], start=False, stop=True)
                    pT = pTb[nps[0] % 3]
                    S.I("act", "activation", out=pT[:, 0:ncol * 128], in_=pss[:, 0:ncol * 128], func=AF.Exp)
                    return (P, c_lo, c_hi, pT)

                def pv_win(ctx, G=G, views=views):
                    P, c_lo, c_hi, pT = ctx
                    for c in range(c_lo, c_hi + 1):
                        S.I("pe", "matmul", out=views[c], lhsT=pT[:, (c - c_lo) * 128:(c - c_lo + 1) * 128], rhs=vwa[:, P, :],
                            start=(P == max(0, 4 * G - 4) and c == 0), stop=(P == 4 * G + c), skip_group_check=True)

                Ps = list(range(max(0, 4 * G - 4), 4 * G + 4))
                prev = qk_win(Ps[0])
                for P in Ps[1:]:
                    cur = qk_win(P)
                    pv_win(prev)
                    prev = cur
                pv_win(prev)
                finish_branch(h, g, G, views, 64, 2, False)
        S.dma(out=k.d["onsa_d"].ap().rearrange("(i p) c -> p i c", p=128)[:, :, kv * 256:(kv + 1) * 256], in_=o_acc[:])
        for i in range(NT):
            o = ob[i % 2]
            S.I("pool", "tensor_copy", out=o[:], in_=o_acc[:, i, :])
            pt = k.pb[i % 2]
            for j in range(2):
                S.I("pe", "transpose", out=pt[:, j * 128:(j + 1) * 128], in_=o[:, j * 128:(j + 1) * 128], identity=k.ident_b[:])
            evac(k, oT[:, :, i * 128:(i + 1) * 128], pt[:, 0:256].rearrange("p (a b) -> p a b", a=2))
        S.dma(out=k.d["onsaT_d"].ap().rearrange("(j p) t -> p j t", p=128)[:, 2 * kv:2 * kv + 2, :], in_=oT[:])
    S.pop()
    return []


def phase_gdn(k):
    S, nc = k.S, k.nc
    S.push()
    gdn_d = S.dram("gdn_d", [SEQ, 2056], F32)

    def v3(ap, a):
        return ap.rearrange("p (a b) -> p a b", a=a)

    def bc2(ap, n, w):
        return ap.unsqueeze(2).to_broadcast([n, ap.shape[1], w])

    def bc1(ap, n, a):
        return ap.unsqueeze(1).to_broadcast([n, a, ap.shape[1]])

    S.push()
    dtb = S.sbuf("dtb", [128, 8], F32)
    negA = S.sbuf("negA", [128, 8], F32)
    S.dma(out=dtb[:], in_=k.inp["dn_dt_bias"].ap().partition_broadcast(128))
    S.dma(out=negA[:], in_=k.inp["dn_A_log"].ap().partition_broadcast(128))
    S.I("act", "activation", out=negA[:], in_=negA[:], func=AF.Exp)
    S.I("dve", "tensor_scalar", out=negA[:], in0=negA[:], scalar1=-1.0, scalar2=None, op0=ALU.mult)
    xin = [S.sbuf(f"gxin{i}", [128, 1536], F32) for i in range(2)]
    bain = [S.sbuf(f"gbain{i}", [128, 16], F32) for i in range(2)]
    xout = [S.sbuf(f"gxout{i}", [128, 2056], F32) for i in range(2)]
    sqt = S.sbuf("gsq", [128, 1024], F32)
    sm = [S.sbuf(f"gsm{i}", [128, 48], F32) for i in range(2)]
    for i in range(NT):
        xq, ba, Xo, s_ = xin[i % 2], bain[i % 2], xout[i % 2], sm[i % 2]
        S.dma(out=xq[:], in_=k.d["dnqkv_d"].ap()[i * 128:(i + 1) * 128, :])
        S.dma(out=ba[:], in_=k.d["ba_d"].ap()[i * 128:(i + 1) * 128, :])
        S.I("act", "activation", out=sqt[:], in_=xq[:, 0:1024], func=AF.Square)
        S.I("dve", "tensor_reduce", out=s_[:, 0:16], in_=v3(sqt[:], 16), axis=AX.X, op=ALU.add)
        S.I("dve", "tensor_scalar", out=s_[:, 0:16], in0=s_[:, 0:16], scalar1=EPS, scalar2=None, op0=ALU.add)
        S.I("act", "activation", out=s_[:, 0:16], in_=s_[:, 0:16], func=AF.Sqrt)
        S.I("dve", "reciprocal", out=s_[:, 16:32], in_=s_[:, 0:16])
        S.I("dve", "tensor_scalar", out=s_[:, 16:24], in0=s_[:, 16:24], scalar1=0.125, scalar2=None, op0=ALU.mult)
        S.I("dve", "tensor_tensor", out=v3(Xo[:, 0:1024], 16), in0=v3(xq[:, 0:1024], 16), in1=bc2(s_[:, 16:32], 128, 64), op=ALU.mult)
        S.I("act", "activation", out=s_[:, 32:40], in_=ba[:, 0:8], func=AF.Sigmoid)
        S.I("dve", "tensor_tensor", out=s_[:, 40:48], in0=ba[:, 8:16], in1=dtb[:], op=ALU.add)
        S.I("act", "activation", out=s_[:, 40:48], in_=s_[:, 40:48], func=AF.Exp)
        S.I("dve", "tensor_scalar", out=s_[:, 40:48], in0=s_[:, 40:48], scalar1=1.0, scalar2=None, op0=ALU.add)
        S.I("act", "activation", out=s_[:, 40:48], in_=s_[:, 40:48], func=AF.Ln)
        S.I("dve", "tensor_tensor", out=Xo[:, 2048:2056], in0=s_[:, 40:48], in1=negA[:], op=ALU.mult)
        S.I("pool", "tensor_tensor", out=v3(Xo[:, 1024:1536], 8), in0=v3(Xo[:, 512:1024], 8), in1=bc2(s_[:, 32:40], 128, 64), op=ALU.mult)
        S.I("pool", "tensor_tensor", out=v3(Xo[:, 1536:2048], 8), in0=v3(xq[:, 1024:1536], 8), in1=bc2(s_[:, 32:40], 128, 64), op=ALU.mult)
        S.dma(out=gdn_d.ap()[i * 128:(i + 1) * 128, :], in_=Xo[:])
    S.pop()
    C = 64
    NCH = SEQ // C
    cm = {}
    for nm in ("m_cum", "m_incl_u", "m_incl_l", "m_nstr_u", "m_nstr_l"):
        cm[nm] = S.sbuf("c_" + nm, [64, 64], F32)
        S.dma(out=cm[nm][:], in_=k.cst[nm].ap())
    ones64 = S.sbuf("ones64", [64, 64], F32)
    S.I("pool", "memset", ones64[:], 1.0, extra_writes=[ones64[:]])
    gng = S.sbuf("gng", [64, 64], F32)
    S.dma(out=gng[:], in_=k.inp["dn_norm_g"].ap().partition_broadcast(64))
    St = S.sbuf("gstate", [64, 8, 64], F32)
    S.I("pool", "memset", St[:], 0.0, extra_writes=[St[:]])
    idf = k.ident_f[0:64, 0:64]

    DT_SOLVE = BF16 if k.gdn_cfg.get('solve_bf16', True) else F32
    DT_PROD = BF16 if k.gdn_cfg.get('prod_bf16', True) else F32
    DT_SCAN = BF16 if k.gdn_cfg.get('scan_bf16', True) else F32
    NS = 3
    def mk(name, shape, n=2, dt=F32):
        n = {2: NS, 4: 2 * NS, 22: 2}[n]
        return [S.sbuf(f"{name}{i}", shape, dt) for i in range(n)]

    X = mk("gX", [64, 2056])
    rhsU = mk("grhsU", [64, 8, 64])
    D1 = mk("gD1", [64, 8, 64])
    ET = mk("gET", [64, 8, 64])
    EE = mk("gEE", [64, 8, 64])
    ETn = mk("gETn", [64, 8, 64])
    En = EE
    tmpd = rhsU
    kT = mk("gkT", [64, 8, 64], 2, DT_PROD)
    kbT = mk("gkbT", [64, 8, 64], 2, DT_PROD)
    NTa = mk("gNTa", [64, 8, 64], 2, DT_SOLVE)
    NTb = mk("gNTb", [64, 8, 64], 2, DT_SOLVE)
    Na = mk("gNa", [64, 8, 64], 2, DT_SOLVE)
    Nb = mk("gNb", [64, 8, 64], 2, DT_SOLVE)
    qT = mk("gqT", [64, 8, 64], 4, DT_PROD)
    attnT = mk("gattnT", [64, 8, 64], 4, DT_SCAN)
    X6 = mk("gX6", [64, 8, 128], 4, DT_SOLVE)
    qTs = mk("gqTs", [64, 8, 64], 4, DT_SCAN)
    Tmat = mk("gTmat", [64, 8, 64])
    TTs = mk("gTTs", [64, 8, 64])
    R6 = mk("gR6", [64, 8, 128])
    Stb = S.sbuf("gstateb", [64, 8, 64], DT_SCAN)
    S.I("pool", "memset", Stb[:], 0.0, extra_writes=[Stb[:]])
    wT = mk("gwT", [64, 8, 64], 4, DT_SCAN)
    kdec = mk("gkdec", [64, 8, 64], 4, DT_SCAN)
    sm = mk("gsmall", [64, 64], 4)
    Z = mk("gZ", [64, 512], 22)
    vnew = mk("gvnew", [64, 8, 64], 22, DT_SCAN)
    osb = mk("gosb", [64, 8, 64], 22)
    odn = mk("godn", [64, 512], 22)
    pbf = [k.pb[i][:].bitcast(F32) for i in range(2)]

    def mm8(out_ps, lhs, rhs, w=64):
        for h in range(8):
            S.I("pe", "matmul", out=out_ps[0:64, h * w:(h + 1) * w], lhsT=lhs(h), rhs=rhs(h), start=True, stop=True)

    def tr8(out_ps, src):
        for h in range(8):
            S.I("pe", "transpose", out=out_ps[0:64, h * 64:(h + 1) * 64], in_=src(h), identity=idf)

    def pv(ps):
        return v3(ps[0:64, :], 8)

    def pre(n):
        b, L = n % NS, n % (2 * NS)
        banks = [k.ps[2 * b + i] for i in range(2)]
        cnt = [0]

        def nps():
            cnt[0] += 1
            return banks[cnt[0] % 2]

        x = X[b]
        S.dma(out=x[:], in_=gdn_d.ap()[n * C:(n + 1) * C, :])
        qn, kn, kb, vb = (v3(x[:, o:o + 512], 8) for o in (0, 512, 1024, 1536))
        g = x[:, 2048:2056]
        s_ = sm[L]
        S.I("pool", "tensor_tensor", out=rhsU[b][:], in0=bc2(g, 64, 64), in1=bc1(cm["m_cum"][:], 64, 8), op=ALU.mult)
        pG = nps()
        S.I("pe", "matmul", out=pG[0:64, :], lhsT=ones64[:], rhs=rhsU[b][:].rearrange("p a b -> p (a b)"), start=True, stop=True)
        pg = nps()
        S.I("pe", "matmul", out=pg[0:64, 0:8], lhsT=cm["m_cum"][:], rhs=g, start=True, stop=True)
        yield
        S.I("dve", "tensor_copy", out=s_[:, 0:8], in_=pg[0:64, 0:8])
        S.I("act", "activation", out=s_[:, 8:16], in_=s_[:, 0:8], func=AF.Exp)
        S.I("dve", "tensor_tensor", out=D1[b][:], in0=pv(pG), in1=bc2(s_[:, 0:8], 64, 64), op=ALU.subtract)
        S.I("dve", "tensor_copy", out=s_[:, 16:24], in_=pv(pG)[:, :, 63])
        S.I("act", "activation", out=s_[:, 24:32], in_=s_[:, 16:24], func=AF.Exp)
        S.I("dve", "tensor_tensor", out=s_[:, 32:40], in0=s_[:, 16:24], in1=s_[:, 0:8], op=ALU.subtract)
        S.I("act", "activation", out=s_[:, 32:40], in_=s_[:, 32:40], func=AF.Exp)
        yield
        S.I("pool", "tensor_tensor", out=tmpd[b][:], in0=D1[b][:], in1=bc1(cm["m_incl_u"][:], 64, 8), op=ALU.add)
        S.I("act", "activation", out=ET[b][:], in_=tmpd[b][:], func=AF.Exp)
        S.I("dve", "scalar_tensor_tensor", out=EE[b][:], in0=D1[b][:], scalar=-1.0, in1=bc1(cm["m_incl_l"][:], 64, 8), op0=ALU.mult, op1=ALU.add)
        S.I("act", "activation", out=EE[b][:], in_=EE[b][:], func=AF.Exp)
        S.I("pool", "tensor_tensor", out=ETn[b][:], in0=ET[b][:], in1=bc1(cm["m_nstr_u"][:], 64, 8), op=ALU.mult)
        S.I("pool", "tensor_tensor", out=En[b][:], in0=EE[b][:], in1=bc1(cm["m_nstr_l"][:], 64, 8), op=ALU.mult)
        yield
        for (dst, src) in ((kT[b], kn), (qT[L], qn), (kbT[b], kb)):
            p = nps()
            tr8(p, lambda h, src=src: src[:, h, :])
            if dst is qT[L]:
                S.I("dve", "tensor_copy", out=dst[:], in_=pv(p))
                S.I("dve", "tensor_copy", out=qTs[L][:], in_=pv(p))
            else:
                evac(k, dst[:], pv(p))
            yield
        p = nps()
        mm8(p, lambda h: kT[b][:, h, :], lambda h: kbT[b][:, h, :])
        S.I("dve", "tensor_tensor", out=NTa[b][:], in0=pv(p), in1=ETn[b][:], op=ALU.mult)
        yield
        p = nps()
        mm8(p, lambda h: kbT[b][:, h, :], lambda h: kT[b][:, h, :])
        S.I("dve", "tensor_tensor", out=Na[b][:], in0=pv(p), in1=En[b][:], op=ALU.mult)
        yield
        p = nps()
        mm8(p, lambda h: kT[b][:, h, :], lambda h: qT[L][:, h, :])
        S.I("dve", "tensor_tensor", out=attnT[L][:], in0=pv(p), in1=ET[b][:], op=ALU.mult)
        S.I("pool", "tensor_copy", out=R6[b][:, :, 0:64], in_=vb)
        S.I("pool", "tensor_tensor", out=R6[b][:, :, 64:128], in0=kb, in1=bc2(s_[:, 8:16], 64, 64), op=ALU.mult)
        S.I("pool", "tensor_tensor", out=kdec[L][:], in0=kn, in1=bc2(s_[:, 32:40], 64, 64), op=ALU.mult)
        yield
        NTc, Nc, NTn, Nn = NTa[b], Na[b], NTb[b], Nb[b]
        Tm = Tmat[b]
        S.I("dve", "tensor_tensor", out=Tm[:], in0=Nc[:], in1=bc1(idf, 64, 8), op=ALU.add)
        for lvl in range(1, 6):
            p2 = nps()
            mm8(p2, lambda h: Nc[:, h, :], lambda h: NTc[:, h, :])
            S.I("act", "activation", out=NTn[:], in_=pv(p2), func=AF.Copy)
            yield
            if lvl < 5:
                p1 = nps()
                tr8(p1, lambda h, NTn=NTn: NTn[:, h, :])
                S.I("act", "activation", out=Nn[:], in_=pv(p1), func=AF.Copy)
            NTc, Nc, NTn, Nn = NTn, Nn, NTc, Nc
            pY = nps()
            mm8(pY, lambda h: NTc[:, h, :], lambda h: Tm[:, h, :])
            S.I("dve", "tensor_tensor", out=Tm[:], in0=Tm[:], in1=pv(pY), op=ALU.add)
            yield
        pT_ = nps()
        tr8(pT_, lambda h: Tm[:, h, :])
        S.I("act", "activation", out=TTs[b][:], in_=pv(pT_), func=AF.Copy)
        yield
        pA, pB = nps(), nps()
        for h in range(8):
            pp = pA if h < 4 else pB
            S.I("pe", "matmul", out=pp[0:64, (h % 4) * 128:(h % 4 + 1) * 128], lhsT=TTs[b][:, h, :], rhs=R6[b][:, h, :], start=True, stop=True)
        S.I("dve", "tensor_copy", out=X6[L][:, 0:4, :], in_=v3(pA[0:64, :], 4))
        S.I("dve", "tensor_copy", out=X6[L][:, 4:8, :], in_=v3(pB[0:64, :], 4))
        yield
        p = nps()
        tr8(p, lambda h: X6[L][:, h, 64:128])
        evac(k, wT[L][:], pv(p))
        yield

    def scan(n):
        b, L = n % 2, n % (2 * NS)
        s_ = sm[L]
        S.dma(out=Z[b][:], in_=k.d["z_d"].ap()[n * C:(n + 1) * C, :])
        S.I("act", "activation", out=Z[b][:], in_=Z[b][:], func=AF.Silu)
        p = pbf[0]
        mm8(p, lambda h: wT[L][:, h, :], lambda h: Stb[:, h, :])
        S.I("dve", "tensor_tensor", out=vnew[b][:], in0=X6[L][:, :, 0:64], in1=pv(p), op=ALU.subtract)
        yield
        pq = pbf[1]
        mm8(pq, lambda h: qTs[L][:, h, :], lambda h: Stb[:, h, :])
        S.I("dve", "tensor_tensor", out=osb[b][:], in0=pv(pq), in1=bc2(s_[:, 8:16], 64, 64), op=ALU.mult)
        yield
        pa_ = pbf[0]
        mm8(pa_, lambda h: attnT[L][:, h, :], lambda h: vnew[b][:, h, :])
        S.I("dve", "tensor_tensor", out=osb[b][:], in0=osb[b][:], in1=pv(pa_), op=ALU.add)
        yield
        pk_ = pbf[1]
        mm8(pk_, lambda h: kdec[L][:, h, :], lambda h: vnew[b][:, h, :])
        S.I("dve", "tensor_tensor", out=St[:], in0=St[:], in1=bc2(s_[:, 24:32], 64, 64), op=ALU.mult)
        S.I("dve", "tensor_tensor", out=St[:], in0=St[:], in1=pv(pk_), op=ALU.add)
        S.I("act", "activation", out=Stb[:], in_=St[:], func=AF.Copy)
        yield
        S.I("act", "activation", out=v3(odn[b][:], 8), in_=osb[b][:], func=AF.Square)
        S.I("dve", "tensor_reduce", out=s_[:, 40:48], in_=v3(odn[b][:], 8), axis=AX.X, op=ALU.add)
        S.I("dve", "tensor_scalar", out=s_[:, 40:48], in0=s_[:, 40:48], scalar1=1.0 / 64, scalar2=EPS, op0=ALU.mult, op1=ALU.add)
        S.I("act", "activation", out=s_[:, 40:48], in_=s_[:, 40:48], func=AF.Sqrt)
        S.I("dve", "reciprocal", out=s_[:, 48:56], in_=s_[:, 40:48])
        yield
        S.I("pool", "tensor_tensor", out=osb[b][:], in0=osb[b][:], in1=bc2(s_[:, 48:56], 64, 64), op=ALU.mult)
        S.I("pool", "tensor_tensor", out=osb[b][:], in0=osb[b][:], in1=bc1(gng[:], 64, 8), op=ALU.mult)
        S.I("pool", "tensor_tensor", out=odn[b][:], in0=osb[b][:].rearrange("p a b -> p (a b)"), in1=Z[b][:], op=ALU.mult)
        S.dma(out=k.d["odn_d"].ap()[n * C:(n + 1) * C, :], in_=odn[b][:])
        yield

    def scans(n0):
        for n in range(n0, min(n0 + NS, NCH)):
            for _ in scan(n):
                yield

    def lockstep(gens):
        gens = list(gens)
        while gens:
            for g_ in list(gens):
                try:
                    next(g_)
                except StopIteration:
                    gens.remove(g_)

    lockstep([pre(i) for i in range(NS)])
    for p_ in range((NCH + NS - 1) // NS):
        gens = [pre(n) for n in range(NS * p_ + NS, min(NS * p_ + 2 * NS, NCH))]
        gens.append(scans(NS * p_))
        lockstep(gens)
    S.pop()
    return []


def phase_merge(k):
    S, nc = k.S, k.nc
    S.push()
    wbn = S.sbuf("wbn", [128, 4, DM], BF16)
    wbd = S.sbuf("wbd", [128, 4, DM], BF16)
    wo = S.sbuf("wo", [128, 8, DM], BF16)
    wstg = [S.sbuf(f"wstg{i}", [128, DM], F32) for i in range(2)]
    n = 0
    for (dst, src, nk) in ((wbn, "w_branch_nsa", 4), (wbd, "w_branch_dn", 4), (wo, "w_out", 8)):
        for kc in range(nk):
            st = wstg[n % 2]
            n += 1
            S.dma(out=st[:], in_=k.inp[src].ap()[kc * 128:(kc + 1) * 128, :])
            S.I("pool", "tensor_copy", out=dst[:, kc, :], in_=st[:])
    onT = S.sbuf("onT", [128, 4, SEQ], BF16)
    odT = S.sbuf("odT", [128, 4, SEQ], BF16)
    S.dma(out=onT[:], in_=k.d["onsaT_d"].ap().rearrange("(j p) t -> p j t", p=128))
    odin = [S.sbuf(f"odin{i}", [128, 512], F32) for i in range(2)]
    odb = [S.sbuf(f"odb{i}", [128, 512], BF16) for i in range(2)]
    for i in range(NT):
        o, ob_ = odin[i % 2], odb[i % 2]
        S.dma(out=o[:], in_=k.d["odn_d"].ap()[i * 128:(i + 1) * 128, :])
        S.I("pool", "tensor_copy", out=ob_[:], in_=o[:])
        pt = k.pb[i % 2]
        for j in range(4):
            S.I("pe", "transpose", out=pt[:, j * 128:(j + 1) * 128], in_=ob_[:, j * 128:(j + 1) * 128], identity=k.ident_b[:])
        evac(k, odT[:, :, i * 128:(i + 1) * 128], pt[:, 0:512].rearrange("p (a b) -> p a b", a=4))
    mT = [S.sbuf(f"mT{i}", [128, 8, 512], BF16) for i in range(2)]
    gnb = [S.sbuf(f"gnb{i}", [128, 512], BF16) for i in range(2)]
    gdb = [S.sbuf(f"gdb{i}", [128, 512], BF16) for i in range(2)]
    t1 = [S.sbuf(f"mt1{i}", [128, 512], F32) for i in range(2)]
    t2 = [S.sbuf(f"mt2{i}", [128, 512], F32) for i in range(2)]
    xt = [S.sbuf(f"mxt{i}", [128, DM], F32) for i in range(2)]
    yt = [S.sbuf(f"myt{i}", [128, DM], F32) for i in range(2)]
    x1 = [S.sbuf(f"mx1{i}", [128, DM], F32) for i in range(2)]
    h2 = [S.sbuf(f"mh2{i}", [128, DM], F32) for i in range(2)]
    tmp = [S.sbuf(f"mtmp{i}", [128, DM], F32) for i in range(2)]
    junk = S.sbuf("mjunk", [128, DM], F32)
    ss = [S.sbuf(f"mss{i}", [128, 4], F32) for i in range(2)]
    q = 0
    for G in range(8):
        m_ = mT[G % 2]
        for m in range(8):
            p1, p2 = k.ps[(2 * q) % 4], k.ps[(2 * q + 1) % 4]
            b = q % 2
            q += 1
            for kc in range(4):
                S.I("pe", "matmul", out=p1[:], lhsT=wbn[:, kc, m * 128:(m + 1) * 128], rhs=onT[:, kc, G * 512:(G + 1) * 512], start=(kc == 0), stop=(kc == 3))
            for kc in range(4):
                S.I("pe", "matmul", out=p2[:], lhsT=wbd[:, kc, m * 128:(m + 1) * 128], rhs=odT[:, kc, G * 512:(G + 1) * 512], start=(kc == 0), stop=(kc == 3))
            S.dma(out=gnb[b][:], in_=k.d["gnT_d"].ap()[m * 128:(m + 1) * 128, G * 512:(G + 1) * 512])
            S.dma(out=gdb[b][:], in_=k.d["gdT_d"].ap()[m * 128:(m + 1) * 128, G * 512:(G + 1) * 512])
            S.I("dve", "tensor_tensor", out=t1[b][:], in0=p1[:], in1=gnb[b][:], op=ALU.mult)
            S.I("dve", "tensor_tensor", out=t2[b][:], in0=p2[:], in1=gdb[b][:], op=ALU.mult)
            S.I("pool", "tensor_tensor", out=m_[:, m, :], in0=t1[b][:], in1=t2[b][:], op=ALU.add)
        for c in range(4):
            i = 4 * G + c
            b = i % 2
            pys = [k.ps[4], k.ps[5]]
            for half in range(2):
                for m in range(8):
                    S.I("pe", "matmul", out=pys[half][:], lhsT=m_[:, m, c * 128:(c + 1) * 128], rhs=wo[:, m, half * 512:(half + 1) * 512],
                        start=(m == 0), stop=(m == 7))
            S.dma(out=xt[b][:], in_=k.inp["x"].ap()[i * 128:(i + 1) * 128, :])
            for half in range(2):
                S.I("dve", "tensor_tensor", out=yt[b][:, half * 512:(half + 1) * 512], in0=pys[half][:], in1=k.modb[:, 2 * DM + half * 512:2 * DM + (half + 1) * 512], op=ALU.mult)
            S.I("pool", "tensor_tensor", out=x1[b][:], in0=yt[b][:], in1=xt[b][:], op=ALU.add)
            S.dma(out=k.d["x1_d"].ap()[i * 128:(i + 1) * 128, :], in_=x1[b][:])
            rms_mod_tile(k, x1[b][:], k.modb[:, 4 * DM:5 * DM], k.modb[:, 3 * DM:4 * DM], h2[b][:], tmp[b][:], ss[b], junk[:])
            S.dma(out=k.d["h2_d"].ap()[i * 128:(i + 1) * 128, :], in_=h2[b][:])
    S.pop()
    return []


def phase_peer(k):
    S, nc = k.S, k.nc
    S.push()
    uv_d = k.d["uv_d"]
    wq = S.sbuf("pwq", [128, 8, DM], BF16)
    keysbd = S.sbuf("pkeysbd", [128, 8, 256], BF16)
    S.push()
    wstg = [S.sbuf(f"pwstg{i}", [128, DM], F32) for i in range(2)]
    for kc in range(8):
        st = wstg[kc % 2]
        S.dma(out=st[:], in_=k.inp["peer_w_query"].ap()[kc * 128:(kc + 1) * 128, :])
        S.I("pool", "tensor_copy", out=wq[:, kc, :], in_=st[:])
    kst = S.sbuf("pkst", [128, 8, 256], F32)
    S.I("pool", "memset", kst[:], 0.0, extra_writes=[kst[:]])
    for h in range(8):
        S.dma(out=kst[0:64, h, 0:128], in_=k.inp["peer_keys1"].ap()[h].rearrange("k d -> d k"), allow_slow_non_contiguous=True)
        S.dma(out=kst[64:128, h, 128:256], in_=k.inp["peer_keys2"].ap()[h].rearrange("k d -> d k"), allow_slow_non_contiguous=True)
    S.I("pool", "tensor_copy", out=keysbd[:], in_=kst[:])
    S.pop()
    fgb = S.sbuf("pfgb", [128, DM], F32)
    S.dma(out=fgb[:], in_=k.inp["final_g"].ap().partition_broadcast(128))
    iota16 = S.sbuf("piota", [128, 16], F32)
    S.dma(out=iota16[:], in_=k.cst["iota16"].ap())
    NB = 16
    gbuf = [S.sbuf(f"pgb{i}", [128, 2 * DM], BF16) for i in range(NB)]
    diag = [S.sbuf(f"pdiag{i}", [128, 128], BF16) for i in range(4)]
    h2t = [S.sbuf(f"ph2t{i}", [128, DM], F32) for i in range(2)]
    h2b = [S.sbuf(f"ph2b{i}", [128, DM], BF16) for i in range(2)]
    h2T = [S.sbuf(f"ph2T{i}", [128, 8, 128], BF16) for i in range(2)]
    qryT = [S.sbuf(f"pqryT{i}", [128, 8, 128], BF16) for i in range(2)]
    scs = [S.sbuf(f"pscs{i}", [128, 8, 256], F32) for i in range(2)]
    s1r = S.sbuf("ps1r", [128, 128], F32)
    cand = S.sbuf("pcand", [128, 16, 16], F32)
    candr = S.sbuf("pcandr", [128, 16, 16], F32)
    v1 = S.sbuf("pv1", [128, 8, 16], F32)
    v2 = S.sbuf("pv2", [128, 8, 16], F32)
    i1 = S.sbuf("pi1", [128, 8, 16], U32)
    i2 = S.sbuf("pi2", [128, 8, 16], U32)
    ts = S.sbuf("pts", [128, 8, 16], F32)
    pos = S.sbuf("ppos", [128, 8, 16], U32)
    ra = S.sbuf("pra", [128, 8, 16], U32)
    rb = S.sbuf("prb", [128, 8, 16], U32)
    raf = S.sbuf("praf", [128, 8, 16], F32)
    rbf = S.sbuf("prbf", [128, 8, 16], F32)
    i1f = S.sbuf("pi1f", [128, 8, 16], F32)
    i2f = S.sbuf("pi2f", [128, 8, 16], F32)
    oh = S.sbuf("poh", [128, 8, 16, 16], F32)
    sel1 = S.sbuf("psel1", [128, 8, 16], F32)
    sel2 = S.sbuf("psel2", [128, 8, 16], F32)
    eidf = S.sbuf("peidf", [128, 128], F32)
    eidx = [S.sbuf(f"peidx{i}", [128, 128], I32) for i in range(2)]
    gate = [S.sbuf(f"pgate{i}", [128, 8, 16], F32) for i in range(2)]
    gsm = S.sbuf("pgsm", [128, 32], F32)
    av = [S.sbuf(f"pav{i}", [128, 128], F32) for i in range(2)]
    gt1 = S.sbuf("pgt1", [128, 128], F32)
    gt2 = S.sbuf("pgt2", [128, 128], F32)
    coef = [S.sbuf(f"pcoef{i}", [128, 128], F32) for i in range(2)]
    junk = S.sbuf("pjunk", [128, DM], F32)
    junk2 = S.sbuf("pjunk2", [128, DM], F32)
    acc = [S.sbuf(f"pacc{i}", [128, DM], F32) for i in range(2)]
    x1t = [S.sbuf(f"px1t{i}", [128, DM], F32) for i in range(2)]
    ss = [S.sbuf(f"pss{i}", [128, 4], F32) for i in range(2)]
    outs = []
    nt_run = NT if k.sub != "peer1" else 1
    gcount = [0]

    def prep(i):
        b = i % 2
        S.dma(out=h2t[b][:], in_=k.d["h2_d"].ap()[i * 128:(i + 1) * 128, :])
        S.I("pool", "tensor_copy", out=h2b[b][:], in_=h2t[b][:])
        pt = k.pb[b]
        ptv = pt[:].rearrange("p (a b) -> p a b", a=8)
        for kc in range(8):
            S.I("pe", "transpose", out=ptv[:, kc, :], in_=h2b[b][:, kc * 128:(kc + 1) * 128], identity=k.ident_b[:])
        evac(k, h2T[b][:], ptv, eng="act")
        for hh in range(2):
            pq = k.ps[hh]
            for h4 in range(4):
                h = hh * 4 + h4
                for kc in range(8):
                    S.I("pe", "matmul", out=pq[:, h4 * 128:(h4 + 1) * 128], lhsT=wq[:, kc, h * 128:(h + 1) * 128], rhs=h2T[b][:, kc, :],
                        start=(kc == 0), stop=(kc == 7))
            evac(k, qryT[b][:, hh * 4:hh * 4 + 4, :], pq[:].rearrange("p (a b) -> p a b", a=4), eng="act")
        for h2_ in range(4):
            psc = k.ps[2 + (h2_ % 2)]
            for e in range(2):
                h = h2_ * 2 + e
                S.I("pe", "matmul", out=psc[:, e * 256:(e + 1) * 256], lhsT=qryT[b][:, h, :], rhs=keysbd[:, h, :], start=True, stop=True)
            S.I("dve", "tensor_copy", out=scs[b][:, 2 * h2_:2 * h2_ + 2, :], in_=psc[:].rearrange("p (a b) -> p a b", a=2))
        yield
        for h in range(8):
            for (vv, ii, off) in ((v1, i1, 0), (v2, i2, 128)):
                s_in = scs[b][:, h, off:off + 128]
                S.I("dve", "max", out=vv[:, h, 0:8], in_=s_in)
                S.I("dve", "max_index", out=ii[:, h, 0:8], in_max=vv[:, h, 0:8], in_values=s_in)
                S.I("dve", "match_replace", out=s1r[:], in_to_replace=vv[:, h, 0:8], in_values=s_in, imm_value=-3.0e38)
                S.I("dve", "max", out=vv[:, h, 8:16], in_=s1r[:])
                S.I("dve", "max_index", out=ii[:, h, 8:16], in_max=vv[:, h, 8:16], in_values=s1r[:])
            S.I("dve", "tensor_tensor", out=cand[:], in0=v1[:, h, :].unsqueeze(2).to_broadcast([128, 16, 16]),
                in1=v2[:, h, :].unsqueeze(1).to_broadcast([128, 16, 16]), op=ALU.add)
            cf = cand[:].rearrange("p a b -> p (a b)")
            crf = candr[:].rearrange("p a b -> p (a b)")
            S.I("dve", "max", out=ts[:, h, 0:8], in_=cf)
            S.I("dve", "max_index", out=pos[:, h, 0:8], in_max=ts[:, h, 0:8], in_values=cf)
            S.I("dve", "match_replace", out=crf, in_to_replace=ts[:, h, 0:8], in_values=cf, imm_value=-3.0e38)
            S.I("dve", "max", out=ts[:, h, 8:16], in_=crf)
            S.I("dve", "max_index", out=pos[:, h, 8:16], in_max=ts[:, h, 8:16], in_values=crf)
            yield
        g_ = gate[b]
        S.I("dve", "tensor_tensor", out=g_[:], in0=ts[:], in1=ts[:, :, 0:1].to_broadcast([128, 8, 16]), op=ALU.subtract)
        S.I("act", "activation", out=g_[:], in_=g_[:], func=AF.Exp)
        S.I("dve", "tensor_reduce", out=gsm[:, 0:8], in_=g_[:], axis=AX.X, op=ALU.add)
        S.I("dve", "reciprocal", out=gsm[:, 8:16], in_=gsm[:, 0:8])
        S.I("dve", "tensor_tensor", out=g_[:], in0=g_[:], in1=gsm[:, 8:16].unsqueeze(2).to_broadcast([128, 8, 16]), op=ALU.mult)
        S.I("dve", "tensor_single_scalar", out=ra[:], in_=pos[:], scalar=4, op=ALU.logical_shift_right)
        S.I("dve", "tensor_single_scalar", out=rb[:], in_=pos[:], scalar=15, op=ALU.bitwise_and)
        for (src, dst) in ((ra, raf), (rb, rbf), (i1, i1f), (i2, i2f)):
            S.I("dve", "tensor_copy", out=dst[:], in_=src[:])
        iob = iota16[:].unsqueeze(1).unsqueeze(1).to_broadcast([128, 8, 16, 16])
        for (rf, idf_, sel) in ((raf, i1f, sel1), (rbf, i2f, sel2)):
            S.I("dve", "tensor_tensor", out=oh[:], in0=rf[:].unsqueeze(3).to_broadcast([128, 8, 16, 16]), in1=iob, op=ALU.is_equal)
            S.I("dve", "tensor_tensor", out=oh[:], in0=oh[:], in1=idf_[:].unsqueeze(2).to_broadcast([128, 8, 16, 16]), op=ALU.mult)
            S.I("dve", "tensor_reduce", out=sel[:], in_=oh[:], axis=AX.X, op=ALU.add)
        S.I("dve", "scalar_tensor_tensor", out=eidf[:], in0=sel1[:].rearrange("p a b -> p (a b)"), scalar=128.0,
            in1=sel2[:].rearrange("p a b -> p (a b)"), op0=ALU.mult, op1=ALU.add)
        S.I("dve", "tensor_copy", out=eidx[b][:], in_=eidf[:])

    def evalx(i, mid=None):
        b = i % 2
        py = [k.ps[4], k.ps[5]]
        for grp in range(16):
            sl = slice(grp * 8, grp * 8 + 8)
            bufs = []
            for jj in range(8):
                j = grp * 8 + jj
                gb = gbuf[gcount[0] % NB]
                gcount[0] += 1
                bufs.append(gb)
                S.idma(out=gb[:], in_=uv_d.ap(), idx_ap=eidx[b][:, j:j + 1])
                S.I("dve", "scalar_tensor_tensor", out=junk2[:], in0=h2t[b][:], scalar=1.0, in1=gb[:, 0:DM], op0=ALU.mult, op1=ALU.mult,
                    accum_out=av[b][:, j:j + 1])
            S.I("dve", "tensor_tensor", out=gt1[:, sl], in0=av[b][:, sl], in1=av[b][:, sl], op=ALU.mult)
            S.I("dve", "tensor_scalar", out=gt1[:, sl], in0=gt1[:, sl], scalar1=0.044715, scalar2=1.0, op0=ALU.mult, op1=ALU.add)
            S.I("dve", "tensor_tensor", out=gt2[:, sl], in0=gt1[:, sl], in1=av[b][:, sl], op=ALU.mult)
            S.I("act", "activation", out=gt1[:, sl], in_=gt2[:, sl], func=AF.Sigmoid, scale=GELU_C)
            S.I("dve", "tensor_tensor", out=gt2[:, sl], in0=gt1[:, sl], in1=av[b][:, sl], op=ALU.mult)
            S.I("dve", "tensor_tensor", out=coef[b][:, sl], in0=gt2[:, sl], in1=gate[b][:].rearrange("p a b -> p (a b)")[:, sl], op=ALU.mult)
            for jj in range(8):
                j = grp * 8 + jj
                dg = diag[j % 4]
                S.I("act", "activation", out=dg[:], in_=k.ident_b[:], func=AF.Copy, scale=coef[b][:, j:j + 1])
                for half in range(2):
                    S.I("pe", "matmul", out=py[half][:], lhsT=dg[:], rhs=bufs[jj][:, DM + half * 512:DM + (half + 1) * 512],
                        start=(j == 0), stop=(j == 127))
            if mid is not None and grp >= 2:
                try:
                    next(mid)
                except StopIteration:
                    mid = None
        return py

    def final(i, py):
        b = i % 2
        S.dma(out=x1t[b][:], in_=k.d["x1_d"].ap()[i * 128:(i + 1) * 128, :])
        for half in range(2):
            S.I("dve", "tensor_tensor", out=acc[b][:, half * 512:(half + 1) * 512], in0=py[half][:], in1=k.modb[:, 5 * DM + half * 512:5 * DM + (half + 1) * 512], op=ALU.mult)
        S.I("pool", "tensor_tensor", out=x1t[b][:], in0=acc[b][:], in1=x1t[b][:], op=ALU.add)
        s_ = ss[b]
        S.I("act", "activation", out=junk[:], in_=x1t[b][:], func=AF.Square, accum_out=s_[:, 0:1])
        S.I("dve", "tensor_scalar", out=s_[:, 1:2], in0=s_[:, 0:1], scalar1=1.0 / DM, scalar2=EPS, op0=ALU.mult, op1=ALU.add)
        S.I("act", "activation", out=s_[:, 2:3], in_=s_[:, 1:2], func=AF.Sqrt)
        S.I("dve", "reciprocal", out=s_[:, 3:4], in_=s_[:, 2:3])
        S.I("dve", "scalar_tensor_tensor", out=acc[b][:], in0=x1t[b][:], scalar=s_[:, 3:4], in1=fgb[:], op0=ALU.mult, op1=ALU.mult)
        outs.append(S.dma(out=k.out.ap()[i * 128:(i + 1) * 128, :], in_=acc[b][:]))

    for _ in prep(0):
        pass
    for i in range(nt_run):
        gen = prep(i + 1) if i + 1 < nt_run else None
        py = evalx(i, mid=gen)
        if gen is not None:
            for _ in gen:
                pass
        final(i, py)
    S.pop()
    return outs


_CACHE = {}


def make_in_maps(inputs, consts, used=None):
    maps = []
    for b in range(8):
        m = {}
        for n, shp in INPUT_SPECS.items():
            if used is not None and n not in used:
                continue
            a = np.asarray(inputs[n])
            if n == "x" or n == "c":
                a = a[b]
            elif n in ("rel_bias", "final_g"):
                pass
            else:
                a = a[0]
            m[n] = np.ascontiguousarray(a.reshape(shp).astype(np.float32, copy=False))
        for n in CONST_SPECS:
            if used is None or n in used:
                m[n] = consts[n]
        maps.append(m)
    return maps


def kernel(**inputs):
    consts = host_consts()
    nc = build_program()
    res = run_bass_kernel_spmd(nc, make_in_maps(inputs, consts, nc.used_inputs), core_ids=list(range(8)))
    return np.stack([np.asarray(r["out"]).reshape(SEQ, DM) for r in res.results], axis=0).astype(np.float32)
```

```python
import numpy as np
import concourse.bass as bass
import concourse.mybir as mybir
from contextlib import ExitStack

F32 = mybir.dt.float32
BF16 = mybir.dt.bfloat16
U32 = mybir.dt.uint32
I32 = mybir.dt.int32
ALU = mybir.AluOpType
AF = mybir.ActivationFunctionType
AX = mybir.AxisListType

EPOCH = 1 << 30
ENGS = ("pe", "act", "dve", "pool", "sp")
WRITE_KW = ("out", "accum_out", "out_max", "out_indices")


class Res:
    __slots__ = ("name", "last_w", "readers", "children", "parent")

    def __init__(self, name, parent=None):
        self.name = name
        self.last_w = None
        self.readers = []
        self.children = {}
        self.parent = parent


class Op:
    __slots__ = ("eng", "fn", "deps", "needed", "is_dma", "sig", "idx", "slotwait", "name")

    def __init__(self, eng, fn, is_dma=False, name=""):
        self.eng = eng
        self.fn = fn
        self.deps = []
        self.needed = False
        self.is_dma = is_dma
        self.sig = None
        self.slotwait = None
        self.name = name


class Sched:
    def __init__(self, nc, dma_slots=None):
        self.nc = nc
        self.ops = {e: [] for e in ENGS}
        self.res = {}
        self.tags = {}
        self._keep = []
        self.dma_count = {e: 0 for e in ENGS}
        self.dma_ops = {e: [] for e in ENGS}
        self.dma_slots = dma_slots or {"sp": 8, "pool": 16, "act": 4}
        self.stack = ExitStack()
        self.scopes = []
        self.nbar = {}
        self.all_ops = []

    def push(self):
        self.scopes.append(ExitStack())

    def pop(self):
        self.barrier()
        self.scopes.pop().close()

    def sbuf(self, name, shape, dtype):
        t = (self.scopes[-1] if self.scopes else self.stack).enter_context(self.nc.sbuf_tensor(name, list(shape), dtype))
        self.res[t.name] = Res(t.name)
        return t

    def psum(self, name, shape, dtype):
        t = self.stack.enter_context(self.nc.psum_tensor(name, list(shape), dtype))
        self.res[t.name] = Res(t.name)
        return t

    def dram(self, name, shape, dtype, kind="Internal"):
        t = self.nc.dram_tensor(name, list(shape), dtype, kind=kind)
        self.res[t.name] = Res(t.name)
        return t

    def tag(self, ap, key):
        base = self.res[ap.name]
        if key not in base.children:
            base.children[key] = Res(base.name + ":" + str(key), parent=base)
        self.tags[id(ap)] = base.children[key]
        self._keep.append(ap)
        return ap

    def _res_of(self, ap):
        r = self.tags.get(id(ap))
        if r is not None:
            return r
        nm = ap.name
        if nm not in self.res:
            self.res[nm] = Res(nm)
        return self.res[nm]

    def _related(self, r):
        out = [r]
        if r.parent is not None:
            out.append(r.parent)
        out.extend(r.children.values())
        return out

    def _track(self, op, reads, writes):
        deps = set()
        for r in reads:
            for rr in self._related(r):
                if rr.last_w is not None:
                    deps.add(rr.last_w)
        for w in writes:
            for rr in self._related(w):
                if rr.last_w is not None:
                    deps.add(rr.last_w)
                for q in rr.readers:
                    deps.add(q)
        deps.discard(op)
        for r in reads:
            r.readers.append(op)
        for w in writes:
            w.last_w = op
            w.readers = []
        best = {}
        final = []
        for d in deps:
            if d.is_dma:
                final.append(d)
            else:
                if d.eng == "pe" and op.eng == "pe":
                    continue
                b = best.get(d.eng)
                if b is None or d.idx > b.idx:
                    best[d.eng] = d
        final.extend(best.values())
        for d in final:
            d.needed = True
        op.deps = final

    def _add(self, op, reads, writes):
        op.idx = len(self.ops[op.eng])
        self.ops[op.eng].append(op)
        self.all_ops.append(op)
        self._track(op, reads, writes)
        return op

    def I(self, eng, meth, *args, extra_reads=(), extra_writes=(), **kw):
        reads, writes = [], []
        for k, v in kw.items():
            if hasattr(v, "tensor") and hasattr(v, "ap"):
                (writes if k in WRITE_KW else reads).append(self._res_of(v))
        for v in args:
            if hasattr(v, "tensor") and hasattr(v, "ap"):
                reads.append(self._res_of(v))
        for v in extra_reads:
            reads.append(self._res_of(v))
        for v in extra_writes:
            writes.append(self._res_of(v))

        def fn(e, meth=meth, args=args, kw=kw):
            return getattr(e, meth)(*args, **kw)

        return self._add(Op(eng, fn, name=meth), reads, writes)

    def dma(self, out, in_, q="sp", **kw):
        def fn(e, out=out, in_=in_, kw=kw):
            return e.dma_start(out=out, in_=in_, **kw)

        op = Op(q, fn, is_dma=True, name="dma")
        op.needed = True
        d = self.dma_count[q]
        self.dma_count[q] += 1
        op.sig = ("dma", q, d)
        self.dma_ops[q].append(op)
        return self._add(op, [self._res_of(in_)], [self._res_of(out)])

    def idma(self, out, in_, idx_ap, axis=0, lean=False, **kw):
        def fn(e, out=out, in_=in_, idx_ap=idx_ap, kw=kw):
            return e.indirect_dma_start(out=out, out_offset=None, in_=in_,
                                        in_offset=bass.IndirectOffsetOnAxis(ap=idx_ap, axis=axis), **kw)

        q = "pool"
        op = Op(q, fn, is_dma=True, name="idma")
        op.needed = True
        d = self.dma_count[q]
        self.dma_count[q] += 1
        op.sig = ("dma", q, d)
        self.dma_ops[q].append(op)
        self._add(op, [self._res_of(in_), self._res_of(idx_ap)], [self._res_of(out)])
        if lean:
            op.slotwait = "skip"
            if any((not d_.is_dma) and d_.eng == "pe" for d_ in op.deps):
                op.deps = [d_ for d_ in op.deps if d_.is_dma or d_.eng != "dve"]
        return op

    def barrier(self):
        marks = []
        for e in ENGS:
            last = None
            for op in reversed(self.ops[e]):
                if (not op.is_dma) and op.fn is not None and op.fn != "SEMINC":
                    last = op
                    break
            if last is not None:
                last.needed = True
                marks.append(last)
            P = self.dma_slots.get(e, 0)
            if P and self.dma_ops[e]:
                sg = Op(e, "SEMINC", name="barsig")
                sg.idx = len(self.ops[e])
                sg.deps = list(self.dma_ops[e][-P:])
                sg.needed = True
                self.nbar[e] = self.nbar.get(e, 0) + 1
                sg.sig = ("b", e, self.nbar[e])
                self.ops[e].append(sg)
                self.all_ops.append(sg)
                marks.append(sg)
        for e in ENGS:
            op = Op(e, None, name="barwait")
            op.idx = len(self.ops[e])
            op.deps = [m for m in marks if m.eng != e]
            self.ops[e].append(op)
            self.all_ops.append(op)
        for r in self.res.values():
            r.last_w = None
            r.readers = []
            for c in r.children.values():
                c.last_w = None
                c.readers = []

    def final_wait(self, dma_ops):
        op = Op("sp", None, name="finalwait")
        op.idx = len(self.ops["sp"])
        op.deps = list(dma_ops)
        self.ops["sp"].append(op)

    def emit(self):
        nc = self.nc
        sems = {}
        nsig = {}
        for e in ENGS:
            k = 0
            for op in self.ops[e]:
                if op.is_dma:
                    continue
                if op.needed and op.fn is not None and op.fn != "SEMINC":
                    op.sig = ("c", e, k)
                    k += 1
            nsig[e] = k
            n_ep = (k + EPOCH - 1) // EPOCH
            sems[e] = [self.stack.enter_context(nc.semaphore(f"s_{e}_{i}")) for i in range(n_ep)]
        dsems = {}
        for e in ENGS:
            if self.dma_count[e] > 0:
                P = self.dma_slots[e]
                dsems[e] = [self.stack.enter_context(nc.semaphore(f"d_{e}_{i}")) for i in range(P)]
                pass

        bsems = {e: self.stack.enter_context(nc.semaphore(f"b_{e}")) for e in self.nbar}

        def sigval(op):
            kind, e, k = op.sig
            if kind == "b":
                return bsems[e], k
            if kind == "c":
                return sems[e][k // EPOCH], k % EPOCH + 1
            P = self.dma_slots[e]
            return dsems[e][k % P], 16 * (k // P + 1)

        self.stats = {e: len(self.ops[e]) for e in ENGS}

        def run(e, eng):
            waited = {}

            def wait(sem, val):
                key = sem.num if hasattr(sem, "num") else id(sem)
                if waited.get(key, 0) >= val:
                    return
                eng.wait_ge(sem, val)
                waited[key] = val

            for op in self.ops[e]:
                for d in op.deps:
                    s, v = sigval(d)
                    wait(s, v)
                if op.is_dma and op.slotwait != "skip":
                    kind, q, k = op.sig
                    P = self.dma_slots[q]
                    if k >= P:
                        wait(dsems[q][k % P], 16 * (k // P))
                if op.fn is None:
                    continue
                if op.fn == "SEMINC":
                    s, v = sigval(op)
                    eng.sem_inc(s, 1)
                    continue
                ins = op.fn(eng)
                if op.needed:
                    s, v = sigval(op)
                    ins.then_inc(s, 16 if op.is_dma else 1)

        with nc.Block() as block:
            @block.tensor
            def _(eng):
                run("pe", eng)

            @block.scalar
            def _(eng):
                run("act", eng)

            @block.vector
            def _(eng):
                run("dve", eng)

            @block.gpsimd
            def _(eng):
                run("pool", eng)

            @block.sync
            def _(eng):
                run("sp", eng)

    def close(self):
        self.stack.close()
import math
import ml_dtypes
from concourse.bass_utils import run_bass_kernel_spmd

SEQ = 4096
DM = 1024
NT = SEQ // 128
IN_COLS = 5416
C_Q, C_KC, C_VC, C_KS, C_VS, C_KW, C_VW, C_GT = 0, 512, 640, 768, 896, 1024, 1152, 1280
C_DQ, C_DK, C_DV, C_DZ, C_DB, C_DA, C_GN, C_GD = 1304, 1816, 2328, 2840, 3352, 3360, 3368, 4392
NEG = -30000.0
EPS = 1e-6
GELU_C = 1.5957691216057308


def _rel_bucket(dist):
    dist = np.maximum(dist, 0)
    scaled = np.log(np.maximum(dist, 1).astype(np.float32) / 16) / np.float32(math.log(128 / 16))
    large = np.minimum(16 + (scaled.astype(np.float32) * 16).astype(np.int32), 31)
    return np.where(dist < 16, dist, large)


def host_consts():
    c = {}
    c["ident_f"] = np.eye(128, dtype=np.float32)
    c["ident_b"] = np.eye(128, dtype=np.float32).astype(ml_dtypes.bfloat16)
    c["anti_f"] = np.eye(128, dtype=np.float32)[::-1].copy()
    m = np.arange(16384) - 8192
    bucket = _rel_bucket(m)
    oh = np.zeros((33, 16384), np.float32)
    pos = m >= 0
    oh[bucket[pos], np.nonzero(pos)[0]] = 1.0
    oh[31, pos] -= 1.0
    oh[32, ~pos] = 1.0
    c["ohp"] = oh
    t = np.arange(SEQ)
    cur = t // 64
    j = np.arange(64)[None, :]
    forced = (j == 0) | (j == cur[:, None]) | (j == cur[:, None] - 1)
    future = j * 64 > t[:, None]
    c["slc_add"] = np.where(future, np.float32(-1e30), np.where(forced, np.float32(1e4), np.float32(0))).astype(np.float32)
    c["e_blk"] = (np.arange(SEQ)[None, :] // 64 == np.arange(64)[:, None]).astype(np.float32).astype(ml_dtypes.bfloat16)
    p = np.arange(128)[:, None]
    f = np.arange(128)[None, :]
    c["w4t"] = np.where(f >= p, np.float32(NEG), np.float32(0)).astype(ml_dtypes.bfloat16)
    n = np.arange(255)
    cs, ce = n * 16, n * 16 + 31
    ss = np.arange(64) * 64
    c["overlap"] = ((cs[:, None] <= ss[None, :] + 63) & (ce[:, None] >= ss[None, :])).astype(np.float32)
    c["m_cum"] = (p[:64] <= f[:, :64]).astype(np.float32)
    c["m_incl_u"] = np.where(p[:64] <= f[:, :64], 0.0, NEG).astype(np.float32)
    c["m_incl_l"] = np.where(f[:, :64] <= p[:64], 0.0, NEG).astype(np.float32)
    c["m_nstr_u"] = np.where(p[:64] < f[:, :64], -1.0, 0.0).astype(np.float32)
    c["m_nstr_l"] = np.where(f[:, :64] < p[:64], -1.0, 0.0).astype(np.float32)
    c["iota16"] = np.tile(np.arange(16, dtype=np.float32)[None, :], (128, 1))
    return c


CONST_SPECS = {
    "ident_f": ([128, 128], F32), "ident_b": ([128, 128], BF16), "anti_f": ([128, 128], F32),
    "ohp": ([33, 16384], F32), "slc_add": ([SEQ, 64], F32), "e_blk": ([64, SEQ], BF16),
    "w4t": ([128, 128], BF16), "overlap": ([255, 64], F32),
    "m_cum": ([64, 64], F32), "m_incl_u": ([64, 64], F32), "m_incl_l": ([64, 64], F32),
    "m_nstr_u": ([64, 64], F32), "m_nstr_l": ([64, 64], F32), "iota16": ([128, 16], F32),
}

INPUT_SPECS = {
    "x": [SEQ, DM], "c": [DM], "rel_bias": [32, 8], "final_g": [1, DM], "w_ada": [DM, 6 * DM], "b_ada": [1, 6 * DM],
    "norm1_g": [1, DM], "w_in": [DM, IN_COLS], "cmp_pos_emb": [32, 64], "w_cmp_k1": [2048, 64], "w_cmp_k2": [64, 64],
    "w_cmp_v1": [2048, 64], "w_cmp_v2": [64, 64], "dn_conv_w": [4, 1536], "dn_A_log": [1, 8], "dn_dt_bias": [1, 8],
    "dn_norm_g": [1, 64], "w_branch_nsa": [512, DM], "w_branch_dn": [512, DM], "w_out": [DM, DM], "norm2_g": [1, DM],
    "peer_w_query": [DM, DM], "peer_keys1": [8, 128, 64], "peer_keys2": [8, 128, 64], "peer_u": [16384, DM], "peer_v": [16384, DM],
}


GDN_CFG = {'solve_bf16': False, 'prod_bf16': True, 'scan_bf16': True}


class K:
    pass


def build_program(stop_after=None, debug=(), sub=None, skip=()):
    nc = bass.Bass("TRN2", target_bir_lowering=False)
    S = Sched(nc)
    k = K()
    k.nc, k.S = nc, S
    k.sub = sub
    k.gdn_cfg = dict(GDN_CFG)
    class _Lazy(dict):
        def __init__(self, specs, isconst):
            self.specs, self.isconst = specs, isconst
        def __missing__(self, n):
            if self.isconst:
                shp, dt = self.specs[n]
            else:
                shp, dt = self.specs[n], F32
            t = nc.dram_tensor(n, shp, dt, kind="ExternalInput")
            S.res[t.name] = Res(t.name)
            self[n] = t
            return t
    k.inp = _Lazy(INPUT_SPECS, False)
    k.cst = _Lazy(CONST_SPECS, True)
    k.out = nc.dram_tensor("out", [SEQ, DM], F32, kind="ExternalOutput")
    S.res[k.out.name] = Res(k.out.name)
    dbg = set(debug)

    def scratch(name, shape, dt):
        return S.dram(name, shape, dt, kind=("ExternalOutput" if name in dbg else "Internal"))

    k.d = {}
    for name, shape, dt in [
        ("qT_d", [8, 64, SEQ], BF16), ("kcr_d", [2, 64, SEQ], F32), ("vcr_d", [2, 64, SEQ], F32),
        ("ksT_d", [2, 64, SEQ], BF16), ("kwT_d", [2, 64, SEQ], BF16), ("vsw_d", [SEQ, 256], BF16),
        ("gts_d", [SEQ, 24], F32), ("dnqkv_d", [SEQ, 1536], F32), ("z_d", [SEQ, 512], F32), ("ba_d", [SEQ, 16], F32),
        ("gnT_d", [DM, SEQ], BF16), ("gdT_d", [DM, SEQ], BF16), ("onsaT_d", [512, SEQ], BF16), ("odnT_d", [512, SEQ], BF16),
        ("x1_d", [SEQ, DM], F32), ("h2_d", [SEQ, DM], F32), ("tab_d", [8, 16384], F32),
        ("onsa_d", [SEQ, 512], F32), ("odn_d", [SEQ, 512], F32), ("uv_d", [16384, 2 * DM], BF16),
    ]:
        k.d[name] = scratch(name, shape, dt)

    k.ps = [S.psum(f"ps{i}", [128, 512], F32) for i in range(6)]
    k.pb = [S.psum(f"pb{i}", [128, 1024], BF16) for i in range(2)]
    k.ident_f = S.sbuf("ident_f_s", [128, 128], F32)
    k.ident_b = S.sbuf("ident_b_s", [128, 128], BF16)
    k.modb = S.sbuf("modb", [128, 6 * DM], F32)
    S.dma(out=k.ident_f[:], in_=k.cst["ident_f"].ap())
    S.dma(out=k.ident_b[:], in_=k.cst["ident_b"].ap())
    k.cnt = 0

    phases = [phase_mod, phase_norm_proj, phase_nsa, phase_gdn, phase_merge, phase_peer]
    last = None
    for ph in phases:
        if ph.__name__ in skip:
            continue
        last = ph(k)
        if stop_after == ph.__name__:
            break
    outs = list(last) if last else []
    S.barrier()
    S.final_wait(outs)
    S.emit()
    S.close()
    nc.used_inputs = list(k.inp.keys()) + list(k.cst.keys())
    return nc


def alt(k):
    k.cnt += 1
    return k.cnt


def evac(k, out, in_, scale=None, eng=None):
    S = k.S
    e = eng or ("act" if alt(k) % 2 else "dve")
    if e == "act":
        if scale is None:
            return S.I("act", "activation", out=out, in_=in_, func=AF.Copy)
        return S.I("act", "activation", out=out, in_=in_, func=AF.Copy, scale=float(scale))
    if scale is None:
        return S.I("dve", "tensor_copy", out=out, in_=in_)
    return S.I("dve", "tensor_scalar", out=out, in0=in_, scalar1=float(scale), scalar2=None, op0=ALU.mult)


def phase_mod(k):
    S, nc = k.S, k.nc
    S.push()
    ct = S.sbuf("ct", [128, 8], F32)
    cs = S.sbuf("cs", [128, 8], F32)
    cb = S.sbuf("cb", [128, 8, 128], F32)
    ones = S.sbuf("ones1", [1, 128], F32)
    brow = S.sbuf("brow", [1, 6 * DM], F32)
    wt = [S.sbuf(f"wt{i}", [128, 8, 512], F32) for i in range(2)]
    gb = S.sbuf("gb", [128, DM], F32)
    S.dma(out=ct[:], in_=k.inp["c"].ap().rearrange("(kc k) -> k kc", k=128), allow_slow_non_contiguous=True)
    S.dma(out=brow[:], in_=k.inp["b_ada"].ap())
    S.I("pool", "memset", ones[:], 1.0, extra_writes=[ones[:]])
    S.I("act", "activation", out=cs[:], in_=ct[:], func=AF.Silu)
    S.I("dve", "tensor_copy", out=cb[:], in_=cs[:].unsqueeze(2).to_broadcast([128, 8, 128]))
    wv = k.inp["w_ada"].ap().rearrange("(kc k) n -> k kc n", k=128)
    for n in range(12):
        w = wt[n % 2]
        S.dma(out=w[:], in_=wv[:, :, n * 512:(n + 1) * 512])
        p = k.ps[n % 2]
        for kc in range(8):
            S.I("pe", "matmul", out=p[:], lhsT=cb[:, kc, :], rhs=w[:, kc, :], start=(kc == 0), stop=False)
        S.I("pe", "matmul", out=p[:], lhsT=ones[:], rhs=brow[:, n * 512:(n + 1) * 512], start=False, stop=True)
        evac(k, k.modb[:, n * 512:(n + 1) * 512], p[:])
    for (gname, off) in (("norm1_g", 1 * DM), ("norm2_g", 4 * DM)):
        S.dma(out=gb[:], in_=k.inp[gname].ap().partition_broadcast(128))
        S.I("dve", "scalar_tensor_tensor", out=k.modb[:, off:off + DM], in0=k.modb[:, off:off + DM], scalar=1.0, in1=gb[:],
            op0=ALU.add, op1=ALU.mult)
    S.pop()
    return []


def rms_mod_tile(k, xt, A, sh, out_ap, tmp, ss, junk):
    S = k.S
    S.I("act", "activation", out=junk, in_=xt, func=AF.Square, accum_out=ss[:, 0:1])
    S.I("dve", "tensor_scalar", out=ss[:, 1:2], in0=ss[:, 0:1], scalar1=1.0 / DM, scalar2=EPS, op0=ALU.mult, op1=ALU.add)
    S.I("act", "activation", out=ss[:, 2:3], in_=ss[:, 1:2], func=AF.Sqrt)
    S.I("dve", "reciprocal", out=ss[:, 3:4], in_=ss[:, 2:3])
    S.I("dve", "scalar_tensor_tensor", out=tmp, in0=xt, scalar=ss[:, 3:4], in1=A, op0=ALU.mult, op1=ALU.mult)
    S.I("pool", "tensor_tensor", out=out_ap, in0=tmp, in1=sh, op=ALU.add)


def phase_norm_proj(k):
    S, nc = k.S, k.nc
    S.push()
    hT = S.sbuf("hT", [128, 8, SEQ], BF16)
    S.push()
    xt = [S.sbuf(f"xt{i}", [128, DM], F32) for i in range(2)]
    tmp = [S.sbuf(f"tmpn{i}", [128, DM], F32) for i in range(2)]
    junk = S.sbuf("junkn", [128, DM], F32)
    hb = [S.sbuf(f"hb{i}", [128, DM], BF16) for i in range(2)]
    ss = [S.sbuf(f"ss{i}", [128, 4], F32) for i in range(2)]
    for i in range(NT):
        x = xt[i % 2]
        S.dma(out=x[:], in_=k.inp["x"].ap()[i * 128:(i + 1) * 128, :])
        rms_mod_tile(k, x[:], k.modb[:, DM:2 * DM], k.modb[:, 0:DM], hb[i % 2][:], tmp[i % 2][:], ss[i % 2], junk[:])
        pt = k.pb[i % 2]
        ptv = pt[:].rearrange("p (a b) -> p a b", a=8)
        for kc in range(8):
            S.I("pe", "transpose", out=ptv[:, kc, :], in_=hb[i % 2][:, kc * 128:(kc + 1) * 128], identity=k.ident_b[:])
        evac(k, hT[:, :, i * 128:(i + 1) * 128], ptv)
    S.pop()
    if k.sub == 'p1':
        S.pop()
        return []
    w_in = k.inp["w_in"].ap().rearrange("(kc k) n -> k kc n", k=128)
    S.push()
    wst = [S.sbuf(f"wst{i}", [128, 8, 128], F32) for i in range(2)]
    wtokA = S.sbuf("wtokA", [128, 8, 296], BF16)
    wtokZ = S.sbuf("wtokZ", [128, 8, 512], BF16)
    vstg = S.sbuf("vstg", [128, NT, 256], BF16)
    gstg = S.sbuf("gstg", [128, NT, 24], F32)
    bastg = S.sbuf("bastg", [128, NT, 16], F32)
    gbstg = S.sbuf("gbstg", [128, NT, 40], F32)
    zstg = [S.sbuf(f"zstg{i}", [128, 512], F32) for i in range(2)]
    pieces = [(C_VS, 128, wtokA, 0), (C_VW, 128, wtokA, 128), (C_GT, 24, wtokA, 256), (C_DB, 16, wtokA, 280)] + \
             [(C_DZ + 128 * q, 128, wtokZ, 128 * q) for q in range(4)]
    for n, (c0, w, dst, off) in enumerate(pieces):
        st = wst[n % 2]
        S.dma(out=st[:, :, 0:w], in_=w_in[:, :, c0:c0 + w])
        S.I("pool", "tensor_copy", out=dst[:, :, off:off + w], in_=st[:, :, 0:w])
    for i in range(NT):
        pa, pz = k.ps[(2 * i) % 4], k.ps[(2 * i + 1) % 4]
        for kc in range(8):
            S.I("pe", "matmul", out=pa[:, 0:296], lhsT=hT[:, kc, i * 128:(i + 1) * 128], rhs=wtokA[:, kc, :], start=(kc == 0), stop=(kc == 7))
        for kc in range(8):
            S.I("pe", "matmul", out=pz[:], lhsT=hT[:, kc, i * 128:(i + 1) * 128], rhs=wtokZ[:, kc, :], start=(kc == 0), stop=(kc == 7))
        S.I("dve", "tensor_copy", out=vstg[:, i, :], in_=pa[:, 0:256])
        S.I("dve", "tensor_copy", out=gbstg[:, i, :], in_=pa[:, 256:296])
        z = zstg[i % 2]
        S.I("act", "activation", out=z[:], in_=pz[:], func=AF.Copy)
        S.dma(out=k.d["z_d"].ap()[i * 128:(i + 1) * 128, :], in_=z[:])
    S.I("act", "activation", out=gstg[:], in_=gbstg[:, :, 0:24], func=AF.Sigmoid)
    S.I("dve", "tensor_copy", out=bastg[:], in_=gbstg[:, :, 24:40])
    S.dma(out=k.d["vsw_d"].ap().rearrange("(i p) c -> p i c", p=128), in_=vstg[:])
    S.dma(out=k.d["gts_d"].ap().rearrange("(i p) c -> p i c", p=128), in_=gstg[:])
    S.dma(out=k.d["ba_d"].ap().rearrange("(i p) c -> p i c", p=128), in_=bastg[:])
    S.pop()
    if k.sub and k.sub.startswith('p2a'):
        S.pop()
        return []
    S.push()
    wst = [S.sbuf(f"wstb{i}", [128, 8, 128], F32) for i in range(2)]
    wbf = [S.sbuf(f"wbf{i}", [128, 8, 128], BF16) for i in range(2)]
    stg_f = S.sbuf("stg_f", [128, SEQ + 3], F32)
    stg_b = [S.sbuf(f"stg_b{i}", [128, SEQ], BF16) for i in range(2)]
    acc = S.sbuf("acc_cv", [128, SEQ], F32)
    tokstg = S.sbuf("tokstg", [128, NT, 128], F32)
    cw = S.sbuf("cw", [128, 4, 12], F32)
    for j in range(4):
        S.dma(out=cw[:, j, :], in_=k.inp["dn_conv_w"].ap()[j, :].rearrange("(g c) -> c g", c=128), allow_slow_non_contiguous=True)
    S.I("pool", "memset", stg_f[:, 0:3], 0.0, extra_writes=[stg_f[:]])
    groups = []
    for q in range(4):
        groups.append((C_Q + 128 * q, "q", k.d["qT_d"].ap().rearrange("h d t -> (h d) t")[128 * q:128 * (q + 1), :]))
    groups.append((C_KC, "f32", k.d["kcr_d"].ap().rearrange("h d t -> (h d) t")))
    groups.append((C_VC, "f32", k.d["vcr_d"].ap().rearrange("h d t -> (h d) t")))
    groups.append((C_KS, "bf", k.d["ksT_d"].ap().rearrange("h d t -> (h d) t")))
    groups.append((C_KW, "bf", k.d["kwT_d"].ap().rearrange("h d t -> (h d) t")))
    for q in range(8):
        groups.append((C_GN + 128 * q, "sig", k.d["gnT_d"].ap()[128 * q:128 * (q + 1), :]))
    for q in range(8):
        groups.append((C_GD + 128 * q, "sig", k.d["gdT_d"].ap()[128 * q:128 * (q + 1), :]))
    for q in range(12):
        groups.append((C_DQ + 128 * q, "dn", q))
    nb = 0
    for g, (c0, kind, dest) in enumerate(groups):
        st, wb = wst[g % 2], wbf[g % 2]
        S.dma(out=st[:], in_=w_in[:, :, c0:c0 + 128])
        S.I("pool", "tensor_copy", out=wb[:], in_=st[:])
        if kind in ("q", "bf", "sig"):
            sb = stg_b[nb % 2]
            nb += 1
        for G in range(8):
            p = k.ps[(g * 8 + G) % 4]
            for kc in range(8):
                S.I("pe", "matmul", out=p[:], lhsT=wb[:, kc, :], rhs=hT[:, kc, G * 512:(G + 1) * 512], start=(kc == 0), stop=(kc == 7))
            if kind == "q":
                evac(k, sb[:, G * 512:(G + 1) * 512], p[:], scale=0.125)
            elif kind == "bf":
                evac(k, sb[:, G * 512:(G + 1) * 512], p[:])
            elif kind == "sig":
                S.I("act", "activation", out=sb[:, G * 512:(G + 1) * 512], in_=p[:], func=AF.Sigmoid)
            else:
                evac(k, stg_f[:, 3 + G * 512:3 + (G + 1) * 512], p[:])
        if kind in ("q", "bf", "sig"):
            S.dma(out=dest, in_=sb[:])
        elif kind == "f32":
            S.dma(out=dest, in_=stg_f[:, 3:3 + SEQ])
        else:
            q = dest
            S.I("dve", "tensor_scalar", out=acc[:], in0=stg_f[:, 0:SEQ], scalar1=cw[:, 0, q:q + 1], scalar2=None, op0=ALU.mult)
            for j in range(1, 4):
                S.I("dve", "scalar_tensor_tensor", out=acc[:], in0=stg_f[:, j:j + SEQ], scalar=cw[:, j, q:q + 1], in1=acc[:],
                    op0=ALU.mult, op1=ALU.add)
            S.I("act", "activation", out=acc[:], in_=acc[:], func=AF.Silu)
            for i4 in range(8):
                p = k.ps[i4 % 4]
                pv = p[:].rearrange("p (a b) -> p a b", a=4)
                for qq in range(4):
                    i = 4 * i4 + qq
                    S.I("pe", "transpose", out=pv[:, qq, :], in_=acc[:, i * 128:(i + 1) * 128], identity=k.ident_f[:])
                evac(k, tokstg[:, 4 * i4:4 * i4 + 4, :], pv)
            S.dma(out=k.d["dnqkv_d"].ap().rearrange("(i p) c -> p i c", p=128)[:, :, 128 * q:128 * (q + 1)], in_=tokstg[:])
    S.pop()
    S.pop()
    return []


def phase_nsa(k):
    S, nc = k.S, k.nc
    from concourse.ap import AP as RawAP
    S.push()
    tab_d = k.d["tab_d"]
    S.push()
    rb33 = S.sbuf("rb33", [33, 8], F32)
    S.I("pool", "memset", rb33[:], NEG, extra_writes=[rb33[:]])
    S.dma(out=rb33[0:32, :], in_=k.inp["rel_bias"].ap())
    ohs = [S.sbuf(f"ohs{i}", [33, 2048], F32) for i in range(2)]
    tbs = [S.sbuf(f"tbs{i}", [8, 2048], F32) for i in range(2)]
    for c8 in range(8):
        oh, tb = ohs[c8 % 2], tbs[c8 % 2]
        S.dma(out=oh[:], in_=k.cst["ohp"].ap()[:, c8 * 2048:(c8 + 1) * 2048])
        for q in range(4):
            p = k.ps[q]
            S.I("pe", "matmul", out=p[0:8, :], lhsT=rb33[:], rhs=oh[:, q * 512:(q + 1) * 512], start=True, stop=True)
            evac(k, tb[:, q * 512:(q + 1) * 512], p[0:8, :])
        S.dma(out=tab_d.ap()[:, c8 * 2048:(c8 + 1) * 2048], in_=tb[:])
    S.pop()
    anti = S.sbuf("anti", [128, 128], F32)
    S.dma(out=anti[:], in_=k.cst["anti_f"].ap())
    strips = S.sbuf("strips", [128, 8, 640], BF16)
    S.I("pool", "memset", strips[:], 0.0, extra_writes=[strips[:]])
    hank = [S.sbuf(f"hank{i}", [128, 512], F32) for i in range(2)]
    for h in range(8):
        S.dma(out=strips[:, h, 512:640], in_=k.cst["w4t"].ap())
        hk = hank[h % 2]
        S.dma(out=hk[:, 0:256], in_=RawAP(tab_d, h * 16384 + 8192 - 127, [[1, 128], [1, 256]]))
        p = k.ps[h % 4]
        S.I("pe", "matmul", out=p[:, 0:256], lhsT=anti[:], rhs=hk[:, 0:256], start=True, stop=True)
        evac(k, strips[:, h, 0:256], p[:, 0:256])
    kcT = S.sbuf("kcT", [64, 2, 256], BF16)
    vcaug = S.sbuf("vcaug", [128, 2, 2, 129], F32)
    S.I("pool", "memset", vcaug[:], 0.0, extra_writes=[vcaug[:]])
    S.I("pool", "memset", vcaug[:, :, :, 64:65], 1.0, extra_writes=[vcaug[:]])
    for kv in range(2):
        for a in range(2):
            rows = 128 if a == 0 else 127
            S.dma(out=vcaug[0:rows, kv, a, 65:129], in_=k.cst["overlap"].ap()[a * 128:a * 128 + rows, :])
    S.push()
    w1 = S.sbuf("w1c", [64, 32, 64], F32)
    w2 = S.sbuf("w2c", [64, 64], F32)
    posT = S.sbuf("posT", [64, 32], F32)
    cbias = S.sbuf("cbias", [64, 1], F32)
    kcr = [S.sbuf(f"kcr{i}", [64, SEQ], F32) for i in range(2)]
    xh = S.sbuf("xh", [64, 256], F32)
    t1 = S.sbuf("t1c", [64, 256], F32)
    t2 = S.sbuf("t2c", [64, 256], F32)
    gh = S.sbuf("ghc", [64, 256], F32)
    S.dma(out=posT[:], in_=k.inp["cmp_pos_emb"].ap().rearrange("l d -> d l"), allow_slow_non_contiguous=True)
    n = 0
    for which in ("k", "v"):
        S.dma(out=w1[:], in_=k.inp["w_cmp_%s1" % which].ap().rearrange("(l d) h -> d l h", d=64))
        S.dma(out=w2[:], in_=k.inp["w_cmp_%s2" % which].ap())
        pc = k.ps[0]
        for l in range(32):
            S.I("pe", "matmul", out=pc[0:64, 0:1], lhsT=w1[:, l, :], rhs=posT[:, l:l + 1], start=(l == 0), stop=(l == 31))
        S.I("dve", "tensor_copy", out=cbias[:], in_=pc[0:64, 0:1])
        for kv in range(2):
            kc = kcr[n % 2]
            n += 1
            S.dma(out=kc[:], in_=k.d["kcr_d" if which == "k" else "vcr_d"].ap()[kv])
            kcv = kc[:].rearrange("p (n s) -> p n s", s=16)
            ph = k.ps[1 + (n % 2)]
            for l in range(32):
                rhs = kcv[:, 0:255, l] if l < 16 else kcv[:, 1:256, l - 16]
                S.I("pe", "matmul", out=ph[0:64, 0:255], lhsT=w1[:, l, :], rhs=rhs, start=(l == 0), stop=(l == 31))
            S.I("act", "activation", out=xh[:, 0:255], in_=ph[0:64, 0:255], func=AF.Identity, bias=cbias[:, 0:1])
            S.I("dve", "tensor_tensor", out=t1[:, 0:255], in0=xh[:, 0:255], in1=xh[:, 0:255], op=ALU.mult)
            S.I("dve", "tensor_scalar", out=t1[:, 0:255], in0=t1[:, 0:255], scalar1=0.044715, scalar2=1.0, op0=ALU.mult, op1=ALU.add)
            S.I("dve", "tensor_tensor", out=t2[:, 0:255], in0=t1[:, 0:255], in1=xh[:, 0:255], op=ALU.mult)
            S.I("act", "activation", out=t1[:, 0:255], in_=t2[:, 0:255], func=AF.Sigmoid, scale=GELU_C)
            S.I("dve", "tensor_tensor", out=gh[:, 0:255], in0=t1[:, 0:255], in1=xh[:, 0:255], op=ALU.mult)
            if which == "k":
                pk = k.ps[3]
                S.I("pe", "matmul", out=pk[0:64, 0:255], lhsT=w2[:], rhs=gh[:, 0:255], start=True, stop=True)
                S.I("dve", "tensor_copy", out=kcT[:, kv, 0:255], in_=pk[0:64, 0:255])
            else:
                for a in range(2):
                    rows = 128 if a == 0 else 127
                    pv = k.ps[4 + a]
                    S.I("pe", "matmul", out=pv[0:rows, 0:64], lhsT=gh[:, a * 128:a * 128 + rows], rhs=w2[:], start=True, stop=True)
                    S.I("dve", "tensor_copy", out=vcaug[0:rows, kv, a, 0:64], in_=pv[0:rows, 0:64])
    S.pop()
    if k.sub == "n1":
        S.pop()
        return []
    gates = S.sbuf("gatesb", [128, NT, 24], F32)
    S.dma(out=gates[:], in_=k.d["gts_d"].ap().rearrange("(i p) c -> p i c", p=128))
    slc = S.sbuf("slcadd", [128, NT, 64], F32)
    S.dma(out=slc[:], in_=k.cst["slc_add"].ap().rearrange("(i p) c -> p i c", p=128))
    o_acc = S.sbuf("o_acc", [128, NT, 256], F32)
    imp = S.sbuf("imp", [128, NT, 64], F32)
    qaug = [S.sbuf(f"qaug{g}", [128, SEQ], BF16) for g in range(4)]
    ksaug = S.sbuf("ksaug", [128, SEQ], BF16)
    kwT = S.sbuf("kwT", [64, SEQ], BF16)
    vsa = S.sbuf("vsa", [128, NT, 65], BF16)
    vwa = S.sbuf("vwa", [128, NT, 65], BF16)
    pTb = [S.sbuf(f"pTb{i}", [128, 512], BF16) for i in range(3)]
    pTc4 = [S.sbuf(f"pTc{i}", [128, 512], F32) for i in range(4)]
    hank4 = hank + [S.sbuf(f"hankx{i}", [128, 512], F32) for i in range(2)]
    ftb4 = [S.sbuf(f"ftb{i}", [128, 512], BF16) for i in range(4)]
    selb = [S.sbuf(f"selb{i}", [128, 128], BF16) for i in range(2)]
    sc = [S.sbuf(f"scr{i}", [128, 64], F32) for i in range(2)]
    sc2 = [S.sbuf(f"scr2{i}", [128, 64], F32) for i in range(2)]
    m8 = [S.sbuf(f"m8{i}", [128, 16], F32) for i in range(2)]
    rz = [S.sbuf(f"rz{i}", [128, 8], F32) for i in range(2)]
    tmpo = [S.sbuf(f"tmpo{i}", [128, 4, 64], F32) for i in range(2)]
    ob = [S.sbuf(f"ob{i}", [128, 256], BF16) for i in range(2)]
    oT = S.sbuf("oTn", [128, 2, SEQ], BF16)
    for i in range(2):
        S.I("pool", "memset", selb[i][:], 0.0, extra_writes=[selb[i][:]])
    S.dma(out=ksaug[64:128, :], in_=k.cst["e_blk"].ap())
    S.I("pool", "memset", vsa[:, :, 64:65], 1.0, extra_writes=[vsa[:]])
    S.I("pool", "memset", vwa[:, :, 64:65], 1.0, extra_writes=[vwa[:]])
    vsw_v = k.d["vsw_d"].ap().rearrange("(i p) c -> p i c", p=128)
    cnt = [0]

    def finish_branch(h, g, G, po_views, zcol, br, first):
        r = rz[cnt[0] % 2]
        tm = tmpo[cnt[0] % 2]
        cnt[0] += 1
        for c in range(4):
            S.I("dve", "tensor_scalar", out=r[:, c:c + 1], in0=po_views[c][:, zcol:zcol + 1], scalar1=1e-30, scalar2=None, op0=ALU.max)
        S.I("dve", "reciprocal", out=r[:, 0:4], in_=r[:, 0:4])
        S.I("dve", "tensor_tensor", out=r[:, 4:8], in0=r[:, 0:4], in1=gates[:, 4 * G:4 * G + 4, h * 3 + br], op=ALU.mult)
        for c in range(4):
            dst = o_acc[:, 4 * G + c, g * 64:(g + 1) * 64]
            if first:
                S.I("dve", "tensor_scalar", out=dst, in0=po_views[c][:, 0:64], scalar1=r[:, 4 + c:5 + c], scalar2=None, op0=ALU.mult)
            else:
                S.I("dve", "scalar_tensor_tensor", out=dst, in0=po_views[c][:, 0:64], scalar=r[:, 4 + c:5 + c], in1=dst,
                    op0=ALU.mult, op1=ALU.add)
        return r

    nps = [0]

    def next_ps():
        nps[0] += 1
        return k.ps[nps[0] % 4]

    for kv in range(2):
        for g in range(4):
            S.dma(out=qaug[g][0:64, :], in_=k.d["qT_d"].ap()[kv * 4 + g])
        S.dma(out=ksaug[0:64, :], in_=k.d["ksT_d"].ap()[kv])
        S.dma(out=kwT[:], in_=k.d["kwT_d"].ap()[kv])
        S.dma(out=vsa[:, :, 0:64], in_=vsw_v[:, :, kv * 64:kv * 64 + 64])
        S.dma(out=vwa[:, :, 0:64], in_=vsw_v[:, :, 128 + kv * 64:128 + kv * 64 + 64])
        def cmp_front(g, G, rnd):
            h = kv * 4 + g
            a_list = [0] if G < 4 else [0, 1]
            tiles = []
            for a in a_list:
                rows = 128 if a == 0 else 127
                need_corr = (a == 0 and G <= 4) or (a == 1)
                pss = next_ps()
                S.I("pe", "matmul", out=pss[0:rows, :], lhsT=kcT[:, kv, a * 128:a * 128 + rows], rhs=qaug[g][0:64, G * 512:(G + 1) * 512],
                    start=True, stop=not need_corr)
                if need_corr:
                    hk = hank4[cnt[0] % 4]
                    ft = ftb4[cnt[0] % 4]
                    cnt[0] += 1
                    S.dma(out=hk[:], in_=RawAP(tab_d, h * 16384 + 8192 + 512 * G - 2048 * a - 2063, [[16, 128], [1, 512]]))
                    pj = next_ps()
                    S.I("pe", "matmul", out=pj[:], lhsT=anti[:], rhs=hk[:], start=True, stop=True)
                    evac(k, ft[:], pj[:], eng="dve")
                    S.I("pe", "matmul", out=pss[0:rows, :], lhsT=k.ident_b[0:rows, 0:rows], rhs=ft[0:rows, :], start=False, stop=True)
                pt_ = pTc4[(rnd % 2) * 2 + a]
                S.I("act", "activation", out=pt_[0:rows, :], in_=pss[0:rows, :], func=AF.Exp)
                tiles.append((a, rows, pt_))
            return (g, G, h, tiles)

        def cmp_back(ctx):
            g, G, h, tiles = ctx
            pA, pB = k.ps[4], k.ps[5]
            views = []
            for c in range(4):
                pv = (pA if c < 2 else pB)[:, (c % 2) * 256:(c % 2) * 256 + 129]
                views.append(pv)
                for ti, (a, rows, pt_) in enumerate(tiles):
                    S.I("pe", "matmul", out=pv, lhsT=pt_[0:rows, c * 128:(c + 1) * 128], rhs=vcaug[0:rows, kv, a, :],
                        start=(ti == 0), stop=(ti == len(tiles) - 1))
            r = finish_branch(h, g, G, views, 64, 0, True)
            for c in range(4):
                dst = imp[:, 4 * G + c, :]
                if g == 0:
                    S.I("dve", "tensor_scalar", out=dst, in0=views[c][:, 65:129], scalar1=r[:, c:c + 1], scalar2=None, op0=ALU.mult)
                else:
                    S.I("dve", "scalar_tensor_tensor", out=dst, in0=views[c][:, 65:129], scalar=r[:, c:c + 1], in1=dst,
                        op0=ALU.mult, op1=ALU.add)

        rounds = [(g, G) for g in range(4) for G in range(8)]
        prev = cmp_front(rounds[0][0], rounds[0][1], 0)
        for ri in range(1, len(rounds)):
            cur = cmp_front(rounds[ri][0], rounds[ri][1], ri)
            cmp_back(prev)
            prev = cur
        cmp_back(prev)
        for i in range(NT):
            s1, s2, mm, sb = sc[i % 2], sc2[i % 2], m8[i % 2], selb[i % 2]
            S.I("dve", "tensor_tensor", out=s1[:], in0=imp[:, i, :], in1=slc[:, i, :], op=ALU.add)
            S.I("dve", "max", out=mm[:, 0:8], in_=s1[:])
            S.I("dve", "match_replace", out=s2[:], in_to_replace=mm[:, 0:8], in_values=s1[:], imm_value=-3.0e38)
            S.I("dve", "max", out=mm[:, 8:16], in_=s2[:])
            S.I("dve", "tensor_scalar", out=sb[:, 64:128], in0=s1[:], scalar1=mm[:, 15:16], scalar2=NEG, op0=ALU.is_lt, op1=ALU.mult)
            pt = k.pb[i % 2]
            S.I("pe", "transpose", out=pt[:, 0:128], in_=sb[:], identity=k.ident_b[:])
            for g in range(4):
                evac(k, qaug[g][64:128, i * 128:(i + 1) * 128], pt[64:128, 0:128], eng="dve" if i % 2 else "act")
        if k.sub == "n2":
            continue
        for g in range(4):
            h = kv * 4 + g
            for G in range(8):
                po = k.ps[4 + (G % 2)]
                views = [po[:, c * 128:c * 128 + 65] for c in range(4)]

                def qk_sel(P, g=g, h=h, G=G):
                    c_lo = max(0, P - 4 * G)
                    ncol = 4 - c_lo
                    t0 = G * 512 + c_lo * 128
                    pss = next_ps()
                    if P >= 4 * G:
                        S.I("pe", "matmul", out=pss[:, 0:ncol * 128], lhsT=ksaug[:, P * 128:(P + 1) * 128], rhs=qaug[g][:, t0:(G + 1) * 512],
                            start=True, stop=False)
                        S.I("pe", "matmul", out=pss[:, 0:ncol * 128], lhsT=k.ident_b[:], rhs=strips[:, h, 0:ncol * 128], start=False, stop=True)
                    elif P == 4 * G - 1:
                        S.I("pe", "matmul", out=pss[:, 0:128], lhsT=ksaug[:, P * 128:(P + 1) * 128], rhs=qaug[g][:, t0:t0 + 128],
                            start=True, stop=False)
                        S.I("pe", "matmul", out=pss[:, 0:128], lhsT=k.ident_b[:], rhs=strips[:, h, 128:256], start=False, stop=True)
                        S.I("pe", "matmul", out=pss[:, 128:512], lhsT=ksaug[:, P * 128:(P + 1) * 128], rhs=qaug[g][:, t0 + 128:(G + 1) * 512],
                            start=True, stop=True)
                    else:
                        S.I("pe", "matmul", out=pss[:, 0:ncol * 128], lhsT=ksaug[:, P * 128:(P + 1) * 128], rhs=qaug[g][:, t0:(G + 1) * 512],
                            start=True, stop=True)
                    pT = pTb[nps[0] % 3]
                    S.I("act", "activation", out=pT[:, 0:ncol * 128], in_=pss[:, 0:ncol * 128], func=AF.Exp)
                    return (P, c_lo, pT)

                def pv_sel(ctx, G=G, views=views):
                    P, c_lo, pT = ctx
                    for c in range(c_lo, 4):
                        S.I("pe", "matmul", out=views[c], lhsT=pT[:, (c - c_lo) * 128:(c - c_lo + 1) * 128], rhs=vsa[:, P, :],
                            start=(P == 0 and c == 0), stop=(P == 4 * G + c), skip_group_check=True)

                ncv = (kv * 4 + g) * 8 + G
                src_t = k.inp["peer_u" if ncv < 32 else "peer_v"].ap()
                r0 = (ncv % 32) * 512
                S.dma(out=k.d["uv_d"].ap()[r0:r0 + 512, (ncv // 32) * DM:(ncv // 32 + 1) * DM], in_=src_t[r0:r0 + 512, :], q="pool")
                Ps = list(range(0, 4 * G + 4))
                prev = qk_sel(Ps[0])
                for P in Ps[1:]:
                    cur = qk_sel(P)
                    pv_sel(prev)
                    prev = cur
                pv_sel(prev)
                finish_branch(h, g, G, views, 64, 1, False)
        for g in range(4):
            h = kv * 4 + g
            for G in range(8):
                po = k.ps[4 + (G % 2)]
                views = [po[:, c * 128:c * 128 + 65] for c in range(4)]

                def qk_win(P, g=g, h=h, G=G):
                    c_lo = max(0, P - 4 * G)
                    c_hi = min(3, P - 4 * G + 4)
                    ncol = c_hi - c_lo + 1
                    r_lo = 4 * G + c_lo - P
                    t0 = G * 512 + c_lo * 128
                    pss = next_ps()
                    S.I("pe", "matmul", out=pss[:, 0:ncol * 128], lhsT=kwT[:, P * 128:(P + 1) * 128], rhs=qaug[g][0:64, t0:t0 + ncol * 128],
                        start=True, stop=False)
                    S.I("pe", "matmul", out=pss[:, 0:ncol * 128], lhsT=k.ident_b[:], rhs=strips[:, h, r_lo * 128:(r_lo + ncol) * 128], start=False, stop=True)
                    pT = pTb[nps[0] % 3]
                    S.I("act", "activation", out=pT[:, 0:ncol * 128], in_=pss[:, 0:ncol * 128], func=AF.Exp)
                    return (P, c_lo, c_hi, pT)

                def pv_win(ctx, G=G, views=views):
                    P, c_lo, c_hi, pT = ctx
                    for c in range(c_lo, c_hi + 1):
                        S.I("pe", "matmul", out=views[c], lhsT=pT[:, (c - c_lo) * 128:(c - c_lo + 1) * 128], rhs=vwa[:, P, :],
                            start=(P == max(0, 4 * G - 4) and c == 0), stop=(P == 4 * G + c), skip_group_check=True)

                Ps = list(range(max(0, 4 * G - 4), 4 * G + 4))
                prev = qk_win(Ps[0])
                for P in Ps[1:]:
                    cur = qk_win(P)
                    pv_win(prev)
                    prev = cur
                pv_win(prev)
                finish_branch(h, g, G, views, 64, 2, False)
        S.dma(out=k.d["onsa_d"].ap().rearrange("(i p) c -> p i c", p=128)[:, :, kv * 256:(kv + 1) * 256], in_=o_acc[:])
        for i in range(NT):
            o = ob[i % 2]
            S.I("pool", "tensor_copy", out=o[:], in_=o_acc[:, i, :])
            pt = k.pb[i % 2]
            for j in range(2):
                S.I("pe", "transpose", out=pt[:, j * 128:(j + 1) * 128], in_=o[:, j * 128:(j + 1) * 128], identity=k.ident_b[:])
            evac(k, oT[:, :, i * 128:(i + 1) * 128], pt[:, 0:256].rearrange("p (a b) -> p a b", a=2))
        S.dma(out=k.d["onsaT_d"].ap().rearrange("(j p) t -> p j t", p=128)[:, 2 * kv:2 * kv + 2, :], in_=oT[:])
    S.pop()
    return []


def phase_gdn(k):
    S, nc = k.S, k.nc
    S.push()
    gdn_d = S.dram("gdn_d", [SEQ, 2056], F32)

    def v3(ap, a):
        return ap.rearrange("p (a b) -> p a b", a=a)

    def bc2(ap, n, w):
        return ap.unsqueeze(2).to_broadcast([n, ap.shape[1], w])

    def bc1(ap, n, a):
        return ap.unsqueeze(1).to_broadcast([n, a, ap.shape[1]])

    S.push()
    dtb = S.sbuf("dtb", [128, 8], F32)
    negA = S.sbuf("negA", [128, 8], F32)
    S.dma(out=dtb[:], in_=k.inp["dn_dt_bias"].ap().partition_broadcast(128))
    S.dma(out=negA[:], in_=k.inp["dn_A_log"].ap().partition_broadcast(128))
    S.I("act", "activation", out=negA[:], in_=negA[:], func=AF.Exp)
    S.I("dve", "tensor_scalar", out=negA[:], in0=negA[:], scalar1=-1.0, scalar2=None, op0=ALU.mult)
    xin = [S.sbuf(f"gxin{i}", [128, 1536], F32) for i in range(2)]
    bain = [S.sbuf(f"gbain{i}", [128, 16], F32) for i in range(2)]
    xout = [S.sbuf(f"gxout{i}", [128, 2056], F32) for i in range(2)]
    sqt = S.sbuf("gsq", [128, 1024], F32)
    sm = [S.sbuf(f"gsm{i}", [128, 48], F32) for i in range(2)]
    for i in range(NT):
        xq, ba, Xo, s_ = xin[i % 2], bain[i % 2], xout[i % 2], sm[i % 2]
        S.dma(out=xq[:], in_=k.d["dnqkv_d"].ap()[i * 128:(i + 1) * 128, :])
        S.dma(out=ba[:], in_=k.d["ba_d"].ap()[i * 128:(i + 1) * 128, :])
        S.I("act", "activation", out=sqt[:], in_=xq[:, 0:1024], func=AF.Square)
        S.I("dve", "tensor_reduce", out=s_[:, 0:16], in_=v3(sqt[:], 16), axis=AX.X, op=ALU.add)
        S.I("dve", "tensor_scalar", out=s_[:, 0:16], in0=s_[:, 0:16], scalar1=EPS, scalar2=None, op0=ALU.add)
        S.I("act", "activation", out=s_[:, 0:16], in_=s_[:, 0:16], func=AF.Sqrt)
        S.I("dve", "reciprocal", out=s_[:, 16:32], in_=s_[:, 0:16])
        S.I("dve", "tensor_scalar", out=s_[:, 16:24], in0=s_[:, 16:24], scalar1=0.125, scalar2=None, op0=ALU.mult)
        S.I("dve", "tensor_tensor", out=v3(Xo[:, 0:1024], 16), in0=v3(xq[:, 0:1024], 16), in1=bc2(s_[:, 16:32], 128, 64), op=ALU.mult)
        S.I("act", "activation", out=s_[:, 32:40], in_=ba[:, 0:8], func=AF.Sigmoid)
        S.I("dve", "tensor_tensor", out=s_[:, 40:48], in0=ba[:, 8:16], in1=dtb[:], op=ALU.add)
        S.I("act", "activation", out=s_[:, 40:48], in_=s_[:, 40:48], func=AF.Exp)
        S.I("dve", "tensor_scalar", out=s_[:, 40:48], in0=s_[:, 40:48], scalar1=1.0, scalar2=None, op0=ALU.add)
        S.I("act", "activation", out=s_[:, 40:48], in_=s_[:, 40:48], func=AF.Ln)
        S.I("dve", "tensor_tensor", out=Xo[:, 2048:2056], in0=s_[:, 40:48], in1=negA[:], op=ALU.mult)
        S.I("pool", "tensor_tensor", out=v3(Xo[:, 1024:1536], 8), in0=v3(Xo[:, 512:1024], 8), in1=bc2(s_[:, 32:40], 128, 64), op=ALU.mult)
        S.I("pool", "tensor_tensor", out=v3(Xo[:, 1536:2048], 8), in0=v3(xq[:, 1024:1536], 8), in1=bc2(s_[:, 32:40], 128, 64), op=ALU.mult)
        S.dma(out=gdn_d.ap()[i * 128:(i + 1) * 128, :], in_=Xo[:])
    S.pop()
    C = 64
    NCH = SEQ // C
    cm = {}
    for nm in ("m_cum", "m_incl_u", "m_incl_l", "m_nstr_u", "m_nstr_l"):
        cm[nm] = S.sbuf("c_" + nm, [64, 64], F32)
        S.dma(out=cm[nm][:], in_=k.cst[nm].ap())
    ones64 = S.sbuf("ones64", [64, 64], F32)
    S.I("pool", "memset", ones64[:], 1.0, extra_writes=[ones64[:]])
    gng = S.sbuf("gng", [64, 64], F32)
    S.dma(out=gng[:], in_=k.inp["dn_norm_g"].ap().partition_broadcast(64))
    St = S.sbuf("gstate", [64, 8, 64], F32)
    S.I("pool", "memset", St[:], 0.0, extra_writes=[St[:]])
    idf = k.ident_f[0:64, 0:64]

    DT_SOLVE = BF16 if k.gdn_cfg.get('solve_bf16', True) else F32
    DT_PROD = BF16 if k.gdn_cfg.get('prod_bf16', True) else F32
    DT_SCAN = BF16 if k.gdn_cfg.get('scan_bf16', True) else F32
    NS = 3
    def mk(name, shape, n=2, dt=F32):
        n = {2: NS, 4: 2 * NS, 22: 2}[n]
        return [S.sbuf(f"{name}{i}", shape, dt) for i in range(n)]

    X = mk("gX", [64, 2056])
    rhsU = mk("grhsU", [64, 8, 64])
    D1 = mk("gD1", [64, 8, 64])
    ET = mk("gET", [64, 8, 64])
    EE = mk("gEE", [64, 8, 64])
    ETn = mk("gETn", [64, 8, 64])
    En = EE
    tmpd = rhsU
    kT = mk("gkT", [64, 8, 64], 2, DT_PROD)
    kbT = mk("gkbT", [64, 8, 64], 2, DT_PROD)
    NTa = mk("gNTa", [64, 8, 64], 2, DT_SOLVE)
    NTb = mk("gNTb", [64, 8, 64], 2, DT_SOLVE)
    Na = mk("gNa", [64, 8, 64], 2, DT_SOLVE)
    Nb = mk("gNb", [64, 8, 64], 2, DT_SOLVE)
    qT = mk("gqT", [64, 8, 64], 4, DT_PROD)
    attnT = mk("gattnT", [64, 8, 64], 4, DT_SCAN)
    X6 = mk("gX6", [64, 8, 128], 4, DT_SOLVE)
    qTs = mk("gqTs", [64, 8, 64], 4, DT_SCAN)
    Tmat = mk("gTmat", [64, 8, 64])
    TTs = mk("gTTs", [64, 8, 64])
    R6 = mk("gR6", [64, 8, 128])
    Stb = S.sbuf("gstateb", [64, 8, 64], DT_SCAN)
    S.I("pool", "memset", Stb[:], 0.0, extra_writes=[Stb[:]])
    wT = mk("gwT", [64, 8, 64], 4, DT_SCAN)
    kdec = mk("gkdec", [64, 8, 64], 4, DT_SCAN)
    sm = mk("gsmall", [64, 64], 4)
    Z = mk("gZ", [64, 512], 22)
    vnew = mk("gvnew", [64, 8, 64], 22, DT_SCAN)
    osb = mk("gosb", [64, 8, 64], 22)
    odn = mk("godn", [64, 512], 22)
    pbf = [k.pb[i][:].bitcast(F32) for i in range(2)]

    def mm8(out_ps, lhs, rhs, w=64):
        for h in range(8):
            S.I("pe", "matmul", out=out_ps[0:64, h * w:(h + 1) * w], lhsT=lhs(h), rhs=rhs(h), start=True, stop=True)

    def tr8(out_ps, src):
        for h in range(8):
            S.I("pe", "transpose", out=out_ps[0:64, h * 64:(h + 1) * 64], in_=src(h), identity=idf)

    def pv(ps):
        return v3(ps[0:64, :], 8)

    def pre(n):
        b, L = n % NS, n % (2 * NS)
        banks = [k.ps[2 * b + i] for i in range(2)]
        cnt = [0]

        def nps():
            cnt[0] += 1
            return banks[cnt[0] % 2]

        x = X[b]
        S.dma(out=x[:], in_=gdn_d.ap()[n * C:(n + 1) * C, :])
        qn, kn, kb, vb = (v3(x[:, o:o + 512], 8) for o in (0, 512, 1024, 1536))
        g = x[:, 2048:2056]
        s_ = sm[L]
        S.I("pool", "tensor_tensor", out=rhsU[b][:], in0=bc2(g, 64, 64), in1=bc1(cm["m_cum"][:], 64, 8), op=ALU.mult)
        pG = nps()
        S.I("pe", "matmul", out=pG[0:64, :], lhsT=ones64[:], rhs=rhsU[b][:].rearrange("p a b -> p (a b)"), start=True, stop=True)
        pg = nps()
        S.I("pe", "matmul", out=pg[0:64, 0:8], lhsT=cm["m_cum"][:], rhs=g, start=True, stop=True)
        yield
        S.I("dve", "tensor_copy", out=s_[:, 0:8], in_=pg[0:64, 0:8])
        S.I("act", "activation", out=s_[:, 8:16], in_=s_[:, 0:8], func=AF.Exp)
        S.I("dve", "tensor_tensor", out=D1[b][:], in0=pv(pG), in1=bc2(s_[:, 0:8], 64, 64), op=ALU.subtract)
        S.I("dve", "tensor_copy", out=s_[:, 16:24], in_=pv(pG)[:, :, 63])
        S.I("act", "activation", out=s_[:, 24:32], in_=s_[:, 16:24], func=AF.Exp)
        S.I("dve", "tensor_tensor", out=s_[:, 32:40], in0=s_[:, 16:24], in1=s_[:, 0:8], op=ALU.subtract)
        S.I("act", "activation", out=s_[:, 32:40], in_=s_[:, 32:40], func=AF.Exp)
        yield
        S.I("pool", "tensor_tensor", out=tmpd[b][:], in0=D1[b][:], in1=bc1(cm["m_incl_u"][:], 64, 8), op=ALU.add)
        S.I("act", "activation", out=ET[b][:], in_=tmpd[b][:], func=AF.Exp)
        S.I("dve", "scalar_tensor_tensor", out=EE[b][:], in0=D1[b][:], scalar=-1.0, in1=bc1(cm["m_incl_l"][:], 64, 8), op0=ALU.mult, op1=ALU.add)
        S.I("act", "activation", out=EE[b][:], in_=EE[b][:], func=AF.Exp)
        S.I("pool", "tensor_tensor", out=ETn[b][:], in0=ET[b][:], in1=bc1(cm["m_nstr_u"][:], 64, 8), op=ALU.mult)
        S.I("pool", "tensor_tensor", out=En[b][:], in0=EE[b][:], in1=bc1(cm["m_nstr_l"][:], 64, 8), op=ALU.mult)
        yield
        for (dst, src) in ((kT[b], kn), (qT[L], qn), (kbT[b], kb)):
            p = nps()
            tr8(p, lambda h, src=src: src[:, h, :])
            if dst is qT[L]:
                S.I("dve", "tensor_copy", out=dst[:], in_=pv(p))
                S.I("dve", "tensor_copy", out=qTs[L][:], in_=pv(p))
            else:
                evac(k, dst[:], pv(p))
            yield
        p = nps()
        mm8(p, lambda h: kT[b][:, h, :], lambda h: kbT[b][:, h, :])
        S.I("dve", "tensor_tensor", out=NTa[b][:], in0=pv(p), in1=ETn[b][:], op=ALU.mult)
        yield
        p = nps()
        mm8(p, lambda h: kbT[b][:, h, :], lambda h: kT[b][:, h, :])
        S.I("dve", "tensor_tensor", out=Na[b][:], in0=pv(p), in1=En[b][:], op=ALU.mult)
        yield
        p = nps()
        mm8(p, lambda h: kT[b][:, h, :], lambda h: qT[L][:, h, :])
        S.I("dve", "tensor_tensor", out=attnT[L][:], in0=pv(p), in1=ET[b][:], op=ALU.mult)
        S.I("pool", "tensor_copy", out=R6[b][:, :, 0:64], in_=vb)
        S.I("pool", "tensor_tensor", out=R6[b][:, :, 64:128], in0=kb, in1=bc2(s_[:, 8:16], 64, 64), op=ALU.mult)
        S.I("pool", "tensor_tensor", out=kdec[L][:], in0=kn, in1=bc2(s_[:, 32:40], 64, 64), op=ALU.mult)
        yield
        NTc, Nc, NTn, Nn = NTa[b], Na[b], NTb[b], Nb[b]
        Tm = Tmat[b]
        S.I("dve", "tensor_tensor", out=Tm[:], in0=Nc[:], in1=bc1(idf, 64, 8), op=ALU.add)
        for lvl in range(1, 6):
            p2 = nps()
            mm8(p2, lambda h: Nc[:, h, :], lambda h: NTc[:, h, :])
            S.I("act", "activation", out=NTn[:], in_=pv(p2), func=AF.Copy)
            yield
            if lvl < 5:
                p1 = nps()
                tr8(p1, lambda h, NTn=NTn: NTn[:, h, :])
                S.I("act", "activation", out=Nn[:], in_=pv(p1), func=AF.Copy)
            NTc, Nc, NTn, Nn = NTn, Nn, NTc, Nc
            pY = nps()
            mm8(pY, lambda h: NTc[:, h, :], lambda h: Tm[:, h, :])
            S.I("dve", "tensor_tensor", out=Tm[:], in0=Tm[:], in1=pv(pY), op=ALU.add)
            yield
        pT_ = nps()
        tr8(pT_, lambda h: Tm[:, h, :])
        S.I("act", "activation", out=TTs[b][:], in_=pv(pT_), func=AF.Copy)
        yield
        pA, pB = nps(), nps()
        for h in range(8):
            pp = pA if h < 4 else pB
            S.I("pe", "matmul", out=pp[0:64, (h % 4) * 128:(h % 4 + 1) * 128], lhsT=TTs[b][:, h, :], rhs=R6[b][:, h, :], start=True, stop=True)
        S.I("dve", "tensor_copy", out=X6[L][:, 0:4, :], in_=v3(pA[0:64, :], 4))
        S.I("dve", "tensor_copy", out=X6[L][:, 4:8, :], in_=v3(pB[0:64, :], 4))
        yield
        p = nps()
        tr8(p, lambda h: X6[L][:, h, 64:128])
        evac(k, wT[L][:], pv(p))
        yield

    def scan(n):
        b, L = n % 2, n % (2 * NS)
        s_ = sm[L]
        S.dma(out=Z[b][:], in_=k.d["z_d"].ap()[n * C:(n + 1) * C, :])
        S.I("act", "activation", out=Z[b][:], in_=Z[b][:], func=AF.Silu)
        p = pbf[0]
        mm8(p, lambda h: wT[L][:, h, :], lambda h: Stb[:, h, :])
        S.I("dve", "tensor_tensor", out=vnew[b][:], in0=X6[L][:, :, 0:64], in1=pv(p), op=ALU.subtract)
        yield
        pq = pbf[1]
        mm8(pq, lambda h: qTs[L][:, h, :], lambda h: Stb[:, h, :])
        S.I("dve", "tensor_tensor", out=osb[b][:], in0=pv(pq), in1=bc2(s_[:, 8:16], 64, 64), op=ALU.mult)
        yield
        pa_ = pbf[0]
        mm8(pa_, lambda h: attnT[L][:, h, :], lambda h: vnew[b][:, h, :])
        S.I("dve", "tensor_tensor", out=osb[b][:], in0=osb[b][:], in1=pv(pa_), op=ALU.add)
        yield
        pk_ = pbf[1]
        mm8(pk_, lambda h: kdec[L][:, h, :], lambda h: vnew[b][:, h, :])
        S.I("dve", "tensor_tensor", out=St[:], in0=St[:], in1=bc2(s_[:, 24:32], 64, 64), op=ALU.mult)
        S.I("dve", "tensor_tensor", out=St[:], in0=St[:], in1=pv(pk_), op=ALU.add)
        S.I("act", "activation", out=Stb[:], in_=St[:], func=AF.Copy)
        yield
        S.I("act", "activation", out=v3(odn[b][:], 8), in_=osb[b][:], func=AF.Square)
        S.I("dve", "tensor_reduce", out=s_[:, 40:48], in_=v3(odn[b][:], 8), axis=AX.X, op=ALU.add)
        S.I("dve", "tensor_scalar", out=s_[:, 40:48], in0=s_[:, 40:48], scalar1=1.0 / 64, scalar2=EPS, op0=ALU.mult, op1=ALU.add)
        S.I("act", "activation", out=s_[:, 40:48], in_=s_[:, 40:48], func=AF.Sqrt)
        S.I("dve", "reciprocal", out=s_[:, 48:56], in_=s_[:, 40:48])
        yield
        S.I("pool", "tensor_tensor", out=osb[b][:], in0=osb[b][:], in1=bc2(s_[:, 48:56], 64, 64), op=ALU.mult)
        S.I("pool", "tensor_tensor", out=osb[b][:], in0=osb[b][:], in1=bc1(gng[:], 64, 8), op=ALU.mult)
        S.I("pool", "tensor_tensor", out=odn[b][:], in0=osb[b][:].rearrange("p a b -> p (a b)"), in1=Z[b][:], op=ALU.mult)
        S.dma(out=k.d["odn_d"].ap()[n * C:(n + 1) * C, :], in_=odn[b][:])
        yield

    def scans(n0):
        for n in range(n0, min(n0 + NS, NCH)):
            for _ in scan(n):
                yield

    def lockstep(gens):
        gens = list(gens)
        while gens:
            for g_ in list(gens):
                try:
                    next(g_)
                except StopIteration:
                    gens.remove(g_)

    lockstep([pre(i) for i in range(NS)])
    for p_ in range((NCH + NS - 1) // NS):
        gens = [pre(n) for n in range(NS * p_ + NS, min(NS * p_ + 2 * NS, NCH))]
        gens.append(scans(NS * p_))
        lockstep(gens)
    S.pop()
    return []


def phase_merge(k):
    S, nc = k.S, k.nc
    S.push()
    wbn = S.sbuf("wbn", [128, 4, DM], BF16)
    wbd = S.sbuf("wbd", [128, 4, DM], BF16)
    wo = S.sbuf("wo", [128, 8, DM], BF16)
    wstg = [S.sbuf(f"wstg{i}", [128, DM], F32) for i in range(2)]
    n = 0
    for (dst, src, nk) in ((wbn, "w_branch_nsa", 4), (wbd, "w_branch_dn", 4), (wo, "w_out", 8)):
        for kc in range(nk):
            st = wstg[n % 2]
            n += 1
            S.dma(out=st[:], in_=k.inp[src].ap()[kc * 128:(kc + 1) * 128, :])
            S.I("pool", "tensor_copy", out=dst[:, kc, :], in_=st[:])
    onT = S.sbuf("onT", [128, 4, SEQ], BF16)
    odT = S.sbuf("odT", [128, 4, SEQ], BF16)
    S.dma(out=onT[:], in_=k.d["onsaT_d"].ap().rearrange("(j p) t -> p j t", p=128))
    odin = [S.sbuf(f"odin{i}", [128, 512], F32) for i in range(2)]
    odb = [S.sbuf(f"odb{i}", [128, 512], BF16) for i in range(2)]
    for i in range(NT):
        o, ob_ = odin[i % 2], odb[i % 2]
        S.dma(out=o[:], in_=k.d["odn_d"].ap()[i * 128:(i + 1) * 128, :])
        S.I("pool", "tensor_copy", out=ob_[:], in_=o[:])
        pt = k.pb[i % 2]
        for j in range(4):
            S.I("pe", "transpose", out=pt[:, j * 128:(j + 1) * 128], in_=ob_[:, j * 128:(j + 1) * 128], identity=k.ident_b[:])
        evac(k, odT[:, :, i * 128:(i + 1) * 128], pt[:, 0:512].rearrange("p (a b) -> p a b", a=4))
    mT = [S.sbuf(f"mT{i}", [128, 8, 512], BF16) for i in range(2)]
    gnb = [S.sbuf(f"gnb{i}", [128, 512], BF16) for i in range(2)]
    gdb = [S.sbuf(f"gdb{i}", [128, 512], BF16) for i in range(2)]
    t1 = [S.sbuf(f"mt1{i}", [128, 512], F32) for i in range(2)]
    t2 = [S.sbuf(f"mt2{i}", [128, 512], F32) for i in range(2)]
    xt = [S.sbuf(f"mxt{i}", [128, DM], F32) for i in range(2)]
    yt = [S.sbuf(f"myt{i}", [128, DM], F32) for i in range(2)]
    x1 = [S.sbuf(f"mx1{i}", [128, DM], F32) for i in range(2)]
    h2 = [S.sbuf(f"mh2{i}", [128, DM], F32) for i in range(2)]
    tmp = [S.sbuf(f"mtmp{i}", [128, DM], F32) for i in range(2)]
    junk = S.sbuf("mjunk", [128, DM], F32)
    ss = [S.sbuf(f"mss{i}", [128, 4], F32) for i in range(2)]
    q = 0
    for G in range(8):
        m_ = mT[G % 2]
        for m in range(8):
            p1, p2 = k.ps[(2 * q) % 4], k.ps[(2 * q + 1) % 4]
            b = q % 2
            q += 1
            for kc in range(4):
                S.I("pe", "matmul", out=p1[:], lhsT=wbn[:, kc, m * 128:(m + 1) * 128], rhs=onT[:, kc, G * 512:(G + 1) * 512], start=(kc == 0), stop=(kc == 3))
            for kc in range(4):
                S.I("pe", "matmul", out=p2[:], lhsT=wbd[:, kc, m * 128:(m + 1) * 128], rhs=odT[:, kc, G * 512:(G + 1) * 512], start=(kc == 0), stop=(kc == 3))
            S.dma(out=gnb[b][:], in_=k.d["gnT_d"].ap()[m * 128:(m + 1) * 128, G * 512:(G + 1) * 512])
            S.dma(out=gdb[b][:], in_=k.d["gdT_d"].ap()[m * 128:(m + 1) * 128, G * 512:(G + 1) * 512])
            S.I("dve", "tensor_tensor", out=t1[b][:], in0=p1[:], in1=gnb[b][:], op=ALU.mult)
            S.I("dve", "tensor_tensor", out=t2[b][:], in0=p2[:], in1=gdb[b][:], op=ALU.mult)
            S.I("pool", "tensor_tensor", out=m_[:, m, :], in0=t1[b][:], in1=t2[b][:], op=ALU.add)
        for c in range(4):
            i = 4 * G + c
            b = i % 2
            pys = [k.ps[4], k.ps[5]]
            for half in range(2):
                for m in range(8):
                    S.I("pe", "matmul", out=pys[half][:], lhsT=m_[:, m, c * 128:(c + 1) * 128], rhs=wo[:, m, half * 512:(half + 1) * 512],
                        start=(m == 0), stop=(m == 7))
            S.dma(out=xt[b][:], in_=k.inp["x"].ap()[i * 128:(i + 1) * 128, :])
            for half in range(2):
                S.I("dve", "tensor_tensor", out=yt[b][:, half * 512:(half + 1) * 512], in0=pys[half][:], in1=k.modb[:, 2 * DM + half * 512:2 * DM + (half + 1) * 512], op=ALU.mult)
            S.I("pool", "tensor_tensor", out=x1[b][:], in0=yt[b][:], in1=xt[b][:], op=ALU.add)
            S.dma(out=k.d["x1_d"].ap()[i * 128:(i + 1) * 128, :], in_=x1[b][:])
            rms_mod_tile(k, x1[b][:], k.modb[:, 4 * DM:5 * DM], k.modb[:, 3 * DM:4 * DM], h2[b][:], tmp[b][:], ss[b], junk[:])
            S.dma(out=k.d["h2_d"].ap()[i * 128:(i + 1) * 128, :], in_=h2[b][:])
    S.pop()
    return []


def phase_peer(k):
    S, nc = k.S, k.nc
    S.push()
    uv_d = k.d["uv_d"]
    wq = S.sbuf("pwq", [128, 8, DM], BF16)
    keysbd = S.sbuf("pkeysbd", [128, 8, 256], BF16)
    S.push()
    wstg = [S.sbuf(f"pwstg{i}", [128, DM], F32) for i in range(2)]
    for kc in range(8):
        st = wstg[kc % 2]
        S.dma(out=st[:], in_=k.inp["peer_w_query"].ap()[kc * 128:(kc + 1) * 128, :])
        S.I("pool", "tensor_copy", out=wq[:, kc, :], in_=st[:])
    kst = S.sbuf("pkst", [128, 8, 256], F32)
    S.I("pool", "memset", kst[:], 0.0, extra_writes=[kst[:]])
    for h in range(8):
        S.dma(out=kst[0:64, h, 0:128], in_=k.inp["peer_keys1"].ap()[h].rearrange("k d -> d k"), allow_slow_non_contiguous=True)
        S.dma(out=kst[64:128, h, 128:256], in_=k.inp["peer_keys2"].ap()[h].rearrange("k d -> d k"), allow_slow_non_contiguous=True)
    S.I("pool", "tensor_copy", out=keysbd[:], in_=kst[:])
    S.pop()
    fgb = S.sbuf("pfgb", [128, DM], F32)
    S.dma(out=fgb[:], in_=k.inp["final_g"].ap().partition_broadcast(128))
    iota16 = S.sbuf("piota", [128, 16], F32)
    S.dma(out=iota16[:], in_=k.cst["iota16"].ap())
    NB = 16
    gbuf = [S.sbuf(f"pgb{i}", [128, 2 * DM], BF16) for i in range(NB)]
    diag = [S.sbuf(f"pdiag{i}", [128, 128], BF16) for i in range(4)]
    h2t = [S.sbuf(f"ph2t{i}", [128, DM], F32) for i in range(2)]
    h2b = [S.sbuf(f"ph2b{i}", [128, DM], BF16) for i in range(2)]
    h2T = [S.sbuf(f"ph2T{i}", [128, 8, 128], BF16) for i in range(2)]
    qryT = [S.sbuf(f"pqryT{i}", [128, 8, 128], BF16) for i in range(2)]
    scs = [S.sbuf(f"pscs{i}", [128, 8, 256], F32) for i in range(2)]
    s1r = S.sbuf("ps1r", [128, 128], F32)
    cand = S.sbuf("pcand", [128, 16, 16], F32)
    candr = S.sbuf("pcandr", [128, 16, 16], F32)
    v1 = S.sbuf("pv1", [128, 8, 16], F32)
    v2 = S.sbuf("pv2", [128, 8, 16], F32)
    i1 = S.sbuf("pi1", [128, 8, 16], U32)
    i2 = S.sbuf("pi2", [128, 8, 16], U32)
    ts = S.sbuf("pts", [128, 8, 16], F32)
    pos = S.sbuf("ppos", [128, 8, 16], U32)
    ra = S.sbuf("pra", [128, 8, 16], U32)
    rb = S.sbuf("prb", [128, 8, 16], U32)
    raf = S.sbuf("praf", [128, 8, 16], F32)
    rbf = S.sbuf("prbf", [128, 8, 16], F32)
    i1f = S.sbuf("pi1f", [128, 8, 16], F32)
    i2f = S.sbuf("pi2f", [128, 8, 16], F32)
    oh = S.sbuf("poh", [128, 8, 16, 16], F32)
    sel1 = S.sbuf("psel1", [128, 8, 16], F32)
    sel2 = S.sbuf("psel2", [128, 8, 16], F32)
    eidf = S.sbuf("peidf", [128, 128], F32)
    eidx = [S.sbuf(f"peidx{i}", [128, 128], I32) for i in range(2)]
    gate = [S.sbuf(f"pgate{i}", [128, 8, 16], F32) for i in range(2)]
    gsm = S.sbuf("pgsm", [128, 32], F32)
    av = [S.sbuf(f"pav{i}", [128, 128], F32) for i in range(2)]
    gt1 = S.sbuf("pgt1", [128, 128], F32)
    gt2 = S.sbuf("pgt2", [128, 128], F32)
    coef = [S.sbuf(f"pcoef{i}", [128, 128], F32) for i in range(2)]
    junk = S.sbuf("pjunk", [128, DM], F32)
    junk2 = S.sbuf("pjunk2", [128, DM], F32)
    acc = [S.sbuf(f"pacc{i}", [128, DM], F32) for i in range(2)]
    x1t = [S.sbuf(f"px1t{i}", [128, DM], F32) for i in range(2)]
    ss = [S.sbuf(f"pss{i}", [128, 4], F32) for i in range(2)]
    outs = []
    nt_run = NT if k.sub != "peer1" else 1
    gcount = [0]

    def prep(i):
        b = i % 2
        S.dma(out=h2t[b][:], in_=k.d["h2_d"].ap()[i * 128:(i + 1) * 128, :])
        S.I("act", "activation", out=h2b[b][:], in_=h2t[b][:], func=AF.Copy)
        pt = k.pb[b]
        ptv = pt[:].rearrange("p (a b) -> p a b", a=8)
        for kc in range(8):
            S.I("pe", "transpose", out=ptv[:, kc, :], in_=h2b[b][:, kc * 128:(kc + 1) * 128], identity=k.ident_b[:])
        evac(k, h2T[b][:], ptv, eng="act")
        for hh in range(2):
            pq = k.ps[hh]
            for h4 in range(4):
                h = hh * 4 + h4
                for kc in range(8):
                    S.I("pe", "matmul", out=pq[:, h4 * 128:(h4 + 1) * 128], lhsT=wq[:, kc, h * 128:(h + 1) * 128], rhs=h2T[b][:, kc, :],
                        start=(kc == 0), stop=(kc == 7))
            evac(k, qryT[b][:, hh * 4:hh * 4 + 4, :], pq[:].rearrange("p (a b) -> p a b", a=4), eng="act")
        for h2_ in range(4):
            psc = k.ps[2 + (h2_ % 2)]
            for e in range(2):
                h = h2_ * 2 + e
                S.I("pe", "matmul", out=psc[:, e * 256:(e + 1) * 256], lhsT=qryT[b][:, h, :], rhs=keysbd[:, h, :], start=True, stop=True)
            S.I("dve", "tensor_copy", out=scs[b][:, 2 * h2_:2 * h2_ + 2, :], in_=psc[:].rearrange("p (a b) -> p a b", a=2))
        for h in range(8):
            for (vv, ii, off) in ((v1, i1, 0), (v2, i2, 128)):
                s_in = scs[b][:, h, off:off + 128]
                S.I("dve", "max", out=vv[:, h, 0:8], in_=s_in)
                S.I("dve", "max_index", out=ii[:, h, 0:8], in_max=vv[:, h, 0:8], in_values=s_in)
                S.I("dve", "match_replace", out=s1r[:], in_to_replace=vv[:, h, 0:8], in_values=s_in, imm_value=-3.0e38)
                S.I("dve", "max", out=vv[:, h, 8:16], in_=s1r[:])
                S.I("dve", "max_index", out=ii[:, h, 8:16], in_max=vv[:, h, 8:16], in_values=s1r[:])
            S.I("dve", "tensor_tensor", out=cand[:], in0=v1[:, h, :].unsqueeze(2).to_broadcast([128, 16, 16]),
                in1=v2[:, h, :].unsqueeze(1).to_broadcast([128, 16, 16]), op=ALU.add)
            cf = cand[:].rearrange("p a b -> p (a b)")
            crf = candr[:].rearrange("p a b -> p (a b)")
            S.I("dve", "max", out=ts[:, h, 0:8], in_=cf)
            S.I("dve", "max_index", out=pos[:, h, 0:8], in_max=ts[:, h, 0:8], in_values=cf)
            S.I("dve", "match_replace", out=crf, in_to_replace=ts[:, h, 0:8], in_values=cf, imm_value=-3.0e38)
            S.I("dve", "max", out=ts[:, h, 8:16], in_=crf)
            S.I("dve", "max_index", out=pos[:, h, 8:16], in_max=ts[:, h, 8:16], in_values=crf)
        g_ = gate[b]
        S.I("dve", "tensor_tensor", out=g_[:], in0=ts[:], in1=ts[:, :, 0:1].to_broadcast([128, 8, 16]), op=ALU.subtract)
        S.I("act", "activation", out=g_[:], in_=g_[:], func=AF.Exp)
        S.I("dve", "tensor_reduce", out=gsm[:, 0:8], in_=g_[:], axis=AX.X, op=ALU.add)
        S.I("dve", "reciprocal", out=gsm[:, 8:16], in_=gsm[:, 0:8])
        S.I("dve", "tensor_tensor", out=g_[:], in0=g_[:], in1=gsm[:, 8:16].unsqueeze(2).to_broadcast([128, 8, 16]), op=ALU.mult)
        S.I("dve", "tensor_single_scalar", out=ra[:], in_=pos[:], scalar=4, op=ALU.logical_shift_right)
        S.I("dve", "tensor_single_scalar", out=rb[:], in_=pos[:], scalar=15, op=ALU.bitwise_and)
        for (src, dst) in ((ra, raf), (rb, rbf), (i1, i1f), (i2, i2f)):
            S.I("dve", "tensor_copy", out=dst[:], in_=src[:])
        iob = iota16[:].unsqueeze(1).unsqueeze(1).to_broadcast([128, 8, 16, 16])
        for (rf, idf_, sel) in ((raf, i1f, sel1), (rbf, i2f, sel2)):
            S.I("dve", "tensor_tensor", out=oh[:], in0=rf[:].unsqueeze(3).to_broadcast([128, 8, 16, 16]), in1=iob, op=ALU.is_equal)
            S.I("dve", "tensor_tensor", out=oh[:], in0=oh[:], in1=idf_[:].unsqueeze(2).to_broadcast([128, 8, 16, 16]), op=ALU.mult)
            S.I("dve", "tensor_reduce", out=sel[:], in_=oh[:], axis=AX.X, op=ALU.add)
        S.I("dve", "scalar_tensor_tensor", out=eidf[:], in0=sel1[:].rearrange("p a b -> p (a b)"), scalar=128.0,
            in1=sel2[:].rearrange("p a b -> p (a b)"), op0=ALU.mult, op1=ALU.add)
        S.I("dve", "tensor_copy", out=eidx[b][:], in_=eidf[:])

    def evalx(i, mid=None):
        b = i % 2
        py = [k.ps[4], k.ps[5]]
        for grp in range(16):
            sl = slice(grp * 8, grp * 8 + 8)
            bufs = []
            for jj in range(8):
                j = grp * 8 + jj
                gb = gbuf[gcount[0] % NB]
                gcount[0] += 1
                bufs.append(gb)
                S.idma(out=gb[:], in_=uv_d.ap(), idx_ap=eidx[b][:, j:j + 1], lean=True)
                S.I("dve", "scalar_tensor_tensor", out=junk2[:], in0=h2t[b][:], scalar=1.0, in1=gb[:, 0:DM], op0=ALU.mult, op1=ALU.mult,
                    accum_out=av[b][:, j:j + 1])
            S.I("dve", "tensor_tensor", out=gt1[:, sl], in0=av[b][:, sl], in1=av[b][:, sl], op=ALU.mult)
            S.I("dve", "tensor_scalar", out=gt1[:, sl], in0=gt1[:, sl], scalar1=0.044715, scalar2=1.0, op0=ALU.mult, op1=ALU.add)
            S.I("dve", "tensor_tensor", out=gt2[:, sl], in0=gt1[:, sl], in1=av[b][:, sl], op=ALU.mult)
            S.I("act", "activation", out=gt1[:, sl], in_=gt2[:, sl], func=AF.Sigmoid, scale=GELU_C)
            S.I("dve", "tensor_tensor", out=gt2[:, sl], in0=gt1[:, sl], in1=av[b][:, sl], op=ALU.mult)
            S.I("dve", "tensor_tensor", out=coef[b][:, sl], in0=gt2[:, sl], in1=gate[b][:].rearrange("p a b -> p (a b)")[:, sl], op=ALU.mult)
            for jj in range(8):
                j = grp * 8 + jj
                dg = diag[j % 4]
                S.I("act", "activation", out=dg[:], in_=k.ident_b[:], func=AF.Copy, scale=coef[b][:, j:j + 1])
                for half in range(2):
                    S.I("pe", "matmul", out=py[half][:], lhsT=dg[:], rhs=bufs[jj][:, DM + half * 512:DM + (half + 1) * 512],
                        start=(j == 0), stop=(j == 127))
            if mid is not None and grp == 7:
                mid()
        return py

    def final(i, py):
        b = i % 2
        S.dma(out=x1t[b][:], in_=k.d["x1_d"].ap()[i * 128:(i + 1) * 128, :])
        for half in range(2):
            S.I("dve", "tensor_tensor", out=acc[b][:, half * 512:(half + 1) * 512], in0=py[half][:], in1=k.modb[:, 5 * DM + half * 512:5 * DM + (half + 1) * 512], op=ALU.mult)
        S.I("dve", "tensor_tensor", out=x1t[b][:], in0=acc[b][:], in1=x1t[b][:], op=ALU.add)
        s_ = ss[b]
        S.I("act", "activation", out=junk[:], in_=x1t[b][:], func=AF.Square, accum_out=s_[:, 0:1])
        S.I("dve", "tensor_scalar", out=s_[:, 1:2], in0=s_[:, 0:1], scalar1=1.0 / DM, scalar2=EPS, op0=ALU.mult, op1=ALU.add)
        S.I("act", "activation", out=s_[:, 2:3], in_=s_[:, 1:2], func=AF.Sqrt)
        S.I("dve", "reciprocal", out=s_[:, 3:4], in_=s_[:, 2:3])
        S.I("dve", "scalar_tensor_tensor", out=acc[b][:], in0=x1t[b][:], scalar=s_[:, 3:4], in1=fgb[:], op0=ALU.mult, op1=ALU.mult)
        outs.append(S.dma(out=k.out.ap()[i * 128:(i + 1) * 128, :], in_=acc[b][:]))

    prep(0)
    for i in range(nt_run):
        py = evalx(i, mid=(lambda i=i: prep(i + 1)) if i + 1 < nt_run else None)
        final(i, py)
    S.pop()
    return outs


_CACHE = {}


def make_in_maps(inputs, consts, used=None):
    maps = []
    for b in range(8):
        m = {}
        for n, shp in INPUT_SPECS.items():
            if used is not None and n not in used:
                continue
            a = np.asarray(inputs[n])
            if n == "x" or n == "c":
                a = a[b]
            elif n in ("rel_bias", "final_g"):
                pass
            else:
                a = a[0]
            m[n] = np.ascontiguousarray(a.reshape(shp).astype(np.float32, copy=False))
        for n in CONST_SPECS:
            if used is None or n in used:
                m[n] = consts[n]
        maps.append(m)
    return maps


def kernel(**inputs):
    consts = host_consts()
    nc = build_program()
    res = run_bass_kernel_spmd(nc, make_in_maps(inputs, consts, nc.used_inputs), core_ids=list(range(8)))
    return np.stack([np.asarray(r["out"]).reshape(SEQ, DM) for r in res.results], axis=0).astype(np.float32)
```

```python
import numpy as np
import concourse.bass as bass
import concourse.mybir as mybir
from contextlib import ExitStack

F32 = mybir.dt.float32
BF16 = mybir.dt.bfloat16
U32 = mybir.dt.uint32
I32 = mybir.dt.int32
ALU = mybir.AluOpType
AF = mybir.ActivationFunctionType
AX = mybir.AxisListType

EPOCH = 1 << 30
ENGS = ("pe", "act", "dve", "pool", "sp")
WRITE_KW = ("out", "accum_out", "out_max", "out_indices")


class Res:
    __slots__ = ("name", "last_w", "readers", "children", "parent")

    def __init__(self, name, parent=None):
        self.name = name
        self.last_w = None
        self.readers = []
        self.children = {}
        self.parent = parent


class Op:
    __slots__ = ("eng", "fn", "deps", "needed", "is_dma", "sig", "idx", "slotwait", "name")

    def __init__(self, eng, fn, is_dma=False, name=""):
        self.eng = eng
        self.fn = fn
        self.deps = []
        self.needed = False
        self.is_dma = is_dma
        self.sig = None
        self.slotwait = None
        self.name = name


class Sched:
    def __init__(self, nc, dma_slots=None):
        self.nc = nc
        self.ops = {e: [] for e in ENGS}
        self.res = {}
        self.tags = {}
        self._keep = []
        self.dma_count = {e: 0 for e in ENGS}
        self.dma_ops = {e: [] for e in ENGS}
        self.dma_slots = dma_slots or {"sp": 8, "pool": 12, "act": 4}
        self.stack = ExitStack()
        self.scopes = []
        self.nbar = {}
        self.all_ops = []

    def push(self):
        self.scopes.append(ExitStack())

    def pop(self):
        self.barrier()
        self.scopes.pop().close()

    def sbuf(self, name, shape, dtype):
        t = (self.scopes[-1] if self.scopes else self.stack).enter_context(self.nc.sbuf_tensor(name, list(shape), dtype))
        self.res[t.name] = Res(t.name)
        return t

    def psum(self, name, shape, dtype):
        t = self.stack.enter_context(self.nc.psum_tensor(name, list(shape), dtype))
        self.res[t.name] = Res(t.name)
        return t

    def dram(self, name, shape, dtype, kind="Internal"):
        t = self.nc.dram_tensor(name, list(shape), dtype, kind=kind)
        self.res[t.name] = Res(t.name)
        return t

    def tag(self, ap, key):
        base = self.res[ap.name]
        if key not in base.children:
            base.children[key] = Res(base.name + ":" + str(key), parent=base)
        self.tags[id(ap)] = base.children[key]
        self._keep.append(ap)
        return ap

    def _res_of(self, ap):
        r = self.tags.get(id(ap))
        if r is not None:
            return r
        nm = ap.name
        if nm not in self.res:
            self.res[nm] = Res(nm)
        return self.res[nm]

    def _related(self, r):
        out = [r]
        if r.parent is not None:
            out.append(r.parent)
        out.extend(r.children.values())
        return out

    def _track(self, op, reads, writes):
        deps = set()
        for r in reads:
            for rr in self._related(r):
                if rr.last_w is not None:
                    deps.add(rr.last_w)
        for w in writes:
            for rr in self._related(w):
                if rr.last_w is not None:
                    deps.add(rr.last_w)
                for q in rr.readers:
                    deps.add(q)
        deps.discard(op)
        for r in reads:
            r.readers.append(op)
        for w in writes:
            w.last_w = op
            w.readers = []
        best = {}
        final = []
        for d in deps:
            if d.is_dma:
                final.append(d)
            else:
                if d.eng == "pe" and op.eng == "pe":
                    continue
                b = best.get(d.eng)
                if b is None or d.idx > b.idx:
                    best[d.eng] = d
        final.extend(best.values())
        for d in final:
            d.needed = True
        op.deps = final

    def _add(self, op, reads, writes):
        op.idx = len(self.ops[op.eng])
        self.ops[op.eng].append(op)
        self.all_ops.append(op)
        self._track(op, reads, writes)
        return op

    def I(self, eng, meth, *args, extra_reads=(), extra_writes=(), **kw):
        reads, writes = [], []
        for k, v in kw.items():
            if hasattr(v, "tensor") and hasattr(v, "ap"):
                (writes if k in WRITE_KW else reads).append(self._res_of(v))
        for v in args:
            if hasattr(v, "tensor") and hasattr(v, "ap"):
                reads.append(self._res_of(v))
        for v in extra_reads:
            reads.append(self._res_of(v))
        for v in extra_writes:
            writes.append(self._res_of(v))

        def fn(e, meth=meth, args=args, kw=kw):
            return getattr(e, meth)(*args, **kw)

        return self._add(Op(eng, fn, name=meth), reads, writes)

    def dma(self, out, in_, q="sp", **kw):
        def fn(e, out=out, in_=in_, kw=kw):
            return e.dma_start(out=out, in_=in_, **kw)

        op = Op(q, fn, is_dma=True, name="dma")
        op.needed = True
        d = self.dma_count[q]
        self.dma_count[q] += 1
        op.sig = ("dma", q, d)
        self.dma_ops[q].append(op)
        return self._add(op, [self._res_of(in_)], [self._res_of(out)])

    def idma(self, out, in_, idx_ap, axis=0, lean=False, **kw):
        def fn(e, out=out, in_=in_, idx_ap=idx_ap, kw=kw):
            return e.indirect_dma_start(out=out, out_offset=None, in_=in_,
                                        in_offset=bass.IndirectOffsetOnAxis(ap=idx_ap, axis=axis), **kw)

        q = "pool"
        op = Op(q, fn, is_dma=True, name="idma")
        op.needed = True
        d = self.dma_count[q]
        self.dma_count[q] += 1
        op.sig = ("dma", q, d)
        self.dma_ops[q].append(op)
        self._add(op, [self._res_of(in_), self._res_of(idx_ap)], [self._res_of(out)])
        if lean:
            op.slotwait = "skip"
            if any((not d_.is_dma) and d_.eng == "pe" for d_ in op.deps):
                op.deps = [d_ for d_ in op.deps if d_.is_dma or d_.eng != "dve"]
        return op

    def barrier(self):
        marks = []
        for e in ENGS:
            last = None
            for op in reversed(self.ops[e]):
                if (not op.is_dma) and op.fn is not None and op.fn != "SEMINC":
                    last = op
                    break
            if last is not None:
                last.needed = True
                marks.append(last)
            P = self.dma_slots.get(e, 0)
            if P and self.dma_ops[e]:
                sg = Op(e, "SEMINC", name="barsig")
                sg.idx = len(self.ops[e])
                sg.deps = list(self.dma_ops[e][-P:])
                sg.needed = True
                self.nbar[e] = self.nbar.get(e, 0) + 1
                sg.sig = ("b", e, self.nbar[e])
                self.ops[e].append(sg)
                self.all_ops.append(sg)
                marks.append(sg)
        for e in ENGS:
            op = Op(e, None, name="barwait")
            op.idx = len(self.ops[e])
            op.deps = [m for m in marks if m.eng != e]
            self.ops[e].append(op)
            self.all_ops.append(op)
        for r in self.res.values():
            r.last_w = None
            r.readers = []
            for c in r.children.values():
                c.last_w = None
                c.readers = []

    def final_wait(self, dma_ops):
        op = Op("sp", None, name="finalwait")
        op.idx = len(self.ops["sp"])
        op.deps = list(dma_ops)
        self.ops["sp"].append(op)

    def emit(self):
        nc = self.nc
        sems = {}
        nsig = {}
        for e in ENGS:
            k = 0
            for op in self.ops[e]:
                if op.is_dma:
                    continue
                if op.needed and op.fn is not None and op.fn != "SEMINC":
                    op.sig = ("c", e, k)
                    k += 1
            nsig[e] = k
            n_ep = (k + EPOCH - 1) // EPOCH
            sems[e] = [self.stack.enter_context(nc.semaphore(f"s_{e}_{i}")) for i in range(n_ep)]
        dsems = {}
        for e in ENGS:
            if self.dma_count[e] > 0:
                P = self.dma_slots[e]
                dsems[e] = [self.stack.enter_context(nc.semaphore(f"d_{e}_{i}")) for i in range(P)]
                pass

        bsems = {e: self.stack.enter_context(nc.semaphore(f"b_{e}")) for e in self.nbar}

        def sigval(op):
            kind, e, k = op.sig
            if kind == "b":
                return bsems[e], k
            if kind == "c":
                return sems[e][k // EPOCH], k % EPOCH + 1
            P = self.dma_slots[e]
            return dsems[e][k % P], 16 * (k // P + 1)

        self.stats = {e: len(self.ops[e]) for e in ENGS}

        def run(e, eng):
            waited = {}

            def wait(sem, val):
                key = sem.num if hasattr(sem, "num") else id(sem)
                if waited.get(key, 0) >= val:
                    return
                eng.wait_ge(sem, val)
                waited[key] = val

            for op in self.ops[e]:
                for d in op.deps:
                    s, v = sigval(d)
                    wait(s, v)
                if op.is_dma and op.slotwait != "skip":
                    kind, q, k = op.sig
                    P = self.dma_slots[q]
                    if k >= P:
                        wait(dsems[q][k % P], 16 * (k // P))
                if op.fn is None:
                    continue
                if op.fn == "SEMINC":
                    s, v = sigval(op)
                    eng.sem_inc(s, 1)
                    continue
                ins = op.fn(eng)
                if op.needed:
                    s, v = sigval(op)
                    ins.then_inc(s, 16 if op.is_dma else 1)

        with nc.Block() as block:
            @block.tensor
            def _(eng):
                run("pe", eng)

            @block.scalar
            def _(eng):
                run("act", eng)

            @block.vector
            def _(eng):
                run("dve", eng)

            @block.gpsimd
            def _(eng):
                run("pool", eng)

            @block.sync
            def _(eng):
                run("sp", eng)

    def close(self):
        self.stack.close()
import math
import ml_dtypes
from concourse.bass_utils import run_bass_kernel_spmd

SEQ = 4096
DM = 1024
NT = SEQ // 128
IN_COLS = 5416
C_Q, C_KC, C_VC, C_KS, C_VS, C_KW, C_VW, C_GT = 0, 512, 640, 768, 896, 1024, 1152, 1280
C_DQ, C_DK, C_DV, C_DZ, C_DB, C_DA, C_GN, C_GD = 1304, 1816, 2328, 2840, 3352, 3360, 3368, 4392
NEG = -30000.0
EPS = 1e-6
GELU_C = 1.5957691216057308


def _rel_bucket(dist):
    dist = np.maximum(dist, 0)
    scaled = np.log(np.maximum(dist, 1).astype(np.float32) / 16) / np.float32(math.log(128 / 16))
    large = np.minimum(16 + (scaled.astype(np.float32) * 16).astype(np.int32), 31)
    return np.where(dist < 16, dist, large)


def host_consts():
    c = {}
    c["ident_f"] = np.eye(128, dtype=np.float32)
    c["ident_b"] = np.eye(128, dtype=np.float32).astype(ml_dtypes.bfloat16)
    c["anti_f"] = np.eye(128, dtype=np.float32)[::-1].copy()
    m = np.arange(16384) - 8192
    bucket = _rel_bucket(m)
    oh = np.zeros((33, 16384), np.float32)
    pos = m >= 0
    oh[bucket[pos], np.nonzero(pos)[0]] = 1.0
    oh[31, pos] -= 1.0
    oh[32, ~pos] = 1.0
    c["ohp"] = oh
    t = np.arange(SEQ)
    cur = t // 64
    j = np.arange(64)[None, :]
    forced = (j == 0) | (j == cur[:, None]) | (j == cur[:, None] - 1)
    future = j * 64 > t[:, None]
    c["slc_add"] = np.where(future, np.float32(-1e30), np.where(forced, np.float32(1e4), np.float32(0))).astype(np.float32)
    c["e_blk"] = (np.arange(SEQ)[None, :] // 64 == np.arange(64)[:, None]).astype(np.float32).astype(ml_dtypes.bfloat16)
    p = np.arange(128)[:, None]
    f = np.arange(128)[None, :]
    c["w4t"] = np.where(f >= p, np.float32(NEG), np.float32(0)).astype(ml_dtypes.bfloat16)
    n = np.arange(255)
    cs, ce = n * 16, n * 16 + 31
    ss = np.arange(64) * 64
    c["overlap"] = ((cs[:, None] <= ss[None, :] + 63) & (ce[:, None] >= ss[None, :])).astype(np.float32)
    c["m_cum"] = (p[:64] <= f[:, :64]).astype(np.float32)
    c["m_incl_u"] = np.where(p[:64] <= f[:, :64], 0.0, NEG).astype(np.float32)
    c["m_incl_l"] = np.where(f[:, :64] <= p[:64], 0.0, NEG).astype(np.float32)
    c["m_nstr_u"] = np.where(p[:64] < f[:, :64], -1.0, 0.0).astype(np.float32)
    c["m_nstr_l"] = np.where(f[:, :64] < p[:64], -1.0, 0.0).astype(np.float32)
    c["iota16"] = np.tile(np.arange(16, dtype=np.float32)[None, :], (128, 1))
    return c


CONST_SPECS = {
    "ident_f": ([128, 128], F32), "ident_b": ([128, 128], BF16), "anti_f": ([128, 128], F32),
    "ohp": ([33, 16384], F32), "slc_add": ([SEQ, 64], F32), "e_blk": ([64, SEQ], BF16),
    "w4t": ([128, 128], BF16), "overlap": ([255, 64], F32),
    "m_cum": ([64, 64], F32), "m_incl_u": ([64, 64], F32), "m_incl_l": ([64, 64], F32),
    "m_nstr_u": ([64, 64], F32), "m_nstr_l": ([64, 64], F32), "iota16": ([128, 16], F32),
}

INPUT_SPECS = {
    "x": [SEQ, DM], "c": [DM], "rel_bias": [32, 8], "final_g": [1, DM], "w_ada": [DM, 6 * DM], "b_ada": [1, 6 * DM],
    "norm1_g": [1, DM], "w_in": [DM, IN_COLS], "cmp_pos_emb": [32, 64], "w_cmp_k1": [2048, 64], "w_cmp_k2": [64, 64],
    "w_cmp_v1": [2048, 64], "w_cmp_v2": [64, 64], "dn_conv_w": [4, 1536], "dn_A_log": [1, 8], "dn_dt_bias": [1, 8],
    "dn_norm_g": [1, 64], "w_branch_nsa": [512, DM], "w_branch_dn": [512, DM], "w_out": [DM, DM], "norm2_g": [1, DM],
    "peer_w_query": [DM, DM], "peer_keys1": [8, 128, 64], "peer_keys2": [8, 128, 64], "peer_u": [16384, DM], "peer_v": [16384, DM],
}


GDN_CFG = {'solve_bf16': False, 'prod_bf16': True, 'scan_bf16': True}


class K:
    pass


def build_program(stop_after=None, debug=(), sub=None, skip=()):
    nc = bass.Bass("TRN2", target_bir_lowering=False)
    S = Sched(nc)
    k = K()
    k.nc, k.S = nc, S
    k.sub = sub
    k.gdn_cfg = dict(GDN_CFG)
    class _Lazy(dict):
        def __init__(self, specs, isconst):
            self.specs, self.isconst = specs, isconst
        def __missing__(self, n):
            if self.isconst:
                shp, dt = self.specs[n]
            else:
                shp, dt = self.specs[n], F32
            t = nc.dram_tensor(n, shp, dt, kind="ExternalInput")
            S.res[t.name] = Res(t.name)
            self[n] = t
            return t
    k.inp = _Lazy(INPUT_SPECS, False)
    k.cst = _Lazy(CONST_SPECS, True)
    k.out = nc.dram_tensor("out", [SEQ, DM], F32, kind="ExternalOutput")
    S.res[k.out.name] = Res(k.out.name)
    dbg = set(debug)

    def scratch(name, shape, dt):
        return S.dram(name, shape, dt, kind=("ExternalOutput" if name in dbg else "Internal"))

    k.d = {}
    for name, shape, dt in [
        ("qT_d", [8, 64, SEQ], BF16), ("kcr_d", [2, 64, SEQ], F32), ("vcr_d", [2, 64, SEQ], F32),
        ("ksT_d", [2, 64, SEQ], BF16), ("kwT_d", [2, 64, SEQ], BF16), ("vsw_d", [SEQ, 256], BF16),
        ("gts_d", [SEQ, 24], F32), ("dnqkv_d", [SEQ, 1536], F32), ("z_d", [SEQ, 512], F32), ("ba_d", [SEQ, 16], F32),
        ("gnT_d", [DM, SEQ], BF16), ("gdT_d", [DM, SEQ], BF16), ("onsaT_d", [512, SEQ], BF16), ("odnT_d", [512, SEQ], BF16),
        ("x1_d", [SEQ, DM], F32), ("h2_d", [SEQ, DM], F32), ("tab_d", [8, 16384], F32),
        ("onsa_d", [SEQ, 512], F32), ("odn_d", [SEQ, 512], F32), ("uv_d", [16384, 2 * DM], BF16),
    ]:
        k.d[name] = scratch(name, shape, dt)

    k.ps = [S.psum(f"ps{i}", [128, 512], F32) for i in range(6)]
    k.pb = [S.psum(f"pb{i}", [128, 1024], BF16) for i in range(2)]
    k.ident_f = S.sbuf("ident_f_s", [128, 128], F32)
    k.ident_b = S.sbuf("ident_b_s", [128, 128], BF16)
    k.modb = S.sbuf("modb", [128, 6 * DM], F32)
    S.dma(out=k.ident_f[:], in_=k.cst["ident_f"].ap())
    S.dma(out=k.ident_b[:], in_=k.cst["ident_b"].ap())
    k.cnt = 0

    phases = [phase_mod, phase_norm_proj, phase_nsa, phase_gdn, phase_merge, phase_peer]
    last = None
    for ph in phases:
        if ph.__name__ in skip:
            continue
        last = ph(k)
        if stop_after == ph.__name__:
            break
    outs = list(last) if last else []
    S.barrier()
    S.final_wait(outs)
    S.emit()
    S.close()
    nc.used_inputs = list(k.inp.keys()) + list(k.cst.keys())
    return nc


def alt(k):
    k.cnt += 1
    return k.cnt


def evac(k, out, in_, scale=None, eng=None):
    S = k.S
    e = eng or ("act" if alt(k) % 2 else "dve")
    if e == "act":
        if scale is None:
            return S.I("act", "activation", out=out, in_=in_, func=AF.Copy)
        return S.I("act", "activation", out=out, in_=in_, func=AF.Copy, scale=float(scale))
    if scale is None:
        return S.I("dve", "tensor_copy", out=out, in_=in_)
    return S.I("dve", "tensor_scalar", out=out, in0=in_, scalar1=float(scale), scalar2=None, op0=ALU.mult)


def phase_mod(k):
    S, nc = k.S, k.nc
    S.push()
    ct = S.sbuf("ct", [128, 8], F32)
    cs = S.sbuf("cs", [128, 8], F32)
    cb = S.sbuf("cb", [128, 8, 128], F32)
    ones = S.sbuf("ones1", [1, 128], F32)
    brow = S.sbuf("brow", [1, 6 * DM], F32)
    wt = [S.sbuf(f"wt{i}", [128, 8, 512], F32) for i in range(2)]
    gb = S.sbuf("gb", [128, DM], F32)
    S.dma(out=ct[:], in_=k.inp["c"].ap().rearrange("(kc k) -> k kc", k=128), allow_slow_non_contiguous=True)
    S.dma(out=brow[:], in_=k.inp["b_ada"].ap())
    S.I("pool", "memset", ones[:], 1.0, extra_writes=[ones[:]])
    S.I("act", "activation", out=cs[:], in_=ct[:], func=AF.Silu)
    S.I("dve", "tensor_copy", out=cb[:], in_=cs[:].unsqueeze(2).to_broadcast([128, 8, 128]))
    wv = k.inp["w_ada"].ap().rearrange("(kc k) n -> k kc n", k=128)
    for n in range(12):
        w = wt[n % 2]
        S.dma(out=w[:], in_=wv[:, :, n * 512:(n + 1) * 512])
        p = k.ps[n % 2]
        for kc in range(8):
            S.I("pe", "matmul", out=p[:], lhsT=cb[:, kc, :], rhs=w[:, kc, :], start=(kc == 0), stop=False)
        S.I("pe", "matmul", out=p[:], lhsT=ones[:], rhs=brow[:, n * 512:(n + 1) * 512], start=False, stop=True)
        evac(k, k.modb[:, n * 512:(n + 1) * 512], p[:])
    for (gname, off) in (("norm1_g", 1 * DM), ("norm2_g", 4 * DM)):
        S.dma(out=gb[:], in_=k.inp[gname].ap().partition_broadcast(128))
        S.I("dve", "scalar_tensor_tensor", out=k.modb[:, off:off + DM], in0=k.modb[:, off:off + DM], scalar=1.0, in1=gb[:],
            op0=ALU.add, op1=ALU.mult)
    S.pop()
    return []


def rms_mod_tile(k, xt, A, sh, out_ap, tmp, ss, junk):
    S = k.S
    S.I("act", "activation", out=junk, in_=xt, func=AF.Square, accum_out=ss[:, 0:1])
    S.I("dve", "tensor_scalar", out=ss[:, 1:2], in0=ss[:, 0:1], scalar1=1.0 / DM, scalar2=EPS, op0=ALU.mult, op1=ALU.add)
    S.I("act", "activation", out=ss[:, 2:3], in_=ss[:, 1:2], func=AF.Sqrt)
    S.I("dve", "reciprocal", out=ss[:, 3:4], in_=ss[:, 2:3])
    S.I("dve", "scalar_tensor_tensor", out=tmp, in0=xt, scalar=ss[:, 3:4], in1=A, op0=ALU.mult, op1=ALU.mult)
    S.I("dve", "tensor_tensor", out=out_ap, in0=tmp, in1=sh, op=ALU.add)


def phase_norm_proj(k):
    S, nc = k.S, k.nc
    S.push()
    hT = S.sbuf("hT", [128, 8, SEQ], BF16)
    S.push()
    xt = [S.sbuf(f"xt{i}", [128, DM], F32) for i in range(2)]
    tmp = [S.sbuf(f"tmpn{i}", [128, DM], F32) for i in range(2)]
    junk = S.sbuf("junkn", [128, DM], F32)
    hb = [S.sbuf(f"hb{i}", [128, DM], BF16) for i in range(2)]
    ss = [S.sbuf(f"ss{i}", [128, 4], F32) for i in range(2)]
    for i in range(NT):
        x = xt[i % 2]
        S.dma(out=x[:], in_=k.inp["x"].ap()[i * 128:(i + 1) * 128, :])
        rms_mod_tile(k, x[:], k.modb[:, DM:2 * DM], k.modb[:, 0:DM], hb[i % 2][:], tmp[i % 2][:], ss[i % 2], junk[:])
        pt = k.pb[i % 2]
        ptv = pt[:].rearrange("p (a b) -> p a b", a=8)
        for kc in range(8):
            S.I("pe", "transpose", out=ptv[:, kc, :], in_=hb[i % 2][:, kc * 128:(kc + 1) * 128], identity=k.ident_b[:])
        evac(k, hT[:, :, i * 128:(i + 1) * 128], ptv)
    S.pop()
    if k.sub == 'p1':
        S.pop()
        return []
    w_in = k.inp["w_in"].ap().rearrange("(kc k) n -> k kc n", k=128)
    S.push()
    wst = [S.sbuf(f"wst{i}", [128, 8, 128], F32) for i in range(2)]
    wtokA = S.sbuf("wtokA", [128, 8, 296], BF16)
    wtokZ = S.sbuf("wtokZ", [128, 8, 512], BF16)
    vstg = S.sbuf("vstg", [128, NT, 256], BF16)
    gstg = S.sbuf("gstg", [128, NT, 24], F32)
    bastg = S.sbuf("bastg", [128, NT, 16], F32)
    gbstg = S.sbuf("gbstg", [128, NT, 40], F32)
    zstg = [S.sbuf(f"zstg{i}", [128, 512], F32) for i in range(2)]
    pieces = [(C_VS, 128, wtokA, 0), (C_VW, 128, wtokA, 128), (C_GT, 24, wtokA, 256), (C_DB, 16, wtokA, 280)] + \
             [(C_DZ + 128 * q, 128, wtokZ, 128 * q) for q in range(4)]
    for n, (c0, w, dst, off) in enumerate(pieces):
        st = wst[n % 2]
        S.dma(out=st[:, :, 0:w], in_=w_in[:, :, c0:c0 + w])
        S.I("pool", "tensor_copy", out=dst[:, :, off:off + w], in_=st[:, :, 0:w])
    for i in range(NT):
        pa, pz = k.ps[(2 * i) % 4], k.ps[(2 * i + 1) % 4]
        for kc in range(8):
            S.I("pe", "matmul", out=pa[:, 0:296], lhsT=hT[:, kc, i * 128:(i + 1) * 128], rhs=wtokA[:, kc, :], start=(kc == 0), stop=(kc == 7))
        for kc in range(8):
            S.I("pe", "matmul", out=pz[:], lhsT=hT[:, kc, i * 128:(i + 1) * 128], rhs=wtokZ[:, kc, :], start=(kc == 0), stop=(kc == 7))
        S.I("dve", "tensor_copy", out=vstg[:, i, :], in_=pa[:, 0:256])
        S.I("dve", "tensor_copy", out=gbstg[:, i, :], in_=pa[:, 256:296])
        z = zstg[i % 2]
        S.I("act", "activation", out=z[:], in_=pz[:], func=AF.Copy)
        S.dma(out=k.d["z_d"].ap()[i * 128:(i + 1) * 128, :], in_=z[:])
    S.I("act", "activation", out=gstg[:], in_=gbstg[:, :, 0:24], func=AF.Sigmoid)
    S.I("dve", "tensor_copy", out=bastg[:], in_=gbstg[:, :, 24:40])
    S.dma(out=k.d["vsw_d"].ap().rearrange("(i p) c -> p i c", p=128), in_=vstg[:])
    S.dma(out=k.d["gts_d"].ap().rearrange("(i p) c -> p i c", p=128), in_=gstg[:])
    S.dma(out=k.d["ba_d"].ap().rearrange("(i p) c -> p i c", p=128), in_=bastg[:])
    S.pop()
    if k.sub and k.sub.startswith('p2a'):
        S.pop()
        return []
    S.push()
    wst = [S.sbuf(f"wstb{i}", [128, 8, 128], F32) for i in range(2)]
    wbf = [S.sbuf(f"wbf{i}", [128, 8, 128], BF16) for i in range(2)]
    stg_f = S.sbuf("stg_f", [128, SEQ + 3], F32)
    stg_b = [S.sbuf(f"stg_b{i}", [128, SEQ], BF16) for i in range(2)]
    acc = S.sbuf("acc_cv", [128, SEQ], F32)
    tokstg = S.sbuf("tokstg", [128, NT, 128], F32)
    cw = S.sbuf("cw", [128, 4, 12], F32)
    for j in range(4):
        S.dma(out=cw[:, j, :], in_=k.inp["dn_conv_w"].ap()[j, :].rearrange("(g c) -> c g", c=128), allow_slow_non_contiguous=True)
    S.I("pool", "memset", stg_f[:, 0:3], 0.0, extra_writes=[stg_f[:]])
    groups = []
    for q in range(4):
        groups.append((C_Q + 128 * q, "q", k.d["qT_d"].ap().rearrange("h d t -> (h d) t")[128 * q:128 * (q + 1), :]))
    groups.append((C_KC, "f32", k.d["kcr_d"].ap().rearrange("h d t -> (h d) t")))
    groups.append((C_VC, "f32", k.d["vcr_d"].ap().rearrange("h d t -> (h d) t")))
    groups.append((C_KS, "bf", k.d["ksT_d"].ap().rearrange("h d t -> (h d) t")))
    groups.append((C_KW, "bf", k.d["kwT_d"].ap().rearrange("h d t -> (h d) t")))
    for q in range(8):
        groups.append((C_GN + 128 * q, "sig", k.d["gnT_d"].ap()[128 * q:128 * (q + 1), :]))
    for q in range(8):
        groups.append((C_GD + 128 * q, "sig", k.d["gdT_d"].ap()[128 * q:128 * (q + 1), :]))
    for q in range(12):
        groups.append((C_DQ + 128 * q, "dn", q))
    nb = 0
    for g, (c0, kind, dest) in enumerate(groups):
        st, wb = wst[g % 2], wbf[g % 2]
        S.dma(out=st[:], in_=w_in[:, :, c0:c0 + 128])
        S.I("pool", "tensor_copy", out=wb[:], in_=st[:])
        if kind in ("q", "bf", "sig"):
            sb = stg_b[nb % 2]
            nb += 1
        for G in range(8):
            p = k.ps[(g * 8 + G) % 4]
            for kc in range(8):
                S.I("pe", "matmul", out=p[:], lhsT=wb[:, kc, :], rhs=hT[:, kc, G * 512:(G + 1) * 512], start=(kc == 0), stop=(kc == 7))
            if kind == "q":
                evac(k, sb[:, G * 512:(G + 1) * 512], p[:], scale=0.125)
            elif kind == "bf":
                evac(k, sb[:, G * 512:(G + 1) * 512], p[:])
            elif kind == "sig":
                S.I("act", "activation", out=sb[:, G * 512:(G + 1) * 512], in_=p[:], func=AF.Sigmoid)
            else:
                evac(k, stg_f[:, 3 + G * 512:3 + (G + 1) * 512], p[:])
        if kind in ("q", "bf", "sig"):
            S.dma(out=dest, in_=sb[:])
        elif kind == "f32":
            S.dma(out=dest, in_=stg_f[:, 3:3 + SEQ])
        else:
            q = dest
            S.I("dve", "tensor_scalar", out=acc[:], in0=stg_f[:, 0:SEQ], scalar1=cw[:, 0, q:q + 1], scalar2=None, op0=ALU.mult)
            for j in range(1, 4):
                S.I("dve", "scalar_tensor_tensor", out=acc[:], in0=stg_f[:, j:j + SEQ], scalar=cw[:, j, q:q + 1], in1=acc[:],
                    op0=ALU.mult, op1=ALU.add)
            S.I("act", "activation", out=acc[:], in_=acc[:], func=AF.Silu)
            for i4 in range(8):
                p = k.ps[i4 % 4]
                pv = p[:].rearrange("p (a b) -> p a b", a=4)
                for qq in range(4):
                    i = 4 * i4 + qq
                    S.I("pe", "transpose", out=pv[:, qq, :], in_=acc[:, i * 128:(i + 1) * 128], identity=k.ident_f[:])
                evac(k, tokstg[:, 4 * i4:4 * i4 + 4, :], pv)
            S.dma(out=k.d["dnqkv_d"].ap().rearrange("(i p) c -> p i c", p=128)[:, :, 128 * q:128 * (q + 1)], in_=tokstg[:])
    S.pop()
    S.pop()
    return []


def phase_nsa(k):
    S, nc = k.S, k.nc
    from concourse.ap import AP as RawAP
    S.push()
    tab_d = k.d["tab_d"]
    S.push()
    rb33 = S.sbuf("rb33", [33, 8], F32)
    S.I("pool", "memset", rb33[:], NEG, extra_writes=[rb33[:]])
    S.dma(out=rb33[0:32, :], in_=k.inp["rel_bias"].ap())
    ohs = [S.sbuf(f"ohs{i}", [33, 2048], F32) for i in range(2)]
    tbs = [S.sbuf(f"tbs{i}", [8, 2048], F32) for i in range(2)]
    for c8 in range(8):
        oh, tb = ohs[c8 % 2], tbs[c8 % 2]
        S.dma(out=oh[:], in_=k.cst["ohp"].ap()[:, c8 * 2048:(c8 + 1) * 2048])
        for q in range(4):
            p = k.ps[q]
            S.I("pe", "matmul", out=p[0:8, :], lhsT=rb33[:], rhs=oh[:, q * 512:(q + 1) * 512], start=True, stop=True)
            evac(k, tb[:, q * 512:(q + 1) * 512], p[0:8, :])
        S.dma(out=tab_d.ap()[:, c8 * 2048:(c8 + 1) * 2048], in_=tb[:])
    S.pop()
    anti = S.sbuf("anti", [128, 128], F32)
    S.dma(out=anti[:], in_=k.cst["anti_f"].ap())
    strips = S.sbuf("strips", [128, 8, 640], BF16)
    S.I("pool", "memset", strips[:], 0.0, extra_writes=[strips[:]])
    hank = [S.sbuf(f"hank{i}", [128, 512], F32) for i in range(2)]
    for h in range(8):
        S.dma(out=strips[:, h, 512:640], in_=k.cst["w4t"].ap())
        hk = hank[h % 2]
        S.dma(out=hk[:, 0:256], in_=RawAP(tab_d, h * 16384 + 8192 - 127, [[1, 128], [1, 256]]))
        p = k.ps[h % 4]
        S.I("pe", "matmul", out=p[:, 0:256], lhsT=anti[:], rhs=hk[:, 0:256], start=True, stop=True)
        evac(k, strips[:, h, 0:256], p[:, 0:256])
    kcT = S.sbuf("kcT", [64, 2, 256], BF16)
    vcaug = S.sbuf("vcaug", [128, 2, 2, 129], F32)
    S.I("pool", "memset", vcaug[:], 0.0, extra_writes=[vcaug[:]])
    S.I("pool", "memset", vcaug[:, :, :, 64:65], 1.0, extra_writes=[vcaug[:]])
    for kv in range(2):
        for a in range(2):
            rows = 128 if a == 0 else 127
            S.dma(out=vcaug[0:rows, kv, a, 65:129], in_=k.cst["overlap"].ap()[a * 128:a * 128 + rows, :])
    S.push()
    w1 = S.sbuf("w1c", [64, 32, 64], F32)
    w2 = S.sbuf("w2c", [64, 64], F32)
    posT = S.sbuf("posT", [64, 32], F32)
    cbias = S.sbuf("cbias", [64, 1], F32)
    kcr = [S.sbuf(f"kcr{i}", [64, SEQ], F32) for i in range(2)]
    xh = S.sbuf("xh", [64, 256], F32)
    t1 = S.sbuf("t1c", [64, 256], F32)
    t2 = S.sbuf("t2c", [64, 256], F32)
    gh = S.sbuf("ghc", [64, 256], F32)
    S.dma(out=posT[:], in_=k.inp["cmp_pos_emb"].ap().rearrange("l d -> d l"), allow_slow_non_contiguous=True)
    n = 0
    for which in ("k", "v"):
        S.dma(out=w1[:], in_=k.inp["w_cmp_%s1" % which].ap().rearrange("(l d) h -> d l h", d=64))
        S.dma(out=w2[:], in_=k.inp["w_cmp_%s2" % which].ap())
        pc = k.ps[0]
        for l in range(32):
            S.I("pe", "matmul", out=pc[0:64, 0:1], lhsT=w1[:, l, :], rhs=posT[:, l:l + 1], start=(l == 0), stop=(l == 31))
        S.I("dve", "tensor_copy", out=cbias[:], in_=pc[0:64, 0:1])
        for kv in range(2):
            kc = kcr[n % 2]
            n += 1
            S.dma(out=kc[:], in_=k.d["kcr_d" if which == "k" else "vcr_d"].ap()[kv])
            kcv = kc[:].rearrange("p (n s) -> p n s", s=16)
            ph = k.ps[1 + (n % 2)]
            for l in range(32):
                rhs = kcv[:, 0:255, l] if l < 16 else kcv[:, 1:256, l - 16]
                S.I("pe", "matmul", out=ph[0:64, 0:255], lhsT=w1[:, l, :], rhs=rhs, start=(l == 0), stop=(l == 31))
            S.I("act", "activation", out=xh[:, 0:255], in_=ph[0:64, 0:255], func=AF.Identity, bias=cbias[:, 0:1])
            S.I("dve", "tensor_tensor", out=t1[:, 0:255], in0=xh[:, 0:255], in1=xh[:, 0:255], op=ALU.mult)
            S.I("dve", "tensor_scalar", out=t1[:, 0:255], in0=t1[:, 0:255], scalar1=0.044715, scalar2=1.0, op0=ALU.mult, op1=ALU.add)
            S.I("dve", "tensor_tensor", out=t2[:, 0:255], in0=t1[:, 0:255], in1=xh[:, 0:255], op=ALU.mult)
            S.I("act", "activation", out=t1[:, 0:255], in_=t2[:, 0:255], func=AF.Sigmoid, scale=GELU_C)
            S.I("dve", "tensor_tensor", out=gh[:, 0:255], in0=t1[:, 0:255], in1=xh[:, 0:255], op=ALU.mult)
            if which == "k":
                pk = k.ps[3]
                S.I("pe", "matmul", out=pk[0:64, 0:255], lhsT=w2[:], rhs=gh[:, 0:255], start=True, stop=True)
                S.I("dve", "tensor_copy", out=kcT[:, kv, 0:255], in_=pk[0:64, 0:255])
            else:
                for a in range(2):
                    rows = 128 if a == 0 else 127
                    pv = k.ps[4 + a]
                    S.I("pe", "matmul", out=pv[0:rows, 0:64], lhsT=gh[:, a * 128:a * 128 + rows], rhs=w2[:], start=True, stop=True)
                    S.I("dve", "tensor_copy", out=vcaug[0:rows, kv, a, 0:64], in_=pv[0:rows, 0:64])
    S.pop()
    if k.sub == "n1":
        S.pop()
        return []
    gates = S.sbuf("gatesb", [128, NT, 24], F32)
    S.dma(out=gates[:], in_=k.d["gts_d"].ap().rearrange("(i p) c -> p i c", p=128))
    slc = S.sbuf("slcadd", [128, NT, 64], F32)
    S.dma(out=slc[:], in_=k.cst["slc_add"].ap().rearrange("(i p) c -> p i c", p=128))
    o_acc = S.sbuf("o_acc", [128, NT, 256], F32)
    imp = S.sbuf("imp", [128, NT, 64], F32)
    qaug = [S.sbuf(f"qaug{g}", [128, SEQ], BF16) for g in range(4)]
    ksaug = S.sbuf("ksaug", [128, SEQ], BF16)
    kwT = S.sbuf("kwT", [64, SEQ], BF16)
    vsa = S.sbuf("vsa", [128, NT, 65], BF16)
    vwa = S.sbuf("vwa", [128, NT, 65], BF16)
    pTb = [S.sbuf(f"pTb{i}", [128, 512], BF16) for i in range(3)]
    pTc4 = [S.sbuf(f"pTc{i}", [128, 512], F32) for i in range(4)]
    hank4 = hank + [S.sbuf(f"hankx{i}", [128, 512], F32) for i in range(2)]
    ftb4 = [S.sbuf(f"ftb{i}", [128, 512], BF16) for i in range(4)]
    selb = [S.sbuf(f"selb{i}", [128, 128], BF16) for i in range(2)]
    sc = [S.sbuf(f"scr{i}", [128, 64], F32) for i in range(2)]
    sc2 = [S.sbuf(f"scr2{i}", [128, 64], F32) for i in range(2)]
    m8 = [S.sbuf(f"m8{i}", [128, 16], F32) for i in range(2)]
    rz = [S.sbuf(f"rz{i}", [128, 8], F32) for i in range(2)]
    tmpo = [S.sbuf(f"tmpo{i}", [128, 4, 64], F32) for i in range(2)]
    ob = [S.sbuf(f"ob{i}", [128, 256], BF16) for i in range(2)]
    oT = S.sbuf("oTn", [128, 2, SEQ], BF16)
    for i in range(2):
        S.I("pool", "memset", selb[i][:], 0.0, extra_writes=[selb[i][:]])
    S.dma(out=ksaug[64:128, :], in_=k.cst["e_blk"].ap())
    S.I("pool", "memset", vsa[:, :, 64:65], 1.0, extra_writes=[vsa[:]])
    S.I("pool", "memset", vwa[:, :, 64:65], 1.0, extra_writes=[vwa[:]])
    vsw_v = k.d["vsw_d"].ap().rearrange("(i p) c -> p i c", p=128)
    cnt = [0]

    def finish_branch(h, g, G, po_views, zcol, br, first):
        r = rz[cnt[0] % 2]
        tm = tmpo[cnt[0] % 2]
        cnt[0] += 1
        for c in range(4):
            S.I("dve", "tensor_scalar", out=r[:, c:c + 1], in0=po_views[c][:, zcol:zcol + 1], scalar1=1e-30, scalar2=None, op0=ALU.max)
        S.I("dve", "reciprocal", out=r[:, 0:4], in_=r[:, 0:4])
        S.I("dve", "tensor_tensor", out=r[:, 4:8], in0=r[:, 0:4], in1=gates[:, 4 * G:4 * G + 4, h * 3 + br], op=ALU.mult)
        for c in range(4):
            dst = o_acc[:, 4 * G + c, g * 64:(g + 1) * 64]
            if first:
                S.I("dve", "tensor_scalar", out=dst, in0=po_views[c][:, 0:64], scalar1=r[:, 4 + c:5 + c], scalar2=None, op0=ALU.mult)
            else:
                S.I("dve", "scalar_tensor_tensor", out=dst, in0=po_views[c][:, 0:64], scalar=r[:, 4 + c:5 + c], in1=dst,
                    op0=ALU.mult, op1=ALU.add)
        return r

    nps = [0]

    def next_ps():
        nps[0] += 1
        return k.ps[nps[0] % 4]

    for kv in range(2):
        for g in range(4):
            S.dma(out=qaug[g][0:64, :], in_=k.d["qT_d"].ap()[kv * 4 + g])
        S.dma(out=ksaug[0:64, :], in_=k.d["ksT_d"].ap()[kv])
        S.dma(out=kwT[:], in_=k.d["kwT_d"].ap()[kv])
        S.dma(out=vsa[:, :, 0:64], in_=vsw_v[:, :, kv * 64:kv * 64 + 64])
        S.dma(out=vwa[:, :, 0:64], in_=vsw_v[:, :, 128 + kv * 64:128 + kv * 64 + 64])
        def cmp_front(g, G, rnd):
            h = kv * 4 + g
            a_list = [0] if G < 4 else [0, 1]
            tiles = []
            for a in a_list:
                rows = 128 if a == 0 else 127
                need_corr = (a == 0 and G <= 4) or (a == 1)
                pss = next_ps()
                S.I("pe", "matmul", out=pss[0:rows, :], lhsT=kcT[:, kv, a * 128:a * 128 + rows], rhs=qaug[g][0:64, G * 512:(G + 1) * 512],
                    start=True, stop=not need_corr)
                if need_corr:
                    hk = hank4[cnt[0] % 4]
                    ft = ftb4[cnt[0] % 4]
                    cnt[0] += 1
                    S.dma(out=hk[:], in_=RawAP(tab_d, h * 16384 + 8192 + 512 * G - 2048 * a - 2063, [[16, 128], [1, 512]]))
                    pj = next_ps()
                    S.I("pe", "matmul", out=pj[:], lhsT=anti[:], rhs=hk[:], start=True, stop=True)
                    evac(k, ft[:], pj[:], eng="dve")
                    S.I("pe", "matmul", out=pss[0:rows, :], lhsT=k.ident_b[0:rows, 0:rows], rhs=ft[0:rows, :], start=False, stop=True)
                pt_ = pTc4[(rnd % 2) * 2 + a]
                S.I("act", "activation", out=pt_[0:rows, :], in_=pss[0:rows, :], func=AF.Exp)
                tiles.append((a, rows, pt_))
            return (g, G, h, tiles)

        def cmp_back(ctx):
            g, G, h, tiles = ctx
            pA, pB = k.ps[4], k.ps[5]
            views = []
            for c in range(4):
                pv = (pA if c < 2 else pB)[:, (c % 2) * 256:(c % 2) * 256 + 129]
                views.append(pv)
                for ti, (a, rows, pt_) in enumerate(tiles):
                    S.I("pe", "matmul", out=pv, lhsT=pt_[0:rows, c * 128:(c + 1) * 128], rhs=vcaug[0:rows, kv, a, :],
                        start=(ti == 0), stop=(ti == len(tiles) - 1))
            r = finish_branch(h, g, G, views, 64, 0, True)
            for c in range(4):
                dst = imp[:, 4 * G + c, :]
                if g == 0:
                    S.I("dve", "tensor_scalar", out=dst, in0=views[c][:, 65:129], scalar1=r[:, c:c + 1], scalar2=None, op0=ALU.mult)
                else:
                    S.I("dve", "scalar_tensor_tensor", out=dst, in0=views[c][:, 65:129], scalar=r[:, c:c + 1], in1=dst,
                        op0=ALU.mult, op1=ALU.add)

        rounds = [(g, G) for g in range(4) for G in range(8)]
        prev = cmp_front(rounds[0][0], rounds[0][1], 0)
        for ri in range(1, len(rounds)):
            cur = cmp_front(rounds[ri][0], rounds[ri][1], ri)
            cmp_back(prev)
            prev = cur
        cmp_back(prev)
        for i in range(NT):
            s1, s2, mm, sb = sc[i % 2], sc2[i % 2], m8[i % 2], selb[i % 2]
            S.I("dve", "tensor_tensor", out=s1[:], in0=imp[:, i, :], in1=slc[:, i, :], op=ALU.add)
            S.I("dve", "max", out=mm[:, 0:8], in_=s1[:])
            S.I("dve", "match_replace", out=s2[:], in_to_replace=mm[:, 0:8], in_values=s1[:], imm_value=-3.0e38)
            S.I("dve", "max", out=mm[:, 8:16], in_=s2[:])
            S.I("dve", "tensor_scalar", out=sb[:, 64:128], in0=s1[:], scalar1=mm[:, 15:16], scalar2=NEG, op0=ALU.is_lt, op1=ALU.mult)
            pt = k.pb[i % 2]
            S.I("pe", "transpose", out=pt[:, 0:128], in_=sb[:], identity=k.ident_b[:])
            for g in range(4):
                evac(k, qaug[g][64:128, i * 128:(i + 1) * 128], pt[64:128, 0:128], eng="dve" if i % 2 else "act")
        if k.sub == "n2":
            continue
        for g in range(4):
            h = kv * 4 + g
            for G in range(8):
                po = k.ps[4 + (G % 2)]
                views = [po[:, c * 128:c * 128 + 65] for c in range(4)]

                def qk_sel(P, g=g, h=h, G=G):
                    c_lo = max(0, P - 4 * G)
                    ncol = 4 - c_lo
                    t0 = G * 512 + c_lo * 128
                    pss = next_ps()
                    if P >= 4 * G:
                        S.I("pe", "matmul", out=pss[:, 0:ncol * 128], lhsT=ksaug[:, P * 128:(P + 1) * 128], rhs=qaug[g][:, t0:(G + 1) * 512],
                            start=True, stop=False)
                        S.I("pe", "matmul", out=pss[:, 0:ncol * 128], lhsT=k.ident_b[:], rhs=strips[:, h, 0:ncol * 128], start=False, stop=True)
                    elif P == 4 * G - 1:
                        S.I("pe", "matmul", out=pss[:, 0:128], lhsT=ksaug[:, P * 128:(P + 1) * 128], rhs=qaug[g][:, t0:t0 + 128],
                            start=True, stop=False)
                        S.I("pe", "matmul", out=pss[:, 0:128], lhsT=k.ident_b[:], rhs=strips[:, h, 128:256], start=False, stop=True)
                        S.I("pe", "matmul", out=pss[:, 128:512], lhsT=ksaug[:, P * 128:(P + 1) * 128], rhs=qaug[g][:, t0 + 128:(G + 1) * 512],
                            start=True, stop=True)
                    else:
                        S.I("pe", "matmul", out=pss[:, 0:ncol * 128], lhsT=ksaug[:, P * 128:(P + 1) * 128], rhs=qaug[g][:, t0:(G + 1) * 512],
                            start=True, stop=True)
                    pT = pTb[nps[0] % 3]
                    S.I("act", "activation", out=pT[:, 0:ncol * 128], in_=pss[:, 0:ncol * 128], func=AF.Exp)
                    return (P, c_lo, pT)

                def pv_sel(ctx, G=G, views=views):
                    P, c_lo, pT = ctx
                    for c in range(c_lo, 4):
                        S.I("pe", "matmul", out=views[c], lhsT=pT[:, (c - c_lo) * 128:(c - c_lo + 1) * 128], rhs=vsa[:, P, :],
                            start=(P == 0 and c == 0), stop=(P == 4 * G + c), skip_group_check=True)

                ncv = (kv * 4 + g) * 8 + G
                src_t = k.inp["peer_u" if ncv < 32 else "peer_v"].ap()
                r0 = (ncv % 32) * 512
                S.dma(out=k.d["uv_d"].ap()[r0:r0 + 512, (ncv // 32) * DM:(ncv // 32 + 1) * DM], in_=src_t[r0:r0 + 512, :], q="pool")
                Ps = list(range(0, 4 * G + 4))
                prev = qk_sel(Ps[0])
                for P in Ps[1:]:
                    cur = qk_sel(P)
                    pv_sel(prev)
                    prev = cur
                pv_sel(prev)
                finish_branch(h, g, G, views, 64, 1, False)
        for g in range(4):
            h = kv * 4 + g
            for G in range(8):
                po = k.ps[4 + (G % 2)]
                views = [po[:, c * 128:c * 128 + 65] for c in range(4)]

                def qk_win(P, g=g, h=h, G=G):
                    c_lo = max(0, P - 4 * G)
                    c_hi = min(3, P - 4 * G + 4)
                    ncol = c_hi - c_lo + 1
                    r_lo = 4 * G + c_lo - P
                    t0 = G * 512 + c_lo * 128
                    pss = next_ps()
                    S.I("pe", "matmul", out=pss[:, 0:ncol * 128], lhsT=kwT[:, P * 128:(P + 1) * 128], rhs=qaug[g][0:64, t0:t0 + ncol * 128],
                        start=True, stop=False)
                    S.I("pe", "matmul", out=pss[:, 0:ncol * 128], lhsT=k.ident_b[:], rhs=strips[:, h, r_lo * 128:(r_lo + ncol) * 128], start=False, stop=True)
                    pT = pTb[nps[0] % 3]
                    S.I("act", "activation", out=pT[:, 0:ncol * 128], in_=pss[:, 0:ncol * 128], func=AF.Exp)
                    return (P, c_lo, c_hi, pT)

                def pv_win(ctx, G=G, views=views):
                    P, c_lo, c_hi, pT = ctx
                    for c in range(c_lo, c_hi + 1):
                        S.I("pe", "matmul", out=views[c], lhsT=pT[:, (c - c_lo) * 128:(c - c_lo + 1) * 128], rhs=vwa[:, P, :],
                            start=(P == max(0, 4 * G - 4) and c == 0), stop=(P == 4 * G + c), skip_group_check=True)

                Ps = list(range(max(0, 4 * G - 4), 4 * G + 4))
                prev = qk_win(Ps[0])
                for P in Ps[1:]:
                    cur = qk_win(P)
                    pv_win(prev)
                    prev = cur
                pv_win(prev)
                finish_branch(h, g, G, views, 64, 2, False)
        S.dma(out=k.d["onsa_d"].ap().rearrange("(i p) c -> p i c", p=128)[:, :, kv * 256:(kv + 1) * 256], in_=o_acc[:])
        for i in range(NT):
            o = ob[i % 2]
            S.I("pool", "tensor_copy", out=o[:], in_=o_acc[:, i, :])
            pt = k.pb[i % 2]
            for j in range(2):
                S.I("pe", "transpose", out=pt[:, j * 128:(j + 1) * 128], in_=o[:, j * 128:(j + 1) * 128], identity=k.ident_b[:])
            evac(k, oT[:, :, i * 128:(i + 1) * 128], pt[:, 0:256].rearrange("p (a b) -> p a b", a=2))
        S.dma(out=k.d["onsaT_d"].ap().rearrange("(j p) t -> p j t", p=128)[:, 2 * kv:2 * kv + 2, :], in_=oT[:])
    S.pop()
    return []


def phase_gdn(k):
    S, nc = k.S, k.nc
    S.push()
    gdn_d = S.dram("gdn_d", [SEQ, 2056], F32)

    def v3(ap, a):
        return ap.rearrange("p (a b) -> p a b", a=a)

    def bc2(ap, n, w):
        return ap.unsqueeze(2).to_broadcast([n, ap.shape[1], w])

    def bc1(ap, n, a):
        return ap.unsqueeze(1).to_broadcast([n, a, ap.shape[1]])

    S.push()
    dtb = S.sbuf("dtb", [128, 8], F32)
    negA = S.sbuf("negA", [128, 8], F32)
    S.dma(out=dtb[:], in_=k.inp["dn_dt_bias"].ap().partition_broadcast(128))
    S.dma(out=negA[:], in_=k.inp["dn_A_log"].ap().partition_broadcast(128))
    S.I("act", "activation", out=negA[:], in_=negA[:], func=AF.Exp)
    S.I("dve", "tensor_scalar", out=negA[:], in0=negA[:], scalar1=-1.0, scalar2=None, op0=ALU.mult)
    xin = [S.sbuf(f"gxin{i}", [128, 1536], F32) for i in range(2)]
    bain = [S.sbuf(f"gbain{i}", [128, 16], F32) for i in range(2)]
    xout = [S.sbuf(f"gxout{i}", [128, 2056], F32) for i in range(2)]
    sqt = S.sbuf("gsq", [128, 1024], F32)
    sm = [S.sbuf(f"gsm{i}", [128, 48], F32) for i in range(2)]
    for i in range(NT):
        xq, ba, Xo, s_ = xin[i % 2], bain[i % 2], xout[i % 2], sm[i % 2]
        S.dma(out=xq[:], in_=k.d["dnqkv_d"].ap()[i * 128:(i + 1) * 128, :])
        S.dma(out=ba[:], in_=k.d["ba_d"].ap()[i * 128:(i + 1) * 128, :])
        S.I("act", "activation", out=sqt[:], in_=xq[:, 0:1024], func=AF.Square)
        S.I("dve", "tensor_reduce", out=s_[:, 0:16], in_=v3(sqt[:], 16), axis=AX.X, op=ALU.add)
        S.I("dve", "tensor_scalar", out=s_[:, 0:16], in0=s_[:, 0:16], scalar1=EPS, scalar2=None, op0=ALU.add)
        S.I("act", "activation", out=s_[:, 0:16], in_=s_[:, 0:16], func=AF.Sqrt)
        S.I("dve", "reciprocal", out=s_[:, 16:32], in_=s_[:, 0:16])
        S.I("dve", "tensor_scalar", out=s_[:, 16:24], in0=s_[:, 16:24], scalar1=0.125, scalar2=None, op0=ALU.mult)
        S.I("dve", "tensor_tensor", out=v3(Xo[:, 0:1024], 16), in0=v3(xq[:, 0:1024], 16), in1=bc2(s_[:, 16:32], 128, 64), op=ALU.mult)
        S.I("act", "activation", out=s_[:, 32:40], in_=ba[:, 0:8], func=AF.Sigmoid)
        S.I("dve", "tensor_tensor", out=s_[:, 40:48], in0=ba[:, 8:16], in1=dtb[:], op=ALU.add)
        S.I("act", "activation", out=s_[:, 40:48], in_=s_[:, 40:48], func=AF.Exp)
        S.I("dve", "tensor_scalar", out=s_[:, 40:48], in0=s_[:, 40:48], scalar1=1.0, scalar2=None, op0=ALU.add)
        S.I("act", "activation", out=s_[:, 40:48], in_=s_[:, 40:48], func=AF.Ln)
        S.I("dve", "tensor_tensor", out=Xo[:, 2048:2056], in0=s_[:, 40:48], in1=negA[:], op=ALU.mult)
        S.I("pool", "tensor_tensor", out=v3(Xo[:, 1024:1536], 8), in0=v3(Xo[:, 512:1024], 8), in1=bc2(s_[:, 32:40], 128, 64), op=ALU.mult)
        S.I("pool", "tensor_tensor", out=v3(Xo[:, 1536:2048], 8), in0=v3(xq[:, 1024:1536], 8), in1=bc2(s_[:, 32:40], 128, 64), op=ALU.mult)
        S.dma(out=gdn_d.ap()[i * 128:(i + 1) * 128, :], in_=Xo[:])
    S.pop()
    C = 64
    NCH = SEQ // C
    cm = {}
    for nm in ("m_cum", "m_incl_u", "m_incl_l", "m_nstr_u", "m_nstr_l"):
        cm[nm] = S.sbuf("c_" + nm, [64, 64], F32)
        S.dma(out=cm[nm][:], in_=k.cst[nm].ap())
    ones64 = S.sbuf("ones64", [64, 64], F32)
    S.I("pool", "memset", ones64[:], 1.0, extra_writes=[ones64[:]])
    gng = S.sbuf("gng", [64, 64], F32)
    S.dma(out=gng[:], in_=k.inp["dn_norm_g"].ap().partition_broadcast(64))
    St = S.sbuf("gstate", [64, 8, 64], F32)
    S.I("pool", "memset", St[:], 0.0, extra_writes=[St[:]])
    idf = k.ident_f[0:64, 0:64]

    DT_SOLVE = BF16 if k.gdn_cfg.get('solve_bf16', True) else F32
    DT_PROD = BF16 if k.gdn_cfg.get('prod_bf16', True) else F32
    DT_SCAN = BF16 if k.gdn_cfg.get('scan_bf16', True) else F32
    NS = 3
    def mk(name, shape, n=2, dt=F32):
        n = {2: NS, 4: 2 * NS, 22: 2}[n]
        return [S.sbuf(f"{name}{i}", shape, dt) for i in range(n)]

    X = mk("gX", [64, 2056])
    rhsU = mk("grhsU", [64, 8, 64])
    D1 = mk("gD1", [64, 8, 64])
    ET = mk("gET", [64, 8, 64])
    EE = mk("gEE", [64, 8, 64])
    ETn = mk("gETn", [64, 8, 64])
    En = EE
    tmpd = rhsU
    kT = mk("gkT", [64, 8, 64], 2, DT_PROD)
    kbT = mk("gkbT", [64, 8, 64], 2, DT_PROD)
    NTa = mk("gNTa", [64, 8, 64], 2, DT_SOLVE)
    NTb = mk("gNTb", [64, 8, 64], 2, DT_SOLVE)
    Na = mk("gNa", [64, 8, 64], 2, DT_SOLVE)
    Nb = mk("gNb", [64, 8, 64], 2, DT_SOLVE)
    qT = mk("gqT", [64, 8, 64], 4, DT_PROD)
    attnT = mk("gattnT", [64, 8, 64], 4, DT_SCAN)
    X6 = mk("gX6", [64, 8, 128], 4, DT_SOLVE)
    qTs = mk("gqTs", [64, 8, 64], 4, DT_SCAN)
    Tmat = mk("gTmat", [64, 8, 64])
    TTs = mk("gTTs", [64, 8, 64])
    R6 = mk("gR6", [64, 8, 128])
    Stb = S.sbuf("gstateb", [64, 8, 64], DT_SCAN)
    S.I("pool", "memset", Stb[:], 0.0, extra_writes=[Stb[:]])
    wT = mk("gwT", [64, 8, 64], 4, DT_SCAN)
    kdec = mk("gkdec", [64, 8, 64], 4, DT_SCAN)
    sm = mk("gsmall", [64, 64], 4)
    Z = mk("gZ", [64, 512], 22)
    vnew = mk("gvnew", [64, 8, 64], 22, DT_SCAN)
    osb = mk("gosb", [64, 8, 64], 22)
    odn = mk("godn", [64, 512], 22)
    pbf = [k.pb[i][:].bitcast(F32) for i in range(2)]

    def mm8(out_ps, lhs, rhs, w=64):
        for h in range(8):
            S.I("pe", "matmul", out=out_ps[0:64, h * w:(h + 1) * w], lhsT=lhs(h), rhs=rhs(h), start=True, stop=True)

    def tr8(out_ps, src):
        for h in range(8):
            S.I("pe", "transpose", out=out_ps[0:64, h * 64:(h + 1) * 64], in_=src(h), identity=idf)

    def pv(ps):
        return v3(ps[0:64, :], 8)

    def pre(n):
        b, L = n % NS, n % (2 * NS)
        banks = [k.ps[2 * b + i] for i in range(2)]
        cnt = [0]

        def nps():
            cnt[0] += 1
            return banks[cnt[0] % 2]

        x = X[b]
        S.dma(out=x[:], in_=gdn_d.ap()[n * C:(n + 1) * C, :])
        qn, kn, kb, vb = (v3(x[:, o:o + 512], 8) for o in (0, 512, 1024, 1536))
        g = x[:, 2048:2056]
        s_ = sm[L]
        S.I("pool", "tensor_tensor", out=rhsU[b][:], in0=bc2(g, 64, 64), in1=bc1(cm["m_cum"][:], 64, 8), op=ALU.mult)
        pG = nps()
        S.I("pe", "matmul", out=pG[0:64, :], lhsT=ones64[:], rhs=rhsU[b][:].rearrange("p a b -> p (a b)"), start=True, stop=True)
        pg = nps()
        S.I("pe", "matmul", out=pg[0:64, 0:8], lhsT=cm["m_cum"][:], rhs=g, start=True, stop=True)
        yield
        S.I("dve", "tensor_copy", out=s_[:, 0:8], in_=pg[0:64, 0:8])
        S.I("act", "activation", out=s_[:, 8:16], in_=s_[:, 0:8], func=AF.Exp)
        S.I("dve", "tensor_tensor", out=D1[b][:], in0=pv(pG), in1=bc2(s_[:, 0:8], 64, 64), op=ALU.subtract)
        S.I("dve", "tensor_copy", out=s_[:, 16:24], in_=pv(pG)[:, :, 63])
        S.I("act", "activation", out=s_[:, 24:32], in_=s_[:, 16:24], func=AF.Exp)
        S.I("dve", "tensor_tensor", out=s_[:, 32:40], in0=s_[:, 16:24], in1=s_[:, 0:8], op=ALU.subtract)
        S.I("act", "activation", out=s_[:, 32:40], in_=s_[:, 32:40], func=AF.Exp)
        yield
        S.I("pool", "tensor_tensor", out=tmpd[b][:], in0=D1[b][:], in1=bc1(cm["m_incl_u"][:], 64, 8), op=ALU.add)
        S.I("act", "activation", out=ET[b][:], in_=tmpd[b][:], func=AF.Exp)
        S.I("dve", "scalar_tensor_tensor", out=EE[b][:], in0=D1[b][:], scalar=-1.0, in1=bc1(cm["m_incl_l"][:], 64, 8), op0=ALU.mult, op1=ALU.add)
        S.I("act", "activation", out=EE[b][:], in_=EE[b][:], func=AF.Exp)
        S.I("pool", "tensor_tensor", out=ETn[b][:], in0=ET[b][:], in1=bc1(cm["m_nstr_u"][:], 64, 8), op=ALU.mult)
        S.I("pool", "tensor_tensor", out=En[b][:], in0=EE[b][:], in1=bc1(cm["m_nstr_l"][:], 64, 8), op=ALU.mult)
        yield
        for (dst, src) in ((kT[b], kn), (qT[L], qn), (kbT[b], kb)):
            p = nps()
            tr8(p, lambda h, src=src: src[:, h, :])
            if dst is qT[L]:
                S.I("dve", "tensor_copy", out=dst[:], in_=pv(p))
                S.I("dve", "tensor_copy", out=qTs[L][:], in_=pv(p))
            else:
                evac(k, dst[:], pv(p))
            yield
        p = nps()
        mm8(p, lambda h: kT[b][:, h, :], lambda h: kbT[b][:, h, :])
        S.I("dve", "tensor_tensor", out=NTa[b][:], in0=pv(p), in1=ETn[b][:], op=ALU.mult)
        yield
        p = nps()
        mm8(p, lambda h: kbT[b][:, h, :], lambda h: kT[b][:, h, :])
        S.I("dve", "tensor_tensor", out=Na[b][:], in0=pv(p), in1=En[b][:], op=ALU.mult)
        yield
        p = nps()
        mm8(p, lambda h: kT[b][:, h, :], lambda h: qT[L][:, h, :])
        S.I("dve", "tensor_tensor", out=attnT[L][:], in0=pv(p), in1=ET[b][:], op=ALU.mult)
        S.I("pool", "tensor_copy", out=R6[b][:, :, 0:64], in_=vb)
        S.I("pool", "tensor_tensor", out=R6[b][:, :, 64:128], in0=kb, in1=bc2(s_[:, 8:16], 64, 64), op=ALU.mult)
        S.I("pool", "tensor_tensor", out=kdec[L][:], in0=kn, in1=bc2(s_[:, 32:40], 64, 64), op=ALU.mult)
        yield
        NTc, Nc, NTn, Nn = NTa[b], Na[b], NTb[b], Nb[b]
        Tm = Tmat[b]
        S.I("dve", "tensor_tensor", out=Tm[:], in0=Nc[:], in1=bc1(idf, 64, 8), op=ALU.add)
        for lvl in range(1, 6):
            p2 = nps()
            mm8(p2, lambda h: Nc[:, h, :], lambda h: NTc[:, h, :])
            S.I("act", "activation", out=NTn[:], in_=pv(p2), func=AF.Copy)
            yield
            if lvl < 5:
                p1 = nps()
                tr8(p1, lambda h, NTn=NTn: NTn[:, h, :])
                S.I("act", "activation", out=Nn[:], in_=pv(p1), func=AF.Copy)
            NTc, Nc, NTn, Nn = NTn, Nn, NTc, Nc
            pY = nps()
            mm8(pY, lambda h: NTc[:, h, :], lambda h: Tm[:, h, :])
            S.I("dve", "tensor_tensor", out=Tm[:], in0=Tm[:], in1=pv(pY), op=ALU.add)
            yield
        pT_ = nps()
        tr8(pT_, lambda h: Tm[:, h, :])
        S.I("act", "activation", out=TTs[b][:], in_=pv(pT_), func=AF.Copy)
        yield
        pA, pB = nps(), nps()
        for h in range(8):
            pp = pA if h < 4 else pB
            S.I("pe", "matmul", out=pp[0:64, (h % 4) * 128:(h % 4 + 1) * 128], lhsT=TTs[b][:, h, :], rhs=R6[b][:, h, :], start=True, stop=True)
        S.I("dve", "tensor_copy", out=X6[L][:, 0:4, :], in_=v3(pA[0:64, :], 4))
        S.I("dve", "tensor_copy", out=X6[L][:, 4:8, :], in_=v3(pB[0:64, :], 4))
        yield
        p = nps()
        tr8(p, lambda h: X6[L][:, h, 64:128])
        evac(k, wT[L][:], pv(p))
        yield

    def scan(n):
        b, L = n % 2, n % (2 * NS)
        s_ = sm[L]
        S.dma(out=Z[b][:], in_=k.d["z_d"].ap()[n * C:(n + 1) * C, :])
        S.I("act", "activation", out=Z[b][:], in_=Z[b][:], func=AF.Silu)
        p = pbf[0]
        mm8(p, lambda h: wT[L][:, h, :], lambda h: Stb[:, h, :])
        S.I("dve", "tensor_tensor", out=vnew[b][:], in0=X6[L][:, :, 0:64], in1=pv(p), op=ALU.subtract)
        yield
        pq = pbf[1]
        mm8(pq, lambda h: qTs[L][:, h, :], lambda h: Stb[:, h, :])
        S.I("dve", "tensor_tensor", out=osb[b][:], in0=pv(pq), in1=bc2(s_[:, 8:16], 64, 64), op=ALU.mult)
        yield
        pa_ = pbf[0]
        mm8(pa_, lambda h: attnT[L][:, h, :], lambda h: vnew[b][:, h, :])
        S.I("dve", "tensor_tensor", out=osb[b][:], in0=osb[b][:], in1=pv(pa_), op=ALU.add)
        yield
        pk_ = pbf[1]
        mm8(pk_, lambda h: kdec[L][:, h, :], lambda h: vnew[b][:, h, :])
        S.I("dve", "tensor_tensor", out=St[:], in0=St[:], in1=bc2(s_[:, 24:32], 64, 64), op=ALU.mult)
        S.I("dve", "tensor_tensor", out=St[:], in0=St[:], in1=pv(pk_), op=ALU.add)
        S.I("act", "activation", out=Stb[:], in_=St[:], func=AF.Copy)
        yield
        S.I("act", "activation", out=v3(odn[b][:], 8), in_=osb[b][:], func=AF.Square)
        S.I("dve", "tensor_reduce", out=s_[:, 40:48], in_=v3(odn[b][:], 8), axis=AX.X, op=ALU.add)
        S.I("dve", "tensor_scalar", out=s_[:, 40:48], in0=s_[:, 40:48], scalar1=1.0 / 64, scalar2=EPS, op0=ALU.mult, op1=ALU.add)
        S.I("act", "activation", out=s_[:, 40:48], in_=s_[:, 40:48], func=AF.Sqrt)
        S.I("dve", "reciprocal", out=s_[:, 48:56], in_=s_[:, 40:48])
        yield
        S.I("pool", "tensor_tensor", out=osb[b][:], in0=osb[b][:], in1=bc2(s_[:, 48:56], 64, 64), op=ALU.mult)
        S.I("pool", "tensor_tensor", out=osb[b][:], in0=osb[b][:], in1=bc1(gng[:], 64, 8), op=ALU.mult)
        S.I("pool", "tensor_tensor", out=odn[b][:], in0=osb[b][:].rearrange("p a b -> p (a b)"), in1=Z[b][:], op=ALU.mult)
        S.dma(out=k.d["odn_d"].ap()[n * C:(n + 1) * C, :], in_=odn[b][:])
        yield

    def scans(n0):
        for n in range(n0, min(n0 + NS, NCH)):
            for _ in scan(n):
                yield

    def lockstep(gens):
        gens = list(gens)
        while gens:
            for g_ in list(gens):
                try:
                    next(g_)
                except StopIteration:
                    gens.remove(g_)

    lockstep([pre(i) for i in range(NS)])
    for p_ in range((NCH + NS - 1) // NS):
        gens = [pre(n) for n in range(NS * p_ + NS, min(NS * p_ + 2 * NS, NCH))]
        gens.append(scans(NS * p_))
        lockstep(gens)
    S.pop()
    return []


def phase_merge(k):
    S, nc = k.S, k.nc
    S.push()
    wbn = S.sbuf("wbn", [128, 4, DM], BF16)
    wbd = S.sbuf("wbd", [128, 4, DM], BF16)
    wo = S.sbuf("wo", [128, 8, DM], BF16)
    wstg = [S.sbuf(f"wstg{i}", [128, DM], F32) for i in range(2)]
    n = 0
    for (dst, src, nk) in ((wbn, "w_branch_nsa", 4), (wbd, "w_branch_dn", 4), (wo, "w_out", 8)):
        for kc in range(nk):
            st = wstg[n % 2]
            n += 1
            S.dma(out=st[:], in_=k.inp[src].ap()[kc * 128:(kc + 1) * 128, :])
            S.I("pool", "tensor_copy", out=dst[:, kc, :], in_=st[:])
    onT = S.sbuf("onT", [128, 4, SEQ], BF16)
    odT = S.sbuf("odT", [128, 4, SEQ], BF16)
    S.dma(out=onT[:], in_=k.d["onsaT_d"].ap().rearrange("(j p) t -> p j t", p=128))
    odin = [S.sbuf(f"odin{i}", [128, 512], F32) for i in range(2)]
    odb = [S.sbuf(f"odb{i}", [128, 512], BF16) for i in range(2)]
    for i in range(NT):
        o, ob_ = odin[i % 2], odb[i % 2]
        S.dma(out=o[:], in_=k.d["odn_d"].ap()[i * 128:(i + 1) * 128, :])
        S.I("pool", "tensor_copy", out=ob_[:], in_=o[:])
        pt = k.pb[i % 2]
        for j in range(4):
            S.I("pe", "transpose", out=pt[:, j * 128:(j + 1) * 128], in_=ob_[:, j * 128:(j + 1) * 128], identity=k.ident_b[:])
        evac(k, odT[:, :, i * 128:(i + 1) * 128], pt[:, 0:512].rearrange("p (a b) -> p a b", a=4))
    mT = [S.sbuf(f"mT{i}", [128, 8, 512], BF16) for i in range(2)]
    gnb = [S.sbuf(f"gnb{i}", [128, 512], BF16) for i in range(2)]
    gdb = [S.sbuf(f"gdb{i}", [128, 512], BF16) for i in range(2)]
    t1 = [S.sbuf(f"mt1{i}", [128, 512], F32) for i in range(2)]
    t2 = [S.sbuf(f"mt2{i}", [128, 512], F32) for i in range(2)]
    xt = [S.sbuf(f"mxt{i}", [128, DM], F32) for i in range(2)]
    yt = [S.sbuf(f"myt{i}", [128, DM], F32) for i in range(2)]
    x1 = [S.sbuf(f"mx1{i}", [128, DM], F32) for i in range(2)]
    h2 = [S.sbuf(f"mh2{i}", [128, DM], F32) for i in range(2)]
    tmp = [S.sbuf(f"mtmp{i}", [128, DM], F32) for i in range(2)]
    junk = S.sbuf("mjunk", [128, DM], F32)
    ss = [S.sbuf(f"mss{i}", [128, 4], F32) for i in range(2)]
    q = 0
    for G in range(8):
        m_ = mT[G % 2]
        for m in range(8):
            p1, p2 = k.ps[(2 * q) % 4], k.ps[(2 * q + 1) % 4]
            b = q % 2
            q += 1
            for kc in range(4):
                S.I("pe", "matmul", out=p1[:], lhsT=wbn[:, kc, m * 128:(m + 1) * 128], rhs=onT[:, kc, G * 512:(G + 1) * 512], start=(kc == 0), stop=(kc == 3))
            for kc in range(4):
                S.I("pe", "matmul", out=p2[:], lhsT=wbd[:, kc, m * 128:(m + 1) * 128], rhs=odT[:, kc, G * 512:(G + 1) * 512], start=(kc == 0), stop=(kc == 3))
            S.dma(out=gnb[b][:], in_=k.d["gnT_d"].ap()[m * 128:(m + 1) * 128, G * 512:(G + 1) * 512])
            S.dma(out=gdb[b][:], in_=k.d["gdT_d"].ap()[m * 128:(m + 1) * 128, G * 512:(G + 1) * 512])
            S.I("dve", "tensor_tensor", out=t1[b][:], in0=p1[:], in1=gnb[b][:], op=ALU.mult)
            S.I("dve", "tensor_tensor", out=t2[b][:], in0=p2[:], in1=gdb[b][:], op=ALU.mult)
            S.I("dve", "tensor_tensor", out=m_[:, m, :], in0=t1[b][:], in1=t2[b][:], op=ALU.add)
        for c in range(4):
            i = 4 * G + c
            b = i % 2
            pys = [k.ps[4], k.ps[5]]
            for half in range(2):
                for m in range(8):
                    S.I("pe", "matmul", out=pys[half][:], lhsT=m_[:, m, c * 128:(c + 1) * 128], rhs=wo[:, m, half * 512:(half + 1) * 512],
                        start=(m == 0), stop=(m == 7))
            S.dma(out=xt[b][:], in_=k.inp["x"].ap()[i * 128:(i + 1) * 128, :])
            for half in range(2):
                S.I("dve", "tensor_tensor", out=yt[b][:, half * 512:(half + 1) * 512], in0=pys[half][:], in1=k.modb[:, 2 * DM + half * 512:2 * DM + (half + 1) * 512], op=ALU.mult)
            S.I("dve", "tensor_tensor", out=x1[b][:], in0=yt[b][:], in1=xt[b][:], op=ALU.add)
            S.dma(out=k.d["x1_d"].ap()[i * 128:(i + 1) * 128, :], in_=x1[b][:])
            rms_mod_tile(k, x1[b][:], k.modb[:, 4 * DM:5 * DM], k.modb[:, 3 * DM:4 * DM], h2[b][:], tmp[b][:], ss[b], junk[:])
            S.dma(out=k.d["h2_d"].ap()[i * 128:(i + 1) * 128, :], in_=h2[b][:])
    S.pop()
    return []


def phase_peer(k):
    S, nc = k.S, k.nc
    S.push()
    uv_d = k.d["uv_d"]
    wq = S.sbuf("pwq", [128, 8, DM], BF16)
    keysbd = S.sbuf("pkeysbd", [128, 8, 256], BF16)
    S.push()
    wstg = [S.sbuf(f"pwstg{i}", [128, DM], F32) for i in range(2)]
    for kc in range(8):
        st = wstg[kc % 2]
        S.dma(out=st[:], in_=k.inp["peer_w_query"].ap()[kc * 128:(kc + 1) * 128, :])
        S.I("pool", "tensor_copy", out=wq[:, kc, :], in_=st[:])
    kst = S.sbuf("pkst", [128, 8, 256], F32)
    S.I("pool", "memset", kst[:], 0.0, extra_writes=[kst[:]])
    for h in range(8):
        S.dma(out=kst[0:64, h, 0:128], in_=k.inp["peer_keys1"].ap()[h].rearrange("k d -> d k"), allow_slow_non_contiguous=True)
        S.dma(out=kst[64:128, h, 128:256], in_=k.inp["peer_keys2"].ap()[h].rearrange("k d -> d k"), allow_slow_non_contiguous=True)
    S.I("pool", "tensor_copy", out=keysbd[:], in_=kst[:])
    S.pop()
    fgb = S.sbuf("pfgb", [128, DM], F32)
    S.dma(out=fgb[:], in_=k.inp["final_g"].ap().partition_broadcast(128))
    iota16 = S.sbuf("piota", [128, 16], F32)
    S.dma(out=iota16[:], in_=k.cst["iota16"].ap())
    NB = 16
    gbuf = [S.sbuf(f"pgb{i}", [128, 2 * DM], BF16) for i in range(NB)]
    diag = [S.sbuf(f"pdiag{i}", [128, 128], BF16) for i in range(4)]
    h2t = [S.sbuf(f"ph2t{i}", [128, DM], F32) for i in range(2)]
    h2b = [S.sbuf(f"ph2b{i}", [128, DM], BF16) for i in range(2)]
    h2T = [S.sbuf(f"ph2T{i}", [128, 8, 128], BF16) for i in range(2)]
    qryT = [S.sbuf(f"pqryT{i}", [128, 8, 128], BF16) for i in range(2)]
    scs = [S.sbuf(f"pscs{i}", [128, 8, 256], F32) for i in range(2)]
    s1r = S.sbuf("ps1r", [128, 128], F32)
    cand = S.sbuf("pcand", [128, 16, 16], F32)
    candr = S.sbuf("pcandr", [128, 16, 16], F32)
    v1 = S.sbuf("pv1", [128, 8, 16], F32)
    v2 = S.sbuf("pv2", [128, 8, 16], F32)
    i1 = S.sbuf("pi1", [128, 8, 16], U32)
    i2 = S.sbuf("pi2", [128, 8, 16], U32)
    ts = S.sbuf("pts", [128, 8, 16], F32)
    pos = S.sbuf("ppos", [128, 8, 16], U32)
    ra = S.sbuf("pra", [128, 8, 16], U32)
    rb = S.sbuf("prb", [128, 8, 16], U32)
    raf = S.sbuf("praf", [128, 8, 16], F32)
    rbf = S.sbuf("prbf", [128, 8, 16], F32)
    i1f = S.sbuf("pi1f", [128, 8, 16], F32)
    i2f = S.sbuf("pi2f", [128, 8, 16], F32)
    oh = S.sbuf("poh", [128, 8, 16, 16], F32)
    sel1 = S.sbuf("psel1", [128, 8, 16], F32)
    sel2 = S.sbuf("psel2", [128, 8, 16], F32)
    eidf = S.sbuf("peidf", [128, 128], F32)
    eidx = [S.sbuf(f"peidx{i}", [128, 128], I32) for i in range(2)]
    gate = [S.sbuf(f"pgate{i}", [128, 8, 16], F32) for i in range(2)]
    gsm = S.sbuf("pgsm", [128, 32], F32)
    av = [S.sbuf(f"pav{i}", [128, 128], F32) for i in range(2)]
    gt1 = S.sbuf("pgt1", [128, 128], F32)
    gt2 = S.sbuf("pgt2", [128, 128], F32)
    coef = [S.sbuf(f"pcoef{i}", [128, 128], F32) for i in range(2)]
    junk = S.sbuf("pjunk", [128, DM], F32)
    junk2 = S.sbuf("pjunk2", [128, DM], F32)
    acc = [S.sbuf(f"pacc{i}", [128, DM], F32) for i in range(2)]
    x1t = [S.sbuf(f"px1t{i}", [128, DM], F32) for i in range(2)]
    ss = [S.sbuf(f"pss{i}", [128, 4], F32) for i in range(2)]
    outs = []
    nt_run = NT if k.sub != "peer1" else 1
    gcount = [0]

    def prep(i):
        b = i % 2
        S.dma(out=h2t[b][:], in_=k.d["h2_d"].ap()[i * 128:(i + 1) * 128, :])
        S.I("act", "activation", out=h2b[b][:], in_=h2t[b][:], func=AF.Copy)
        pt = k.pb[b]
        ptv = pt[:].rearrange("p (a b) -> p a b", a=8)
        for kc in range(8):
            S.I("pe", "transpose", out=ptv[:, kc, :], in_=h2b[b][:, kc * 128:(kc + 1) * 128], identity=k.ident_b[:])
        evac(k, h2T[b][:], ptv, eng="act")
        for hh in range(2):
            pq = k.ps[hh]
            for h4 in range(4):
                h = hh * 4 + h4
                for kc in range(8):
                    S.I("pe", "matmul", out=pq[:, h4 * 128:(h4 + 1) * 128], lhsT=wq[:, kc, h * 128:(h + 1) * 128], rhs=h2T[b][:, kc, :],
                        start=(kc == 0), stop=(kc == 7))
            evac(k, qryT[b][:, hh * 4:hh * 4 + 4, :], pq[:].rearrange("p (a b) -> p a b", a=4), eng="act")
        for h2_ in range(4):
            psc = k.ps[2 + (h2_ % 2)]
            for e in range(2):
                h = h2_ * 2 + e
                S.I("pe", "matmul", out=psc[:, e * 256:(e + 1) * 256], lhsT=qryT[b][:, h, :], rhs=keysbd[:, h, :], start=True, stop=True)
            S.I("dve", "tensor_copy", out=scs[b][:, 2 * h2_:2 * h2_ + 2, :], in_=psc[:].rearrange("p (a b) -> p a b", a=2))
        for h in range(8):
            for (vv, ii, off) in ((v1, i1, 0), (v2, i2, 128)):
                s_in = scs[b][:, h, off:off + 128]
                S.I("dve", "max", out=vv[:, h, 0:8], in_=s_in)
                S.I("dve", "max_index", out=ii[:, h, 0:8], in_max=vv[:, h, 0:8], in_values=s_in)
                S.I("dve", "match_replace", out=s1r[:], in_to_replace=vv[:, h, 0:8], in_values=s_in, imm_value=-3.0e38)
                S.I("dve", "max", out=vv[:, h, 8:16], in_=s1r[:])
                S.I("dve", "max_index", out=ii[:, h, 8:16], in_max=vv[:, h, 8:16], in_values=s1r[:])
            S.I("dve", "tensor_tensor", out=cand[:], in0=v1[:, h, :].unsqueeze(2).to_broadcast([128, 16, 16]),
                in1=v2[:, h, :].unsqueeze(1).to_broadcast([128, 16, 16]), op=ALU.add)
            cf = cand[:].rearrange("p a b -> p (a b)")
            crf = candr[:].rearrange("p a b -> p (a b)")
            S.I("dve", "max", out=ts[:, h, 0:8], in_=cf)
            S.I("dve", "max_index", out=pos[:, h, 0:8], in_max=ts[:, h, 0:8], in_values=cf)
            S.I("dve", "match_replace", out=crf, in_to_replace=ts[:, h, 0:8], in_values=cf, imm_value=-3.0e38)
            S.I("dve", "max", out=ts[:, h, 8:16], in_=crf)
            S.I("dve", "max_index", out=pos[:, h, 8:16], in_max=ts[:, h, 8:16], in_values=crf)
        g_ = gate[b]
        S.I("dve", "tensor_tensor", out=g_[:], in0=ts[:], in1=ts[:, :, 0:1].to_broadcast([128, 8, 16]), op=ALU.subtract)
        S.I("act", "activation", out=g_[:], in_=g_[:], func=AF.Exp)
        S.I("dve", "tensor_reduce", out=gsm[:, 0:8], in_=g_[:], axis=AX.X, op=ALU.add)
        S.I("dve", "reciprocal", out=gsm[:, 8:16], in_=gsm[:, 0:8])
        S.I("dve", "tensor_tensor", out=g_[:], in0=g_[:], in1=gsm[:, 8:16].unsqueeze(2).to_broadcast([128, 8, 16]), op=ALU.mult)
        S.I("dve", "tensor_single_scalar", out=ra[:], in_=pos[:], scalar=4, op=ALU.logical_shift_right)
        S.I("dve", "tensor_single_scalar", out=rb[:], in_=pos[:], scalar=15, op=ALU.bitwise_and)
        for (src, dst) in ((ra, raf), (rb, rbf), (i1, i1f), (i2, i2f)):
            S.I("dve", "tensor_copy", out=dst[:], in_=src[:])
        iob = iota16[:].unsqueeze(1).unsqueeze(1).to_broadcast([128, 8, 16, 16])
        for (rf, idf_, sel) in ((raf, i1f, sel1), (rbf, i2f, sel2)):
            S.I("dve", "tensor_tensor", out=oh[:], in0=rf[:].unsqueeze(3).to_broadcast([128, 8, 16, 16]), in1=iob, op=ALU.is_equal)
            S.I("dve", "tensor_tensor", out=oh[:], in0=oh[:], in1=idf_[:].unsqueeze(2).to_broadcast([128, 8, 16, 16]), op=ALU.mult)
            S.I("dve", "tensor_reduce", out=sel[:], in_=oh[:], axis=AX.X, op=ALU.add)
        S.I("dve", "scalar_tensor_tensor", out=eidf[:], in0=sel1[:].rearrange("p a b -> p (a b)"), scalar=128.0,
            in1=sel2[:].rearrange("p a b -> p (a b)"), op0=ALU.mult, op1=ALU.add)
        S.I("dve", "tensor_copy", out=eidx[b][:], in_=eidf[:])

    def evalx(i, mid=None):
        b = i % 2
        py = [k.ps[4], k.ps[5]]
        for grp in range(16):
            sl = slice(grp * 8, grp * 8 + 8)
            bufs = []
            for jj in range(8):
                j = grp * 8 + jj
                gb = gbuf[gcount[0] % NB]
                gcount[0] += 1
                bufs.append(gb)
                S.idma(out=gb[:], in_=uv_d.ap(), idx_ap=eidx[b][:, j:j + 1])
                S.I("dve", "scalar_tensor_tensor", out=junk2[:], in0=h2t[b][:], scalar=1.0, in1=gb[:, 0:DM], op0=ALU.mult, op1=ALU.mult,
                    accum_out=av[b][:, j:j + 1])
            S.I("dve", "tensor_tensor", out=gt1[:, sl], in0=av[b][:, sl], in1=av[b][:, sl], op=ALU.mult)
            S.I("dve", "tensor_scalar", out=gt1[:, sl], in0=gt1[:, sl], scalar1=0.044715, scalar2=1.0, op0=ALU.mult, op1=ALU.add)
            S.I("dve", "tensor_tensor", out=gt2[:, sl], in0=gt1[:, sl], in1=av[b][:, sl], op=ALU.mult)
            S.I("act", "activation", out=gt1[:, sl], in_=gt2[:, sl], func=AF.Sigmoid, scale=GELU_C)
            S.I("dve", "tensor_tensor", out=gt2[:, sl], in0=gt1[:, sl], in1=av[b][:, sl], op=ALU.mult)
            S.I("dve", "tensor_tensor", out=coef[b][:, sl], in0=gt2[:, sl], in1=gate[b][:].rearrange("p a b -> p (a b)")[:, sl], op=ALU.mult)
            for jj in range(8):
                j = grp * 8 + jj
                dg = diag[j % 4]
                S.I("act", "activation", out=dg[:], in_=k.ident_b[:], func=AF.Copy, scale=coef[b][:, j:j + 1])
                for half in range(2):
                    S.I("pe", "matmul", out=py[half][:], lhsT=dg[:], rhs=bufs[jj][:, DM + half * 512:DM + (half + 1) * 512],
                        start=(j == 0), stop=(j == 127))
            if mid is not None and grp == 7:
                mid()
        return py

    def final(i, py):
        b = i % 2
        S.dma(out=x1t[b][:], in_=k.d["x1_d"].ap()[i * 128:(i + 1) * 128, :])
        for half in range(2):
            S.I("dve", "tensor_tensor", out=acc[b][:, half * 512:(half + 1) * 512], in0=py[half][:], in1=k.modb[:, 5 * DM + half * 512:5 * DM + (half + 1) * 512], op=ALU.mult)
        S.I("dve", "tensor_tensor", out=x1t[b][:], in0=acc[b][:], in1=x1t[b][:], op=ALU.add)
        s_ = ss[b]
        S.I("act", "activation", out=junk[:], in_=x1t[b][:], func=AF.Square, accum_out=s_[:, 0:1])
        S.I("dve", "tensor_scalar", out=s_[:, 1:2], in0=s_[:, 0:1], scalar1=1.0 / DM, scalar2=EPS, op0=ALU.mult, op1=ALU.add)
        S.I("act", "activation", out=s_[:, 2:3], in_=s_[:, 1:2], func=AF.Sqrt)
        S.I("dve", "reciprocal", out=s_[:, 3:4], in_=s_[:, 2:3])
        S.I("dve", "scalar_tensor_tensor", out=acc[b][:], in0=x1t[b][:], scalar=s_[:, 3:4], in1=fgb[:], op0=ALU.mult, op1=ALU.mult)
        outs.append(S.dma(out=k.out.ap()[i * 128:(i + 1) * 128, :], in_=acc[b][:]))

    prep(0)
    for i in range(nt_run):
        py = evalx(i, mid=(lambda i=i: prep(i + 1)) if i + 1 < nt_run else None)
        final(i, py)
    S.pop()
    return outs


_CACHE = {}


def make_in_maps(inputs, consts, used=None):
    maps = []
    for b in range(8):
        m = {}
        for n, shp in INPUT_SPECS.items():
            if used is not None and n not in used:
                continue
            a = np.asarray(inputs[n])
            if n == "x" or n == "c":
                a = a[b]
            elif n in ("rel_bias", "final_g"):
                pass
            else:
                a = a[0]
            m[n] = np.ascontiguousarray(a.reshape(shp).astype(np.float32, copy=False))
        for n in CONST_SPECS:
            if used is None or n in used:
                m[n] = consts[n]
        maps.append(m)
    return maps


def kernel(**inputs):
    consts = host_consts()
    nc = build_program()
    res = run_bass_kernel_spmd(nc, make_in_maps(inputs, consts, nc.used_inputs), core_ids=list(range(8)))
    return np.stack([np.asarray(r["out"]).reshape(SEQ, DM) for r in res.results], axis=0).astype(np.float32)
```
